# Optimizing a Trainium2 kernel written in Bass

```python
import math
import jax, jax.numpy as jnp
from jax import lax
import numpy as np

D_MODEL = 1024
BATCH = 2
SEQ = 8192
DEPTH = 1

HEAD_DIM = 64
RWKV_WIDTH = D_MODEL // 2
MOBA_WIDTH = D_MODEL - RWKV_WIDTH
RWKV_HEADS = RWKV_WIDTH // HEAD_DIM
MOBA_HEADS = MOBA_WIDTH // HEAD_DIM
DECAY_RANK = 32
AAA_RANK = 32
GATE_RANK = 96
GN_EPS = 64e-5
L2_EPS = 1e-12
MOBA_BLOCK = 256
MOBA_TOPK = 3
Q_CHUNK = 128
N_GROUPS = 4
EXPERTS_PER_GROUP = 8
D_EXPERT = 256
EXPERT_TOPK = 2
LN_EPS = 1e-5
DEEPNORM_ALPHA = float((2 * DEPTH) ** 0.25)
DEEPNORM_BETA = float((8 * DEPTH) ** -0.25)
NEG_INF = -1e30

RWKV_SPLITS = (RWKV_WIDTH, RWKV_WIDTH, RWKV_WIDTH, DECAY_RANK, AAA_RANK, GATE_RANK)
MOBA_SPLITS = (MOBA_WIDTH, MOBA_WIDTH, MOBA_WIDTH)
RWKV_COLS = sum(RWKV_SPLITS)
IN_COLS = RWKV_COLS + sum(MOBA_SPLITS)

kernel_name = "rwkv7_moba_hier_moe_deepnorm"


def _split(p, sizes):
    idx = [int(i) for i in np.cumsum(sizes)[:-1]]
    return jnp.split(p, idx, axis=-1)


def _layer_norm(x, g, b):
    xf = x.astype(jnp.float32)
    mu = jnp.mean(xf, axis=-1, keepdims=True)
    var = jnp.mean(jnp.square(xf - mu), axis=-1, keepdims=True)
    y = (xf - mu) * lax.rsqrt(var + LN_EPS) * g.astype(jnp.float32) + b.astype(jnp.float32)
    return y.astype(x.dtype)


def _rwkv7_group(p_r, p_k, p_v, p_wd, p_ad, p_gd, w0, w_lora_up, a0, a_lora_up,
                 g_lora_up, k_k, k_a, r_k, gn_w, gn_b):
    f32 = jnp.float32
    B, T, C = p_r.shape
    H, N = RWKV_HEADS, HEAD_DIM
    r, k, v = p_r.astype(f32), p_k.astype(f32), p_v.astype(f32)
    w_log = -jax.nn.softplus(-(w0.astype(f32) + jnp.tanh(p_wd.astype(f32)) @ w_lora_up.astype(f32))) - 0.5
    decay = jnp.exp(-jnp.exp(w_log))
    a = jax.nn.sigmoid(a0.astype(f32) + p_ad.astype(f32) @ a_lora_up.astype(f32))
    g = jax.nn.sigmoid(p_gd.astype(f32)) @ g_lora_up.astype(f32)
    kk = (k * k_k.astype(f32)).reshape(B, T, H, N)
    kk = kk / jnp.maximum(jnp.linalg.norm(kk, axis=-1, keepdims=True), L2_EPS)
    k = k * (1.0 + (a - 1.0) * k_a.astype(f32))
    hd = lambda t: t.reshape(B, T, H, N)
    r_h, w_h, k_h, v_h, a_h = hd(r), hd(decay), hd(k), hd(v), hd(a)
    seq = tuple(t.transpose(1, 0, 2, 3) for t in (r_h, w_h, k_h, v_h, -kk, kk * a_h))

    def step(S, inp):
        rt, wt, kt, vt, at, bt = inp
        sa = jnp.einsum('bhij,bhj->bhi', S, at)
        S = S * wt[:, :, None, :] + sa[..., None] * bt[:, :, None, :] + vt[..., None] * kt[:, :, None, :]
        yt = jnp.einsum('bhij,bhj->bhi', S, rt)
        return S, yt

    S0 = jnp.zeros((B, H, N, N), f32)
    _, y = lax.scan(step, S0, seq)
    y = y.transpose(1, 0, 2, 3)
    mu = jnp.mean(y, axis=-1, keepdims=True)
    var = jnp.mean(jnp.square(y - mu), axis=-1, keepdims=True)
    y = ((y - mu) * lax.rsqrt(var + GN_EPS)).reshape(B, T, C) * gn_w.astype(f32) + gn_b.astype(f32)
    bonus = jnp.sum(r_h * k_h * r_k.astype(f32), axis=-1, keepdims=True) * v_h
    y = (y + bonus.reshape(B, T, C)) * g
    return y.astype(p_r.dtype)


def _moba_group(p_q, p_k, p_v):
    f32 = jnp.float32
    B, T, _ = p_q.shape
    H, Dh, L = MOBA_HEADS, HEAD_DIM, MOBA_BLOCK
    NB = T // L
    NC = T // Q_CHUNK
    K_EFF = min(MOBA_TOPK, NB)
    scale = 1.0 / math.sqrt(Dh)
    q = p_q.reshape(B, T, H, Dh).transpose(0, 2, 1, 3)
    Kb = p_k.reshape(B, T, H, Dh).transpose(0, 2, 1, 3).reshape(B, H, NB, L, Dh)
    Vb = p_v.reshape(B, T, H, Dh).transpose(0, 2, 1, 3).reshape(B, H, NB, L, Dh)
    kmean = jnp.mean(Kb.astype(f32), axis=3)
    slopes = 2.0 ** (-8.0 * (jnp.arange(H, dtype=f32) + 1.0) / H)
    gather = jax.vmap(jax.vmap(lambda kb, i: kb[i]))

    def chunk(c):
        start = c * Q_CHUNK
        q_c = lax.dynamic_slice_in_dim(q, start, Q_CHUNK, axis=2)
        t_pos = start + jnp.arange(Q_CHUNK)
        blk = start // L
        gate = jnp.einsum('bhcd,bhnd->bhcn', q_c.astype(f32), kmean)
        gate = jnp.where(jnp.arange(NB) < blk, gate, NEG_INF)
        _, idx = lax.top_k(gate, K_EFF)
        valid = jnp.arange(K_EFF) < blk
        K_sel = gather(Kb, idx)
        V_sel = gather(Vb, idx)
        s_sel = jnp.einsum('bhcd,bhckld->bhckl', q_c, K_sel).astype(f32) * scale
        key_pos = idx[..., None] * L + jnp.arange(L)
        dist = (t_pos[None, None, :, None, None] - key_pos).astype(f32)
        s_sel = s_sel - slopes[None, :, None, None, None] * dist
        s_sel = jnp.where(valid[None, None, None, :, None], s_sel, NEG_INF)
        s_sel = s_sel.reshape(B, H, Q_CHUNK, K_EFF * L)
        K_own = lax.dynamic_index_in_dim(Kb, blk, axis=2, keepdims=False)
        V_own = lax.dynamic_index_in_dim(Vb, blk, axis=2, keepdims=False)
        own_pos = blk * L + jnp.arange(L)
        d_own = (t_pos[:, None] - own_pos[None, :]).astype(f32)
        s_own = jnp.einsum('bhcd,bhld->bhcl', q_c, K_own).astype(f32) * scale
        s_own = s_own - slopes[None, :, None, None] * d_own[None, None]
        s_own = jnp.where((d_own >= 0)[None, None], s_own, NEG_INF)
        p = jax.nn.softmax(jnp.concatenate([s_sel, s_own], axis=-1), axis=-1)
        p_sel = p[..., :K_EFF * L].reshape(B, H, Q_CHUNK, K_EFF, L).astype(V_sel.dtype)
        p_own = p[..., K_EFF * L:].astype(V_own.dtype)
        out = (jnp.einsum('bhckl,bhckld->bhcd', p_sel, V_sel)
               + jnp.einsum('bhcl,bhld->bhcd', p_own, V_own))
        return out

    out = lax.map(chunk, jnp.arange(NC))
    out = out.transpose(1, 0, 3, 2, 4).reshape(B, T, H * Dh)
    return out


def _hier_moe(h, w_group, b_group, w_expert, b_expert, w1_exp, w3_exp, w2_exp):
    f32 = jnp.float32
    B, T, D = h.shape
    tok = h.reshape(B * T, D)
    g_logits = (tok @ w_group).astype(f32) + b_group.astype(f32)
    g_prob = jax.nn.softmax(g_logits, axis=-1)
    p_g, g_idx = lax.top_k(g_prob, 1)
    g_onehot = jax.nn.one_hot(g_idx[:, 0], N_GROUPS, dtype=f32)
    e_logits = ((tok @ w_expert).astype(f32) + b_expert.astype(f32)).reshape(-1, N_GROUPS, EXPERTS_PER_GROUP)
    e_in_group = jnp.einsum('ng,nge->ne', g_onehot, e_logits)
    e_prob = jax.nn.softmax(e_in_group, axis=-1)
    e_val, e_idx = lax.top_k(e_prob, EXPERT_TOPK)
    e_w = e_val / jnp.sum(e_val, axis=-1, keepdims=True) * p_g
    within = jnp.sum(jax.nn.one_hot(e_idx, EXPERTS_PER_GROUP, dtype=f32) * e_w[..., None], axis=1)
    gates = g_onehot[:, :, None] * within[:, None, :]
    gates = gates.astype(tok.dtype)
    out = jnp.zeros_like(tok)
    for gi in range(N_GROUPS):
        hid = jax.nn.silu(jnp.einsum('nd,edf->nef', tok, w1_exp[gi])) * jnp.einsum('nd,edf->nef', tok, w3_exp[gi])
        out = out + jnp.einsum('nef,efd->nd', hid * gates[:, gi, :, None], w2_exp[gi])
    return out.reshape(B, T, D)


def setup_inputs(seed: int = 0) -> dict:
    key = jax.random.key(seed)
    ks = jax.random.split(key, 32)
    f32 = jnp.float32
    D = D_MODEL
    nrm = lambda k, shape, s: jax.random.normal(k, shape, f32) * s
    return {
        "x": nrm(ks[0], (BATCH, SEQ, D), 1.0),
        "w_in": nrm(ks[1], (D, IN_COLS), D ** -0.5),
        "mu_shift": jax.random.uniform(ks[2], (RWKV_COLS,), f32),
        "w0": nrm(ks[3], (RWKV_WIDTH,), 0.5),
        "w_lora_up": nrm(ks[4], (DECAY_RANK, RWKV_WIDTH), 0.1 * DECAY_RANK ** -0.5),
        "a0": nrm(ks[5], (RWKV_WIDTH,), 0.1),
        "a_lora_up": nrm(ks[6], (AAA_RANK, RWKV_WIDTH), 0.1 * AAA_RANK ** -0.5),
        "g_lora_up": nrm(ks[7], (GATE_RANK, RWKV_WIDTH), GATE_RANK ** -0.5),
        "k_k": 0.85 + nrm(ks[8], (RWKV_WIDTH,), 0.05),
        "k_a": 1.0 + nrm(ks[9], (RWKV_WIDTH,), 0.05),
        "r_k": nrm(ks[10], (RWKV_HEADS, HEAD_DIM), 0.1),
        "gn_w": 1.0 + nrm(ks[11], (RWKV_WIDTH,), 0.01),
        "gn_b": nrm(ks[12], (RWKV_WIDTH,), 0.01),
        "w_out": nrm(ks[13], (D, D), DEEPNORM_BETA * D ** -0.5),
        "ln1_g": 1.0 + nrm(ks[14], (D,), 0.01),
        "ln1_b": nrm(ks[15], (D,), 0.01),
        "w_group": nrm(ks[16], (D, N_GROUPS), D ** -0.5),
        "b_group": nrm(ks[17], (N_GROUPS,), 0.01),
        "w_expert": nrm(ks[18], (D, N_GROUPS * EXPERTS_PER_GROUP), D ** -0.5),
        "b_expert": nrm(ks[19], (N_GROUPS * EXPERTS_PER_GROUP,), 0.01),
        "w1_exp": nrm(ks[20], (N_GROUPS, EXPERTS_PER_GROUP, D, D_EXPERT), D ** -0.5),
        "w3_exp": nrm(ks[21], (N_GROUPS, EXPERTS_PER_GROUP, D, D_EXPERT), D ** -0.5),
        "w2_exp": nrm(ks[22], (N_GROUPS, EXPERTS_PER_GROUP, D_EXPERT, D), DEEPNORM_BETA * D_EXPERT ** -0.5),
        "ln2_g": 1.0 + nrm(ks[23], (D,), 0.01),
        "ln2_b": nrm(ks[24], (D,), 0.01),
    }


def reference(x, w_in, mu_shift, w0, w_lora_up, a0, a_lora_up, g_lora_up, k_k, k_a, r_k,
              gn_w, gn_b, w_out, ln1_g, ln1_b, w_group, b_group, w_expert, b_expert,
              w1_exp, w3_exp, w2_exp, ln2_g, ln2_b):
    h = x
    for _ in range(DEPTH):
        p = h @ w_in
        p_rwkv, p_moba = p[..., :RWKV_COLS], p[..., RWKV_COLS:]
        p_prev = jnp.pad(p_rwkv[:, :-1], ((0, 0), (1, 0), (0, 0)))
        p_rwkv = p_rwkv + (p_prev - p_rwkv) * mu_shift
        p_r, p_k, p_v, p_wd, p_ad, p_gd = _split(p_rwkv, RWKV_SPLITS)
        y_a = _rwkv7_group(p_r, p_k, p_v, p_wd, p_ad, p_gd, w0, w_lora_up, a0, a_lora_up,
                           g_lora_up, k_k, k_a, r_k, gn_w, gn_b)
        q_m, k_m, v_m = _split(p_moba, MOBA_SPLITS)
        y_b = _moba_group(q_m, k_m, v_m)
        mix = jnp.concatenate([y_a, y_b.astype(y_a.dtype)], axis=-1) @ w_out
        h = _layer_norm(DEEPNORM_ALPHA * h + mix, ln1_g, ln1_b)
        ffn = _hier_moe(h, w_group, b_group, w_expert, b_expert, w1_exp, w3_exp, w2_exp)
        h = _layer_norm(DEEPNORM_ALPHA * h + ffn, ln2_g, ln2_b)
    return h
```

```python
import numpy as np
import ml_dtypes
from contextlib import ExitStack
import concourse.bass as bass
import concourse.mybir as mybir
from concourse.bass_utils import run_bass_kernel_spmd

F32 = mybir.dt.float32
BF16 = mybir.dt.bfloat16
ALU = mybir.AluOpType
AF = mybir.ActivationFunctionType
AX = mybir.AxisListType

D = 1024
ALPHA = float(2.0 ** 0.25)
LN_EPS = 1e-5


class _Rec:
    def __init__(self):
        self.call = None

    def __getattr__(self, name):
        def f(*a, **k):
            self.call = (name, a, k)
            return self
        return f


class Sched:
    SELF_SYNC = True

    def __init__(self, nc, es, nlanes=10):
        self.nc = nc
        self.E = {}
        self.src = {}
        for n in ("pe", "act", "dve", "pool", "sp"):
            sem = es.enter_context(nc.semaphore("sem_" + n))
            self.E[n] = dict(name=n, sem=sem, cnt=0, prog=[], waited={})
            self.src[n] = (sem, 1)
        self.lanes = {}
        for q in ("sp", "pool"):
            L = []
            for i in range(nlanes):
                lid = "L%s%d" % (q, i)
                sem = es.enter_context(nc.semaphore("sem_" + lid))
                self.src[lid] = (sem, 16)
                L.append(dict(id=lid, cnt=0))
            self.lanes[q] = dict(lanes=L, nxt=0)
        self.B = {}
        csem = es.enter_context(nc.semaphore("sem_coll"))
        self.src["coll"] = (csem, 1)
        self.ncoll = 0

    def coll(self, fn, reads=(), writes=()):
        e = self.E["pool"]
        self._emit_waits(e, self._deps(reads, writes))
        self.ncoll += 1
        r = _Rec()
        fn(r)
        e["prog"].append(("coll", r.call))
        self._mark(("coll", self.ncoll), reads, writes)

    def barrier(self, include_coll=True):
        for e in self.E.values():
            deps = {}
            for n, o in self.E.items():
                if n != e["name"] and o["cnt"] > 0:
                    deps[n] = o["cnt"]
            for q in self.lanes.values():
                for lane in q["lanes"]:
                    if lane["cnt"] > 0:
                        deps[lane["id"]] = lane["cnt"]
            if self.ncoll > 0 and include_coll:
                deps["coll"] = self.ncoll
            self._emit_waits(e, deps)

    @staticmethod
    def is_psum(k):
        return isinstance(k, tuple) and isinstance(k[0], str) and k[0].startswith("P")

    def _buf(self, k):
        b = self.B.get(k)
        if b is None:
            b = self.B[k] = dict(w=None, r={})
        return b

    def _deps(self, reads, writes):
        deps = {}

        def add(t):
            if t is None:
                return
            s, n = t
            if deps.get(s, 0) < n:
                deps[s] = n
        for k in reads:
            b = self._buf(k)
            add(b["w"])
            if self.is_psum(k):
                for s, n in b["r"].items():
                    add((s, n))
        for k in writes:
            b = self._buf(k)
            add(b["w"])
            for s, n in b["r"].items():
                add((s, n))
        return deps

    def _emit_waits(self, e, deps):
        for s, n in deps.items():
            if s == e["name"]:
                if s == "pe" or not self.SELF_SYNC or n > e["cnt"]:
                    continue
            if e["waited"].get(s, 0) < n:
                e["prog"].append(("wait", s, n))
                e["waited"][s] = n

    def _mark(self, t, reads, writes):
        s, n = t
        for k in reads:
            b = self._buf(k)
            if b["r"].get(s, 0) < n:
                b["r"][s] = n
        for k in writes:
            b = self._buf(k)
            b["w"] = t
            b["r"] = {}

    def op(self, eng, fn, reads=(), writes=(), inc=True):
        e = self.E[eng]
        self._emit_waits(e, self._deps(reads, writes))
        t = (eng, e["cnt"] + 1)
        if inc:
            e["cnt"] += 1
        r = _Rec()
        fn(r)
        e["prog"].append(("inst", r.call, inc))
        self._mark(t, reads, writes)

    def dma(self, q, out, in_, reads=(), writes=()):
        e = self.E[q]
        self._emit_waits(e, self._deps(reads, writes))
        LL = self.lanes[q]
        lane = LL["lanes"][LL["nxt"]]
        LL["nxt"] = (LL["nxt"] + 1) % len(LL["lanes"])
        if lane["cnt"] > 0 and e["waited"].get(lane["id"], 0) < lane["cnt"]:
            e["prog"].append(("wait", lane["id"], lane["cnt"]))
            e["waited"][lane["id"]] = lane["cnt"]
        lane["cnt"] += 1
        t = (lane["id"], lane["cnt"])
        e["prog"].append(("dma", out, in_, lane["id"]))
        self._mark(t, reads, writes)

    def finish(self, keys):
        e = self.E["sp"]
        self._emit_waits(e, self._deps(keys, ()))

    def replay(self):
        nc = self.nc
        with nc.Block() as block:
            def run(name):
                def f(eng):
                    mysem = self.E[name]["sem"]
                    start = self.E[name].get("done", 0)
                    self.E[name]["done"] = len(self.E[name]["prog"])
                    for it in self.E[name]["prog"][start:]:
                        if it[0] == "wait":
                            sem, sc = self.src[it[1]]
                            eng.wait_ge(sem, it[2] * sc)
                        elif it[0] == "inst":
                            name_, a_, k_ = it[1]
                            ins = getattr(eng, name_)(*a_, **k_)
                            if it[2]:
                                ins.then_inc(mysem, 1)
                        elif it[0] == "coll":
                            name_, a_, k_ = it[1]
                            getattr(eng, name_)(*a_, **k_).then_inc(self.src["coll"][0], 1)
                        else:
                            sem, _ = self.src[it[3]]
                            eng.dma_start(out=it[1], in_=it[2]).then_inc(sem, 16)
                return f
            block.tensor(run("pe"))
            block.scalar(run("act"))
            block.vector(run("dve"))
            block.gpsimd(run("pool"))
            block.sync(run("sp"))


class _Cut(Exception):
    pass


def token_phase(S, nc, es, T, dr, NE=32, do_route=True):
    import os
    CUT = int(os.environ.get("TOKEN_CUT", "0"))

    def cut(k, keys):
        if CUT == k:
            S.finish(keys)
            raise _Cut()

    NT = T // 128
    NG = T // 512

    def sb(name, shape, dt):
        return es.enter_context(nc.sbuf_tensor(name, shape, dt))

    P = [es.enter_context(nc.psum_tensor("tP%d" % i, [128, 1024], F32)) for i in range(4)]

    ident = sb("t_ident", [128, 128], F32)
    ones1 = sb("t_ones1", [1, 128], F32)
    wout = sb("t_wout", [128, 8, 1024], BF16)
    lnp = sb("t_lnp", [128, 2, 1024], F32)
    stg0 = sb("t_stg0", [128, 2048], F32)
    stg = None
    wr = sb("t_wr", [128, 8, 36], F32)
    br = sb("t_br", [1, 36], F32)
    ycF = sb("t_ycT", [128, max(8 * T, 16384)], BF16)
    ycT = ycF[:, 0:8 * T].rearrange("p (a b) -> p a b", a=8)
    h1T = sb("t_h1T", [128, 8, T], BF16)
    acc = sb("t_acc", [128, NT, 1024], F32)
    xb = [sb("t_xb%d" % i, [128, 1024], F32) for i in range(2)]
    hb1 = sb("t_hb", [128, 1024], F32)
    hb = [hb1, hb1]
    h1b1 = sb("t_h1b", [128, 1024], F32)
    h1b = [h1b1, h1b1]
    hTf = sb("t_hTf", [128, 8, 128], F32)
    st6 = sb("t_st6", [128, 2, 6], F32)
    mv = sb("t_mv", [128, 2], F32)
    sm = sb("t_sm", [128, 8], F32)
    rlog = sb("t_rlog", [128, NT, 36], F32)
    gates = sb("t_gates", [128, NT, 32], F32)
    rt = sb("t_rt", [128, 12, NT, 8], F32)
    w1b = [ycF[:, (0 + i) * 2048:(1 + i) * 2048].rearrange("p (a b) -> p a b", a=8) for i in range(2)]
    w3b = [ycF[:, (2 + i) * 2048:(3 + i) * 2048].rearrange("p (a b) -> p a b", a=8) for i in range(2)]
    w2b = [ycF[:, (4 + i) * 2048:(5 + i) * 2048].rearrange("p (a b) -> p a b", a=2) for i in range(2)]
    ssb = [sb("t_ssb%d" % i, [128, 512], F32) for i in range(2)]
    hidT = [sb("t_hidT%d" % i, [128, 2, 512], BF16) for i in range(2)]
    ob = xb

    S.dma("sp", ident[:], dr["ident"], writes=["ident"])
    S.op("pool", lambda e: e.memset(ones1[:], 1.0), writes=["ones1"])
    for i in range(4):
        S.dma("sp", stg0[:].rearrange("p (a b) -> p a b", a=2),
              dr["w_out"].rearrange("(kc p) n -> p kc n", p=128)[:, 2 * i:2 * i + 2, :], writes=["stg0"])
        S.op("pool", lambda e, i=i: e.tensor_copy(wout[:, 2 * i, :], stg0[:, 0:1024]), reads=["stg0"], writes=[("wout", 2 * i)])
        S.op("dve", lambda e, i=i: e.tensor_copy(wout[:, 2 * i + 1, :], stg0[:, 1024:2048]), reads=["stg0"], writes=[("wout", 2 * i + 1)])
    for i, nm in enumerate(("ln1_g", "ln1_b")):
        S.dma("sp", lnp[:, i, :], dr[nm].partition_broadcast(128), writes=["lnp"])
    S.dma("sp", wr[:], dr["w_route"].rearrange("(kc p) n -> p kc n", p=128), writes=["wr"])
    S.dma("sp", br[:], dr["b_route"].unsqueeze(0), writes=["br"])
    def load_ycT(q):
        if "ycat_q" in dr:
            S.dma("sp", ycT[:, :, q * 512:(q + 1) * 512], dr["ycat_q"][q].rearrange("(kc p) t -> p kc t", p=128),
                  reads=[("rs_out", q)], writes=[("ycT", q)])
            return
        S.dma("sp", ycT[:, :, q * 512:(q + 1) * 512],
              dr["ycatT"].rearrange("(kc p) t -> p kc t", p=128)[:, :, q * 512:(q + 1) * 512],
              reads=dr.get("ycat_keys", []), writes=[("ycT", q)])

    stg = [stg0[:], ycF[:, 12288:16384].bitcast(F32)]
    pc = [0]

    def load_piece(dst, src, a, dkey, extra):
        k = pc[0] % 2
        pc[0] += 1
        S.dma("sp", stg[k].rearrange("p (a b) -> p a b", a=a), src, writes=["stg%d" % k] + (extra if k == 1 else []))
        S.op("pool", lambda e, k=k: e.tensor_copy(dst, stg[k].rearrange("p (a b) -> p a b", a=a)),
             reads=["stg%d" % k], writes=[dkey] + extra)

    def load_expert(e):
        s = e % 2
        yk = [("ycT", q) for q in range(NG)] if e < 2 else []
        load_piece(w1b[s], dr["w1"][e].rearrange("(kc p) f -> p kc f", p=128), 8, ("w1b", s), yk)
        load_piece(w3b[s], dr["w3"][e].rearrange("(kc p) f -> p kc f", p=128), 8, ("w3b", s), yk)
        load_piece(w2b[s], dr["w2"][e].rearrange("(fc p) d -> p fc d", p=128), 2, ("w2b", s), yk)

    def layer_norm(src_ap, srck, gi, dst_ap, dstk, tmp_ap, tmpk, eps=LN_EPS):
        for h in range(2):
            S.op("dve", lambda e, h=h: e.bn_stats(st6[:, h, :], src_ap[:, h * 512:(h + 1) * 512]),
                 reads=[srck], writes=["st6"])
        S.op("dve", lambda e: e.bn_aggr(mv[:], st6[:].rearrange("p a b -> p (a b)")), reads=["st6"], writes=["mv"])
        S.op("dve", lambda e: e.tensor_scalar(sm[:, 0:1], mv[:, 1:2], eps, None, ALU.add),
             reads=["mv"], writes=["sm0"])
        S.op("act", lambda e: e.activation(sm[:, 1:2], sm[:, 0:1], AF.Sqrt), reads=["sm0"], writes=["sm1"])
        S.op("dve", lambda e: e.reciprocal(sm[:, 2:3], sm[:, 1:2]), reads=["sm1"], writes=["sm2"])
        S.op("dve", lambda e: e.tensor_scalar(sm[:, 3:4], mv[:, 0:1], sm[:, 2:3], -1.0, ALU.mult, ALU.mult),
             reads=["mv", "sm2"], writes=["sm3"])
        S.op("act", lambda e: e.activation(tmp_ap, src_ap, AF.Identity, bias=sm[:, 3:4], scale=sm[:, 2:3]),
             reads=[srck, "sm2", "sm3"], writes=[tmpk])
        S.op("dve", lambda e: e.tensor_tensor(tmp_ap, tmp_ap, lnp[:, gi, :], ALU.mult),
             reads=[tmpk, "lnp"], writes=[tmpk])
        S.op("pool", lambda e: e.tensor_tensor(dst_ap, tmp_ap, lnp[:, gi + 1, :], ALU.add),
             reads=[tmpk, "lnp"], writes=[dstk])

    cut(1, ['ident', 'wout', 'lnp', 'wr', 'br', ('ycT', 0), ('ycT', NG - 1)])
    def t1_outproj(t):
        s = t % 2
        if t % 4 == 0:
            load_ycT(t // 4)
        S.dma("sp", xb[s][:], dr["x_tok"][t * 128:(t + 1) * 128, :], writes=[("xbt", s)])
        for half in range(2):
            for kc in range(8):
                S.op("pe", lambda e, half=half, kc=kc: e.matmul(
                    P[s][:, half * 512:(half + 1) * 512], lhsT=ycT[:, kc, t * 128:(t + 1) * 128],
                    rhs=wout[:, kc, half * 512:(half + 1) * 512], start=(kc == 0), stop=(kc == 7)),
                    reads=[("ycT", t // 4), ("wout", kc)], writes=[("P", s)], inc=(kc == 7 and half == 1))

    def t1_norm(t):
        s = t % 2
        hbt = (hb1, h1b1)[s]
        hk = ("hb", s)
        S.op("dve", lambda e: e.scalar_tensor_tensor(hbt[:], xb[s][:], ALPHA, P[s][:], ALU.mult, ALU.add),
             reads=[("xbt", s), ("P", s)], writes=[hk])
        layer_norm(hbt[:], hk, 0, acc[:, t, :], ("acc", t), hbt[:], hk)

    def t1_route(t):
        for j in range(8):
            S.op("pe", lambda e, j=j: e.transpose(P[2][:, j * 128:(j + 1) * 128], acc[:, t, j * 128:(j + 1) * 128], ident[:]),
                 reads=[("acc", t), "ident"], writes=[("PA", 0), ("PB", 0)], inc=(j == 7))
        S.op("act", lambda e: e.activation(h1T[:, :, t * 128:(t + 1) * 128],
                                           P[2][:].rearrange("p (a b) -> p a b", a=8), AF.Copy),
             reads=[("PA", 0), ("PB", 0)], writes=[("h1T", t // 4)])
        S.op("dve", lambda e: e.tensor_copy(hTf[:], P[2][:].rearrange("p (a b) -> p a b", a=8)),
             reads=[("PA", 0), ("PB", 0)], writes=["hTf"])
        for kc in range(8):
            S.op("pe", lambda e, kc=kc: e.matmul(P[3][:, 0:36], lhsT=hTf[:, kc, :], rhs=wr[:, kc, :],
                                                 start=(kc == 0), stop=False),
                 reads=["hTf", "wr"], writes=[("PA", 1), ("PB", 1)], inc=False)
        S.op("pe", lambda e: e.matmul(P[3][:, 0:36], lhsT=ones1[:], rhs=br[:], start=False, stop=True),
             reads=["ones1", "br"], writes=[("PA", 1), ("PB", 1)])
        S.op("act", lambda e: e.activation(rlog[:, t, :], P[3][:, 0:36], AF.Copy),
             reads=[("PA", 1), ("PB", 1)], writes=["rlog"])

    t1_outproj(0)
    if NT > 1:
        t1_outproj(1)
    t1_norm(0)
    for t in range(NT):
        if t + 1 < NT:
            t1_norm(t + 1)
        if t + 2 < NT:
            t1_outproj(t + 2)
        t1_route(t)
    if not do_route:
        S.op('dve', lambda e: e.memset(gates[:], 0.03), writes=['gates'])
    gl = rlog[:, :, 0:4]
    _op = S.op
    if not do_route:
        S.op = lambda *a, **k: None
    R = lambda i, w=8: rt[:, i, :, 0:w]
    R1 = lambda i: rt[:, i, :, 0]
    bc = lambda ap, w: ap.unsqueeze(2).to_broadcast([128, NT, w])

    def dv(fn, reads, writes):
        S.op("dve", fn, reads=reads, writes=writes)
    dv(lambda e: e.tensor_reduce(R1(0), gl, AX.X, ALU.max), ["rlog"], ["r0"])
    dv(lambda e: e.tensor_tensor(R(1, 4), gl, bc(R1(0), 4), ALU.is_equal), ["rlog", "r0"], ["r1"])
    dv(lambda e: e.tensor_tensor(R(2, 4), gl, bc(R1(0), 4), ALU.subtract), ["rlog", "r0"], ["r2"])
    S.op("act", lambda e: e.activation(R(2, 4), R(2, 4), AF.Exp), reads=["r2"], writes=["r2"])
    dv(lambda e: e.tensor_reduce(R1(3), R(2, 4), AX.X, ALU.add), ["r2"], ["r3"])
    for g in range(4):
        oh = rt[:, 1, :, g]
        el = rlog[:, :, 4 + g * 8:12 + g * 8]
        if g == 0:
            dv(lambda e, oh=oh, el=el: e.tensor_tensor(R(4), el, bc(oh, 8), ALU.mult), ["rlog", "r1"], ["r4"])
        else:
            dv(lambda e, oh=oh, el=el: e.tensor_tensor(R(5), el, bc(oh, 8), ALU.mult), ["rlog", "r1"], ["r5"])
            dv(lambda e: e.tensor_tensor(R(4), R(4), R(5), ALU.add), ["r4", "r5"], ["r4"])
    dv(lambda e: e.tensor_reduce(R1(6), R(4), AX.X, ALU.max), ["r4"], ["r6"])
    dv(lambda e: e.tensor_tensor(R(7), R(4), bc(R1(6), 8), ALU.is_equal), ["r4", "r6"], ["r7"])
    dv(lambda e: e.scalar_tensor_tensor(R(8), R(7), -1e30, R(4), ALU.mult, ALU.add), ["r7", "r4"], ["r8"])
    dv(lambda e: e.tensor_reduce(R1(9), R(8), AX.X, ALU.max), ["r8"], ["r9"])
    dv(lambda e: e.tensor_tensor(R(5), R(4), bc(R1(6), 8), ALU.subtract), ["r4", "r6"], ["r5"])
    S.op("act", lambda e: e.activation(R(5), R(5), AF.Exp), reads=["r5"], writes=["r5"])
    dv(lambda e: e.tensor_tensor(R(7), R(4), bc(R1(9), 8), ALU.is_ge), ["r4", "r9"], ["r7"])
    dv(lambda e: e.tensor_tensor(R(5), R(5), R(7), ALU.mult), ["r5", "r7"], ["r5"])
    dv(lambda e: e.tensor_reduce(R1(10), R(5), AX.X, ALU.add), ["r5"], ["r10"])
    dv(lambda e: e.tensor_tensor(R1(10), R1(10), R1(3), ALU.mult), ["r10", "r3"], ["r10"])
    dv(lambda e: e.tensor_scalar(R1(10), R1(10), ALPHA, None, ALU.mult), ["r10"], ["r10"])
    dv(lambda e: e.reciprocal(R1(11), R1(10)), ["r10"], ["r11"])
    dv(lambda e: e.tensor_tensor(R(5), R(5), bc(R1(11), 8), ALU.mult), ["r5", "r11"], ["r5"])
    for g in range(4):
        oh = rt[:, 1, :, g]
        dv(lambda e, oh=oh, g=g: e.tensor_tensor(gates[:, :, g * 8:(g + 1) * 8], R(5), bc(oh, 8), ALU.mult),
           ["r5", "r1"], ["gates"])

    S.op = _op
    if NE > 0:
        load_expert(0)
    if NE > 1:
        load_expert(1)
    PA = [P[2][:, 0:512], P[3][:, 0:512]]
    PB = [P[2][:, 512:1024], P[3][:, 512:1024]]
    it = 0
    pend = None
    for ex in range(NE):
        s = ex % 2
        for tg in range(NG):
            hs = (ex * NG + tg) % 2
            for fc in range(2):
                u = it % 2
                it += 1
                for kc in range(8):
                    S.op("pe", lambda e, kc=kc, fc=fc, tg=tg, s=s, u=u: e.matmul(
                        PA[u], lhsT=w1b[s][:, kc, fc * 128:(fc + 1) * 128], rhs=h1T[:, kc, tg * 512:(tg + 1) * 512],
                        start=(kc == 0), stop=(kc == 7)),
                        reads=[("w1b", s), ("h1T", tg)], writes=[("PA", u)], inc=(kc == 7))
                for kc in range(8):
                    S.op("pe", lambda e, kc=kc, fc=fc, tg=tg, s=s, u=u: e.matmul(
                        PB[u], lhsT=w3b[s][:, kc, fc * 128:(fc + 1) * 128], rhs=h1T[:, kc, tg * 512:(tg + 1) * 512],
                        start=(kc == 0), stop=(kc == 7)),
                        reads=[("w3b", s), ("h1T", tg)], writes=[("PB", u)], inc=(kc == 7))
                S.op("act", lambda e, u=u: e.activation(ssb[u][:], PA[u], AF.Silu),
                     reads=[("PA", u)], writes=[("ssb", u)])
                S.op("dve", lambda e, u=u, hs=hs, fc=fc: e.tensor_tensor(hidT[hs][:, fc, :], PB[u], ssb[u][:], ALU.mult),
                     reads=[("PB", u), ("ssb", u)], writes=[("hidT", hs)])
                if pend is not None:
                    pend(fc)

            def second(part, ex=ex, tg=tg, s=s, hs=hs):
                for tt in range(2 * part, 2 * part + 2):
                    t = tg * 4 + tt
                    o = t % 2
                    for half in range(2):
                        for fc in range(2):
                            S.op("pe", lambda e, half=half, fc=fc, tt=tt, o=o: e.matmul(
                                P[o][:, half * 512:(half + 1) * 512], lhsT=hidT[hs][:, fc, tt * 128:(tt + 1) * 128],
                                rhs=w2b[s][:, fc, half * 512:(half + 1) * 512], start=(fc == 0), stop=(fc == 1)),
                                reads=[("hidT", hs), ("w2b", s)], writes=[("P", o)], inc=(half == 1 and fc == 1))
                    S.op("dve", lambda e, t=t, o=o: e.scalar_tensor_tensor(
                        acc[:, t, :], P[o][:], gates[:, t, ex:ex + 1], acc[:, t, :], ALU.mult, ALU.add),
                        reads=[("P", o), "gates", ("acc", t)], writes=[("acc", t)])
                if part == 1 and tg == NG - 1 and ex + 2 < NE:
                    load_expert(ex + 2)
            pend = second
    if pend is not None:
        pend(0)
        pend(1)
    for i, nm in enumerate(("ln2_g", "ln2_b")):
        S.dma("sp", lnp[:, i, :], dr[nm].partition_broadcast(128), writes=["lnp"])
    for t in range(NT):
        s = t % 2
        layer_norm(acc[:, t, :], ("acc", t), 0, ob[s][:], ("xbt", s), acc[:, t, :], ("acc", t), eps=LN_EPS / (ALPHA * ALPHA))
        S.dma("sp", dr["out"][t * 128:(t + 1) * 128, :], ob[s][:], reads=[("xbt", s)], writes=[("out", t)])
    S.finish([("out", t) for t in range(NT)])


def build_token_nc(T=2048, NE=32, do_route=True):
    nc = bass.Bass("TRN2", target_bir_lowering=False)
    dr = {}

    def inp(name, shape, dt=F32):
        dr[name] = nc.dram_tensor(name, list(shape), dt, kind="ExternalInput").ap()
    inp("ycatT", [1024, T], BF16)
    inp("x_tok", [T, 1024])
    inp("w_out", [1024, 1024])
    for nm in ("ln1_g", "ln1_b", "ln2_g", "ln2_b"):
        inp(nm, [1024])
    inp("w_route", [1024, 36])
    inp("b_route", [36])
    inp("w1", [32, 1024, 256])
    inp("w3", [32, 1024, 256])
    inp("w2", [32, 256, 1024])
    inp("ident", [128, 128])
    dr["out"] = nc.dram_tensor("out", [T, 1024], F32, kind="ExternalOutput").ap()
    with ExitStack() as es:
        S = Sched(nc, es)
        try:
            token_phase(S, nc, es, T, dr, NE, do_route)
        except _Cut:
            pass
        S.replay()
    return nc


NCOL = 928
G_R, G_K, G_V, G_L1, G_GD, G_Q, G_MK, G_MV = 0, 128, 256, 384, 448, 544, 672, 800
MNEG = -30000.0


def mixer_phase(S, nc, es, Tm, dr, do_rwkv=True, do_moba=True, fused=False):
    NGm = Tm // 512
    NKT = Tm // 128

    def sb(name, shape, dt):
        return es.enter_context(nc.sbuf_tensor(name, shape, dt))

    def ps(name, shape, dt=F32):
        return es.enter_context(nc.psum_tensor(name, shape, dt))

    PI = [ps("mPI%d" % i, [128, 512]) for i in range(2)]
    PS = [ps("mPS%d" % i, [128, 512]) for i in range(2)]
    PO = ps("mPO", [128, 512])
    PM = ps("mPM", [128, 512])
    PR = [ps("mPR%d" % i, [128, 512]) for i in range(2)]

    ident = sb("m_ident", [128, 128], F32)
    identb = sb("m_identb", [128, 128], BF16)
    wb = sb("m_wb", [128, 8, NCOL], BF16)
    xs0 = sb("m_xs", [128, 4, 512], F32)
    xs = [xs0, xs0]
    xb0 = sb("m_xb", [128, 8, 512], BF16)
    xb = [xb0, xb0]
    QT = [sb("m_QT%d" % h, [98, 512], BF16) for h in range(2)]
    KT = [sb("m_KT%d" % h, [98, Tm], BF16) for h in range(2)]
    VA = sb("m_VA", [128, NKT, 2, 65], BF16)
    qf = [sb("m_qf%d" % h, [64, 512], F32) for h in range(2)]
    kmean = [sb("m_km%d" % h, [64, 32], F32) for h in range(2)]
    biasT = sb("m_biasT", [128, 2, 68], F32)
    cm = sb("m_cm", [128, 4, 512], BF16)
    est = xs0[:].rearrange("p a b -> p (a b)")[:, 0:2048]
    wst = xs0[:].rearrange("p a b -> p (a b)")[:, 0:NCOL]
    PT = [sb("m_PT%d" % i, [128, 512], BF16) for i in range(2)]
    gsel = sb("m_gsel", [128, 4, 32], F32)
    top8 = sb("m_top8", [128, 4, 8], F32)
    mbp = sb("m_mbp", [128, 4, 96], F32)
    osb = sb("m_osb", [65, 512], F32)
    rcp = sb("m_rcp", [65, 512], F32)
    ones65 = sb("m_ones65", [65, 64], F32)
    ybs = [sb("m_yb%d" % h, [64, 512], BF16) for h in range(2 if fused else 1)]
    if fused:
        Esel = sb("m_Esel", [64, 4, 1024], BF16)
        pstg = [sb("m_pstg%d" % i, [128, 512], BF16) for i in range(2)]

    S.dma("sp", ident[:], dr["ident"], writes=["ident"])
    S.op("dve", lambda e: e.tensor_copy(identb[:], ident[:]), reads=["ident"], writes=["identb"])
    S.dma("sp", biasT[:], dr["biasT"], writes=["biasT"])
    S.op("dve", lambda e: e.memset(ones65[:], 1.0), writes=["ones65"])
    S.op("dve", lambda e: e.memset(mbp[:], 0.0), writes=[("mbp", i) for i in range(4)])
    S.op("pool", lambda e: e.memset(VA[:], 1.0), writes=["VA"])
    for h in range(2):
        S.op("dve", lambda e, h=h: e.memset(kmean[h][:], 0.0), writes=[("kmean", h)])
    wst2 = [xs0[:].rearrange("p a b -> p (a b)")[:, 0:NCOL], xs0[:].rearrange("p a b -> p (a b)")[:, 1024:1024 + NCOL]]
    for kc in range(8):
        S.dma("sp", wst2[kc % 2], dr["w_sel"][kc * 128:(kc + 1) * 128, :], writes=[("wst", kc % 2)])
        S.op("pool" if kc % 2 == 0 else "dve", lambda e, kc=kc: e.tensor_copy(wb[:, kc, :], wst2[kc % 2]),
             reads=[("wst", kc % 2)], writes=[("wb", kc)])

    rw = rwkv_setup(S, nc, es, dr, sb) if do_rwkv else None

    pic = [0]
    NY = [None]

    def inproj_group(g, col0, M, consume):
        s = g % 2
        u = pic[0] % 2
        pic[0] += 1
        for kc in range(8):
            S.op("pe", lambda e, kc=kc, u=u, s=s: e.matmul(PI[u][0:M, :], lhsT=wb[:, kc, col0:col0 + M], rhs=xb[s][:, kc, :],
                                                          start=(kc == 0), stop=(kc == 7)),
                 reads=[("wb", kc), "xb"], writes=[("PI", u)], inc=(kc == 7))
        consume(PI[u], ("PI", u))

    def load_x(gg):
        for half in range(2):
            S.dma("sp", xs0[:], dr["xT"].rearrange("(kc p) t -> p kc t", p=128)[:, 4 * half:4 * half + 4, gg * 512:(gg + 1) * 512],
                  writes=["xs", ("wst", 0), ("wst", 1)])
            S.op("pool", lambda e, half=half: e.tensor_copy(xb0[:, 4 * half:4 * half + 4, :], xs0[:]),
                 reads=["xs"], writes=["xb"])
    load_x(0)
    S.dma("sp", cm[:], dr["cmask"].rearrange("j p t -> p j t"), writes=["cm"])
    for h in range(2):
        S.dma("sp", KT[h][64:98, :], dr["epat"][h], writes=[("KTe", h)])
        S.dma("sp", QT[h][96:98, :], dr["qpos"][:, 0:512], writes=[("QTp", h)])
    if fused:
        S.dma("sp", Esel[:], dr["esel"], writes=["Esel"])
    for g in range(NGm):
        s = g % 2
        tsl = slice(g * 512, (g + 1) * 512)
        if do_moba:
            for h in range(2):
                def cq(P, pk, h=h):
                    S.op("act", lambda e: e.activation(QT[h][0:64, :], P[0:64, :], AF.Copy), reads=[pk], writes=[("QTq", h)])
                    S.op("dve", lambda e: e.tensor_copy(qf[h][:], P[0:64, :]), reads=[pk], writes=[("qf", h)])
                inproj_group(g, G_Q + 64 * h, 64, cq)

                def ck(P, pk, h=h):
                    S.op("act", lambda e: e.activation(KT[h][0:64, tsl], P[0:64, :], AF.Copy), reads=[pk], writes=[("KTk", h)])
                    S.op("dve", lambda e: e.tensor_reduce(kmean[h][:, 2 * g:2 * g + 2], P[0:64, :].rearrange("p (a b) -> p a b", a=2), AX.X, ALU.add),
                         reads=[pk], writes=[("kmean", h)])
                inproj_group(g, G_MK + 64 * h, 64, ck)
            for tt in range(4):
                u = pic[0] % 2
                pic[0] += 1
                kt = g * 4 + tt
                for kc in range(8):
                    S.op("pe", lambda e, kc=kc, u=u, s=s, tt=tt: e.matmul(PI[u][:, 0:128], lhsT=xb[s][:, kc, tt * 128:(tt + 1) * 128],
                                                                         rhs=wb[:, kc, G_MV:G_MV + 128], start=(kc == 0), stop=(kc == 7)),
                         reads=[("wb", kc), "xb"], writes=[("PI", u)], inc=(kc == 7))
                S.op("act", lambda e, u=u, kt=kt: e.activation(VA[:, kt, :, 0:64], PI[u][:, 0:128].rearrange("p (a b) -> p a b", a=2), AF.Copy),
                     reads=[("PI", u)], writes=["VA"])
        adv = lambda: None
        gen = None
        if do_rwkv:
            rwkv_inproj(S, nc, rw, g, inproj_group, dr, ident)
            if NY[0] is None:
                class _Null:
                    op = staticmethod(lambda *a, **k: None)
                    dma = staticmethod(lambda *a, **k: None)
                NY[0] = sum(1 for _ in rwkv_compute(_Null, nc, rw, g, dr, PR, PI[0], ("PI", 0), ident))
            gen = rwkv_compute(S, nc, rw, g, dr, PR, PI[0], ("PI", 0), ident)
            n_it = 2 * (4 * g + 4) + 8
            per = -(-NY[0] // n_it)

            def adv(gen=gen, per=per):
                for _ in range(per):
                    try:
                        next(gen)
                    except StopIteration:
                        return
        pf = []
        if g + 1 < NGm:
            def x_dma(half, gg=g + 1):
                S.dma("sp", xs0[:], dr["xT"].rearrange("(kc p) t -> p kc t", p=128)[:, 4 * half:4 * half + 4, gg * 512:(gg + 1) * 512],
                      writes=["xs"])

            def x_cast(half):
                S.op("act", lambda e: e.activation(xb0[:, 4 * half:4 * half + 4, :], xs0[:], AF.Copy), reads=["xs"], writes=["xb"])
            x_dma(0)
            pf = [lambda: (x_cast(0), x_dma(1)), lambda: x_cast(1)]
        if not do_moba:
            for _ in gen:
                pass
            while pf:
                pf.pop(0)()
            continue
        for h in range(2):
            for cq4 in range(4):
                blk = (g * 4 + cq4) // 2
                if blk > 0:
                    S.op("pe", lambda e, h=h, cq4=cq4, blk=blk: e.matmul(PM[:, cq4 * 32:cq4 * 32 + blk], lhsT=qf[h][:, cq4 * 128:(cq4 + 1) * 128],
                                                                       rhs=kmean[h][:, 0:blk], start=True, stop=True),
                         reads=[("qf", h), ("kmean", h)], writes=[("PM", 0)])
            adv()
            for cq4 in range(4):
                blk = (g * 4 + cq4) // 2
                mk, gk, tk = ("mbp", cq4), ("gsel", cq4), ("top8", cq4)
                mb_, gs_, t8_ = mbp[:, cq4, :], gsel[:, cq4, :], top8[:, cq4, :]
                S.op("dve", lambda e, mb_=mb_: e.memset(mb_[:, 64:96], MNEG), writes=[mk])
                if blk > 3:
                    S.op("dve", lambda e, gs_=gs_: e.memset(gs_, -1e30), writes=[gk])
                    S.op("dve", lambda e, gs_=gs_, cq4=cq4, blk=blk: e.tensor_copy(gs_[:, 0:blk], PM[:, cq4 * 32:cq4 * 32 + blk]),
                         reads=[("PM", 0)], writes=[gk])
                    S.op("dve", lambda e, gs_=gs_, t8_=t8_: e.max(t8_, gs_), reads=[gk], writes=[tk])
                    S.op("dve", lambda e, mb_=mb_, gs_=gs_, t8_=t8_, blk=blk: e.tensor_scalar(mb_[:, 64:64 + blk], gs_[:, 0:blk], t8_[:, 2:3], -MNEG,
                                                                                   ALU.is_ge, ALU.mult),
                         reads=[gk, tk], writes=[mk])
                    S.op("dve", lambda e, mb_=mb_, blk=blk: e.tensor_scalar(mb_[:, 64:64 + blk], mb_[:, 64:64 + blk], MNEG, None, ALU.add),
                         reads=[mk], writes=[mk])
                elif blk > 0:
                    S.op("dve", lambda e, mb_=mb_, blk=blk: e.memset(mb_[:, 64:64 + blk], 0.0), writes=[mk])
                S.op("dve", lambda e, mb_=mb_, blk=blk: e.memset(mb_[:, 64 + blk:65 + blk], 0.0), writes=[mk])
                adv()
            for cq4 in range(4):
                S.op("pe", lambda e, cq4=cq4: e.matmul(PO[0:96, cq4 * 128:(cq4 + 1) * 128],
                                                       lhsT=mbp[:, cq4, :], rhs=ident[:], start=True, stop=True),
                     reads=[("mbp", cq4), "ident"], writes=[("PO", 0)])
            S.op("act", lambda e, h=h: e.activation(QT[h][64:96, :], PO[64:96, :], AF.Copy), reads=[("PO", 0)], writes=[("QTm", h)])
            if pf:
                pf.pop(0)()
            nkt = 4 * g + 4

            def emit_st(kt, h=h):
                u = kt % 2
                dl = kt - 4 * g
                S.op("pe", lambda e: e.matmul(PS[u][:], lhsT=KT[h][0:98, kt * 128:(kt + 1) * 128], rhs=QT[h][0:98, :],
                                              start=True, stop=(dl < 0)),
                     reads=[("KTk", h), ("KTe", h), ("QTq", h), ("QTm", h), ("QTp", h)], writes=[("PS", u)], inc=(dl < 0))
                if dl >= 0:
                    S.op("pe", lambda e: e.matmul(PS[u][:], lhsT=identb[:], rhs=cm[:, dl, :], start=False, stop=True),
                         reads=["identb", "cm"], writes=[("PS", u)])
            emit_st(0)
            for kt in range(nkt):
                u = kt % 2
                dl = kt - 4 * g
                if kt + 1 < nkt:
                    emit_st(kt + 1)
                S.op("act", lambda e, h=h, u=u, dl=dl: e.activation(PT[u][:], PS[u][:], AF.Exp, bias=biasT[:, h, dl + 64:dl + 65], scale=0.125),
                     reads=[("PS", u), "biasT"], writes=[("PT", u)])
                adv()
                S.op("pe", lambda e, h=h, kt=kt, u=u, nkt=nkt: e.matmul(PO[0:65, :], lhsT=VA[:, kt, h, :], rhs=PT[u][:],
                                                                       start=(kt == 0), stop=(kt == nkt - 1)),
                     reads=["VA", ("PT", u)], writes=[("PO", 0)])
            S.op("act", lambda e: e.activation(rcp[64:65, :], PO[64:65, :], AF.Ln), reads=[("PO", 0)], writes=["rcp"])
            S.op("act", lambda e: e.activation(rcp[64:65, :], rcp[64:65, :], AF.Exp, scale=-1.0), reads=["rcp"], writes=["rcp"])
            S.op("act", lambda e: e.activation(osb[0:64, :], PO[0:64, :], AF.Copy), reads=[("PO", 0)], writes=["osb"])
            adv()
            adv()
            S.op("pe", lambda e: e.matmul(PM[0:64, :], lhsT=ones65[64:65, :], rhs=rcp[64:65, :], start=True, stop=True),
                 reads=["ones65", "rcp"], writes=[("PM", 0)])
            yb = ybs[h if fused else 0]
            ybk = ("yb", h if fused else 0)
            S.op("dve", lambda e: e.tensor_tensor(yb[:], osb[0:64, :], PM[0:64, :], ALU.mult), reads=["osb", ("PM", 0)], writes=[ybk])
            if not fused:
                S.dma("sp", dr["ycT"][128 + 64 * h:192 + 64 * h, tsl], yb[:], reads=[ybk], writes=[("ycT_out", g, h)])
        if gen is not None:
            for _ in gen:
                pass
        while pf:
            pf.pop(0)()
        if fused:
            seg, qo = g // 4, (g % 4) * 512
            srcs = [(rw["ya", 0], ("r_ya", 0)), (rw["ya", 1], ("r_ya", 1)), (ybs[0], ("yb", 0)), (ybs[1], ("yb", 1))]
            for kc in range(8):
                u = pic[0] % 2
                pic[0] += 1
                for si, (yt_, yk_) in enumerate(srcs):
                    S.op("pe", lambda e, si=si, yt_=yt_, u=u, kc=kc: e.matmul(PI[u][:, :], lhsT=Esel[:, si, kc * 128:(kc + 1) * 128], rhs=yt_[:],
                                                                             start=(si == 0), stop=(si == 3)),
                         reads=["Esel", yk_], writes=[("PI", u)], inc=(si == 3))
                st = pstg[kc % 2]
                S.op("act" if kc % 2 == 0 else "dve",
                     (lambda e, st=st, u=u: e.activation(st[:], PI[u][:, :], AF.Copy)) if kc % 2 == 0 else
                     (lambda e, st=st, u=u: e.tensor_copy(st[:], PI[u][:, :])),
                     reads=[("PI", u)], writes=[("pstg", kc % 2)])
                S.dma("sp", dr["rs_in"][g % 4][seg * 1024 + kc * 128:seg * 1024 + (kc + 1) * 128, :], st[:],
                      reads=[("pstg", kc % 2)], writes=[("rsin", g, kc)])
            if "on_group_done" in dr:
                dr["on_group_done"](g)
    outs = []
    for g in range(NGm):
        if fused:
            outs += [("rsin", g, kc) for kc in range(8)]
            continue
        for h in range(2):
            if do_moba:
                outs.append(("ycT_out", g, h))
            if do_rwkv:
                outs.append(("ya_out", g, h))
    if not fused:
        S.finish(outs)
    return outs


def build_mixer_nc(Tm=8192, do_rwkv=True, do_moba=True):
    nc = bass.Bass("TRN2", target_bir_lowering=False)
    dr = {}

    def inp(name, shape, dt=F32):
        dr[name] = nc.dram_tensor(name, list(shape), dt, kind="ExternalInput").ap()
    inp("xT", [1024, Tm])
    inp("w_sel", [1024, NCOL])
    inp("ident", [128, 128])
    inp("biasT", [128, 2, 68])
    inp("cmask", [4, 128, 512], BF16)
    inp("epat", [2, 34, Tm], BF16)
    inp("qpos", [2, Tm], BF16)
    rwkv_inputs(inp)
    dr["ycT"] = nc.dram_tensor("ycT", [256, Tm], BF16, kind="ExternalOutput").ap()
    with ExitStack() as es:
        S = Sched(nc, es)
        mixer_phase(S, nc, es, Tm, dr, do_rwkv, do_moba)
        S.replay()
    return nc


def rwkv_inputs(inp):
    pass


def mixer_consts(hg, Tm):
    heads = [2 * hg, 2 * hg + 1]
    slopes = [2.0 ** (-(h + 1)) for h in heads]
    p = np.arange(128, dtype=np.float32)[:, None]
    dl = (np.arange(68, dtype=np.float32) - 64)[None, :]
    biasT = np.stack([sl * (dl * 128 + p) for sl in slopes], 1).astype(np.float32)
    k = np.arange(128)[:, None]
    q = np.arange(512)[None, :]
    cmask = np.stack([np.where(j * 128 + k <= q, 0.0, MNEG) for j in range(4)], 0).astype(np.float32)
    epat = np.zeros((2, 34, Tm), np.float32)
    for n in range(min(32, Tm // 256)):
        epat[:, n, n * 256:(n + 1) * 256] = 1.0
    for i, sl in enumerate(slopes):
        epat[i, 32, :] = -8.0 * sl * 64
        epat[i, 33, :] = -8.0 * sl
    t = np.arange(Tm) % 512
    qpos = np.stack([t // 64, t % 64], 0).astype(np.float32)
    bf = ml_dtypes.bfloat16
    return dict(biasT=biasT, cmask=cmask.astype(bf), epat=epat.astype(bf), qpos=qpos.astype(bf), ident=np.eye(128, dtype=np.float32))


def mixer_inputs(d, c, Tm=8192):
    b, hg = c // 4, c % 4
    w_in = d["w_in"]
    cols = []
    for base in (0, 512, 1024):
        cols.append(np.arange(base + hg * 128, base + hg * 128 + 128))
    cols.append(np.arange(1536, 1696))
    for base in (1696, 1696 + 512, 1696 + 1024):
        cols.append(np.arange(base + hg * 128, base + hg * 128 + 128))
    cols = np.concatenate(cols)
    m = dict(mixer_consts(hg, Tm))
    m["xT"] = np.ascontiguousarray(d["x"][b, :Tm, :].T)
    m["w_sel"] = np.ascontiguousarray(w_in[:, cols])
    return m, cols


LAM = 0.6065306597126334
GN_EPS = 64e-5


def rwkv_inputs(inp):
    inp("mu_cols", [128, 8])
    inp("pcols", [64, 2, 5])
    inp("wlu", [32, 128])
    inp("alu", [32, 128])
    inp("glu", [96, 128])
    inp("gnw", [128])
    inp("gnb", [128])
    inp("rmask", [64, 384])


def rwkv_host_inputs(d, c):
    b, hg = c // 4, c % 4
    sl = slice(hg * 128, hg * 128 + 128)
    mu = d["mu_shift"]
    mu_cols = np.zeros((128, 8), np.float32)
    for i, base in enumerate((0, 512, 1024)):
        for h in range(2):
            mu_cols[0:64, 2 * i + h] = mu[base + hg * 128 + h * 64: base + hg * 128 + h * 64 + 64]
    mu_cols[0:64, 6] = mu[1536:1600]
    mu_cols[0:96, 7] = mu[1600:1696]
    pcols = np.zeros((64, 2, 5), np.float32)
    for h in range(2):
        s2 = slice(hg * 128 + h * 64, hg * 128 + h * 64 + 64)
        pcols[:, h, 0] = d["w0"][s2]
        pcols[:, h, 1] = d["a0"][s2]
        pcols[:, h, 2] = d["k_k"][s2]
        pcols[:, h, 3] = d["k_a"][s2]
        pcols[:, h, 4] = d["r_k"].reshape(-1)[s2]
    s_ = np.arange(64)[:, None]
    t_ = np.arange(64)[None, :]
    Ms = (s_ < t_).astype(np.float32)
    Mi = (s_ <= t_).astype(np.float32)
    rmask = np.concatenate([Ms, Mi, Ms, Mi, Ms.T, Ms.T], 1).astype(np.float32)[:, :384]
    return dict(mu_cols=mu_cols, pcols=pcols, wlu=np.ascontiguousarray(d["w_lora_up"][:, sl]),
                alu=np.ascontiguousarray(d["a_lora_up"][:, sl]), glu=np.ascontiguousarray(d["g_lora_up"][:, sl]),
                gnw=np.ascontiguousarray(d["gn_w"][sl]), gnb=np.ascontiguousarray(d["gn_b"][sl]), rmask=rmask)


def rwkv_setup(S, nc, es, dr, sb):
    rw = {}
    for nm, shp in (("mu", [128, 8]), ("pc", [64, 2, 5]), ("omk", [64, 2]), ("wlu", [32, 128]), ("alu", [64, 128]), ("glu", [96, 128]),
                    ("gnw", [64, 128]), ("gnb", [64, 128]), ("rmask", [64, 384]), ("ones", [64, 64]),
                    ("l1m", [64, 512]), ("gdm", [96, 512]), ("tmp", [96, 512])):
        rw[nm] = sb("r_" + nm, shp, F32)
    rw["tw"] = rw["l1m"]
    rw["gs"] = rw["gdm"]
    rw["praw"] = sb("r_praw", [96, 513], F32)
    rw["last"] = sb("r_last", [96, 8], F32)
    for nm in ("sg", "asig", "kk", "kkn", "kmod", "E1", "E3", "rkr"):
        rw[nm, 0] = rw[nm, 1] = sb("r_%s" % nm, [64, 512], F32)
    rw["cum", 0] = rw["cum", 1] = rw["kk", 0]
    rw["E2", 0] = rw["E2", 1] = rw["sg", 0]
    for nm in ("AR", "BK", "BKh"):
        rw[nm, 0] = rw[nm, 1] = sb("r_%s" % nm, [64, 8, 128], F32)
    for h in range(2):
        for nm in ("rm", "km", "vm"):
            rw[nm, h] = sb("r_%s%d" % (nm, h), [64, 512], F32)
        rw["S", h] = sb("r_S%d" % h, [64, 64], F32)
        rw["ya", h] = sb("r_ya%d" % h, [64, 512], BF16)
    for nm, shp in (("AAm", [64, 4, 256]), ("Nt", [64, 4, 64]), ("NPg", [64, 2, 4, 128]), ("TK", [64, 4, 256]), ("TG", [64, 4, 65]),
                    ("Z", [64, 2, 4, 128]), ("Tt", [64, 2, 4, 64]), ("Afm", [64, 4, 64]), ("M", [64, 4, 64]), ("Sl", [64, 4, 64]), ("Rf", [64, 4, 64]),
                    ("y", [64, 4, 64]), ("yt", [64, 4, 64]), ("ysq", [64, 4, 64]), ("sst", [64, 6, 4])):
        rw[nm] = sb("r_" + nm, shp, F32)
    S.dma("sp", rw["mu"][:], dr["mu_cols"], writes=["r_mu"])
    S.dma("sp", rw["pc"][:], dr["pcols"], writes=["r_pc"])
    S.dma("sp", rw["wlu"][:], dr["wlu"], writes=["r_wlu"])
    S.dma("sp", rw["alu"][32:64, :], dr["alu"], writes=["r_alu"])
    S.dma("sp", rw["glu"][:], dr["glu"], writes=["r_glu"])
    S.dma("sp", rw["gnw"][:], dr["gnw"].partition_broadcast(64), writes=["r_gn"])
    S.dma("sp", rw["gnb"][:], dr["gnb"].partition_broadcast(64), writes=["r_gn"])
    S.dma("sp", rw["rmask"][:], dr["rmask"], writes=["r_rmask"])
    S.op("dve", lambda e: e.memset(rw["ones"][:], 1.0), writes=["r_ones"])
    S.op("dve", lambda e: e.tensor_scalar(rw["omk"][:], rw["pc"][:, :, 3], -1.0, 1.0, ALU.mult, ALU.add), reads=["r_pc"], writes=["r_omk"])
    S.op("pool", lambda e: e.memset(rw["last"][:], 0.0), writes=["r_last"])
    for h in range(2):
        S.op("pool", lambda e, h=h: e.memset(rw["S", h][:], 0.0), writes=[("r_S", h)])
    return rw


def rwkv_inproj(S, nc, rw, g, inproj_group, dr, ident):
    tsl = slice(g * 512, (g + 1) * 512)
    I64 = ident[0:64, 0:64]
    mu = rw["mu"]
    tmp = rw["tmp"]

    def shift_mix(gi, M, dst, dkey):
        praw = rw["praw"]
        last = rw["last"]

        def consume(P, pk):
            S.op("act", lambda e: e.activation(praw[0:M, 1:513], P[0:M, :], AF.Copy), reads=[pk], writes=["r_praw"])
            S.op("act", lambda e: e.activation(praw[0:M, 0:1], last[0:M, gi:gi + 1], AF.Copy), reads=["r_last"], writes=["r_praw"])
            S.op("dve", lambda e: e.tensor_tensor(tmp[0:M, :], praw[0:M, 0:512], praw[0:M, 1:513], ALU.subtract),
                 reads=["r_praw"], writes=["r_tmp"])
            S.op("dve", lambda e: e.scalar_tensor_tensor(dst[0:M, :], tmp[0:M, :], mu[0:M, gi:gi + 1], praw[0:M, 1:513], ALU.mult, ALU.add),
                 reads=["r_tmp", "r_mu", "r_praw"], writes=[dkey])
            S.op("act", lambda e: e.activation(last[0:M, gi:gi + 1], praw[0:M, 512:513], AF.Copy), reads=["r_praw"], writes=["r_last"])
        return consume
    for h in range(2):
        inproj_group(g, G_R + 64 * h, 64, shift_mix(0 + h, 64, rw["rm", h], ("r_rm", h)))
        inproj_group(g, G_K + 64 * h, 64, shift_mix(2 + h, 64, rw["km", h], ("r_km", h)))
        inproj_group(g, G_V + 64 * h, 64, shift_mix(4 + h, 64, rw["vm", h], ("r_vm", h)))
    inproj_group(g, G_L1, 64, shift_mix(6, 64, rw["l1m"], "r_l1m"))
    inproj_group(g, G_GD, 96, shift_mix(7, 96, rw["gdm"], "r_gdm"))


def rwkv_compute(S, nc, rw, g, dr, PR, PM, kPM, ident):
    tsl = slice(g * 512, (g + 1) * 512)
    I64 = ident[0:64, 0:64]
    tmp = rw["tmp"]
    S.op("act", lambda e: e.activation(rw["l1m"][0:32, :], rw["l1m"][0:32, :], AF.Tanh), reads=["r_l1m"], writes=["r_l1m"])
    yield
    S.op("act", lambda e: e.activation(rw["gdm"][:], rw["gdm"][:], AF.Sigmoid), reads=["r_gdm"], writes=["r_gdm"])
    yield
    pc = rw["pc"]
    for h in range(2):
        hs = slice(h * 64, h * 64 + 64)
        rm, km, vm, sg, asig, kk, kkn, kmod, cum, E1, E2, E3, rkr = [rw[n, h] for n in
                                                                      ("rm", "km", "vm", "sg", "asig", "kk", "kkn", "kmod", "cum", "E1", "E2", "E3", "rkr")]
        AR, BK, BKh, Sst, ya = rw["AR", h], rw["BK", h], rw["BKh", h], rw["S", h], rw["ya", h]
        P0, P1 = PR[0], PR[1]
        k0, k1 = ("PR", 0), ("PR", 1)
        S.op("pe", lambda e: e.matmul(P0[0:64, :], lhsT=rw["wlu"][:, hs], rhs=rw["l1m"][0:32, :], start=True, stop=True),
             reads=["r_wlu", "r_l1m"], writes=[k0])
        S.op("act", lambda e: e.activation(sg[:], P0[0:64, :], AF.Sigmoid, bias=pc[:, h, 0:1]), reads=[k0, "r_pc"], writes=[("r_sg", 0)])
        yield
        S.op("pe", lambda e: e.matmul(P1[0:64, :], lhsT=rw["alu"][32:64, hs], rhs=rw["l1m"][32:64, :], start=True, stop=True),
             reads=["r_alu", "r_l1m"], writes=[k1])
        S.op("act", lambda e: e.activation(asig[:], P1[0:64, :], AF.Sigmoid, bias=pc[:, h, 1:2]), reads=[k1, "r_pc"], writes=[("r_asig", 0)])
        yield
        S.op("dve", lambda e: e.tensor_scalar(kk[:], km[:], pc[:, h, 2:3], None, ALU.mult), reads=[("r_km", h), "r_pc"], writes=[("r_kk", 0)])
        yield
        S.op("dve", lambda e: e.tensor_tensor(tmp[0:64, :], kk[:], kk[:], ALU.mult), reads=[("r_kk", 0)], writes=["r_tmp"])
        yield
        S.op("pe", lambda e: e.matmul(P0[0:64, :], lhsT=rw["ones"][:], rhs=tmp[0:64, :], start=True, stop=True),
             reads=["r_ones", "r_tmp"], writes=[k0])
        S.op("dve", lambda e: e.tensor_scalar(tmp[0:64, :], P0[0:64, :], 1e-18, None, ALU.max), reads=[k0], writes=["r_tmp"])
        yield
        S.op("act", lambda e: e.activation(tmp[0:64, :], tmp[0:64, :], AF.Ln), reads=["r_tmp"], writes=["r_tmp"])
        yield
        S.op("act", lambda e: e.activation(tmp[0:64, :], tmp[0:64, :], AF.Exp, scale=-0.5), reads=["r_tmp"], writes=["r_tmp"])
        yield
        S.op("dve", lambda e: e.tensor_tensor(kkn[:], kk[:], tmp[0:64, :], ALU.mult), reads=[("r_kk", 0), "r_tmp"], writes=[("r_kkn", 0)])
        yield
        S.op("dve", lambda e: e.tensor_scalar(tmp[0:64, :], asig[:], pc[:, h, 3:4], rw["omk"][:, h:h + 1], ALU.mult, ALU.add),
             reads=[("r_asig", 0), "r_pc", "r_omk"], writes=["r_tmp"])
        yield
        S.op("dve", lambda e: e.tensor_tensor(kmod[:], km[:], tmp[0:64, :], ALU.mult), reads=[("r_km", h), "r_tmp"], writes=[("r_kmod", 0)])
        yield
        S.op("dve", lambda e: e.scalar_tensor_tensor(rkr[:], rm[:], pc[:, h, 4:5], kmod[:], ALU.mult, ALU.mult),
             reads=[("r_rm", h), ("r_kmod", 0), "r_pc"], writes=[("r_rkr", 0)])
        yield
        for c in range(8):
            cs = slice(c * 64, c * 64 + 64)
            S.op("dve", lambda e, cs=cs: e.tensor_tensor_scan(cum[:, cs], rw["ones"][:], sg[:, cs], 0.0, ALU.mult, ALU.add),
                 reads=[("r_sg", 0), "r_ones"], writes=[("r_kk", 0)])
            yield
        S.op("act", lambda e: e.activation(E1[:], cum[:], AF.Exp, scale=-LAM), reads=[("r_kk", 0)], writes=[("r_E1", 0)])
        yield
        S.op("dve", lambda e: e.tensor_tensor(tmp[0:64, :], cum[:], sg[:], ALU.subtract), reads=[("r_kk", 0), ("r_sg", 0)], writes=["r_tmp"])
        yield
        S.op("act", lambda e: e.activation(E3[:], tmp[0:64, :], AF.Exp, scale=-LAM), reads=["r_tmp"], writes=[("r_E3", 0)])
        yield
        S.op("act", lambda e: e.activation(E2[:], cum[:], AF.Exp, scale=LAM), reads=[("r_kk", 0)], writes=[("r_sg", 0)])
        yield
        v3 = lambda ap: ap.rearrange("p (c t) -> p c t", c=8)
        S.op("dve", lambda e: e.scalar_tensor_tensor(AR[:, :, 0:64], v3(kkn[:]), -1.0, v3(E3[:]), ALU.mult, ALU.mult),
             reads=[("r_kkn", 0), ("r_E3", 0)], writes=[("r_AR", 0)])
        yield
        S.op("dve", lambda e: e.tensor_tensor(AR[:, :, 64:128], v3(rm[:]), v3(E1[:]), ALU.mult),
             reads=[("r_rm", h), ("r_E1", 0)], writes=[("r_AR", 0)])
        yield
        S.op("dve", lambda e: e.tensor_tensor(tmp[0:64, :], kkn[:], asig[:], ALU.mult), reads=[("r_kkn", 0), ("r_asig", 0)], writes=["r_tmp"])
        yield
        S.op("dve", lambda e: e.tensor_tensor(BK[:, :, 0:64], v3(tmp[0:64, :]), v3(E2[:]), ALU.mult), reads=["r_tmp", ("r_sg", 0)], writes=[("r_BK", 0)])
        yield
        S.op("dve", lambda e: e.tensor_tensor(BK[:, :, 64:128], v3(kmod[:]), v3(E2[:]), ALU.mult), reads=[("r_kmod", 0), ("r_sg", 0)], writes=[("r_BK", 0)])
        yield
        S.op("dve", lambda e: e.tensor_tensor(BKh[:], BK[:], v3(E1[:])[:, :, 63:64].to_broadcast([64, 8, 128]), ALU.mult),
             reads=[("r_BK", 0), ("r_E1", 0)], writes=[("r_BKh", 0)])
        yield
        Tt = rw["Tt"]
        AAm, Nt, NPg, TK, TG, Z, Afm, Mm, Sl, Rf, y, yt, ysq, sst = [rw[n] for n in
                                                                       ("AAm", "Nt", "NPg", "TK", "TG", "Z", "Afm", "M", "Sl", "Rf", "y", "yt", "ysq", "sst")]
        rmask = rw["rmask"]
        B0, B1, B2 = PR[0], PR[1], PM
        kB0, kB1, kB2 = ("PR", 0), ("PR", 1), kPM
        NB = 4
        E1v = v3(E1[:])
        rd = [("r_AR", 0), ("r_BK", 0)]

        def mm(out, lhsT, rhs, reads, wk, inc, start=True, stop=True):
            S.op("pe", lambda e: e.matmul(out, lhsT=lhsT, rhs=rhs, start=start, stop=stop), reads=reads, writes=[wk], inc=inc)
        for hb in range(2):
            c0 = hb * NB
            banks = [(B0, kB0), (B0, kB0), (B1, kB1), (B1, kB1)]
            for j in range(NB):
                c = c0 + j
                Bj, kBj = banks[j]
                off = (j % 2) * 256
                mm(Bj[0:64, off:off + 128], BK[:, c, 0:64], AR[:, c, :], rd, kBj, False)
                mm(Bj[0:64, off + 128:off + 256], BK[:, c, 64:128], AR[:, c, :], rd, kBj, j % 2 == 1)
            for b2, (Bj, kBj) in enumerate(((B0, kB0), (B1, kB1))):
                S.op("dve", lambda e, b2=b2, Bj=Bj: e.tensor_tensor(AAm[:, 2 * b2:2 * b2 + 2, :], Bj[0:64, 0:512].rearrange("p (a b) -> p a b", a=2),
                                                                  rmask[:, 0:256].unsqueeze(1).to_broadcast([64, 2, 256]), ALU.mult),
                     reads=[kBj, "r_rmask"], writes=["r_AAm"])
                yield
            for j in range(NB):
                c = c0 + j
                mm(B2[0:64, j * 64:(j + 1) * 64], AR[:, c, 0:64], BK[:, c, 0:64], rd, kB2, j == NB - 1)
            S.op("dve", lambda e: e.tensor_tensor(Nt[:], B2[0:64, 0:256].rearrange("p (a b) -> p a b", a=NB),
                                                  rmask[:, 256:320].unsqueeze(1).to_broadcast([64, NB, 64]), ALU.mult),
                 reads=[kB2, "r_rmask"], writes=["r_Nt"])
            yield
            for j in range(NB):
                c = c0 + j
                cs = slice(c * 64, c * 64 + 64)
                Bj, kBj = banks[j]
                off = (j % 2) * 256
                mm(Bj[0:64, off:off + 64], vm[:, cs], I64, [("r_vm", h), "ident"], kBj, False)
                mm(Bj[0:64, off + 64:off + 128], AR[:, c, 0:64], I64, rd + ["ident"], kBj, False)
                mm(Bj[0:64, off + 128:off + 192], BKh[:, c, 0:64], I64, [("r_BKh", 0), "ident"], kBj, False)
                mm(Bj[0:64, off + 192:off + 256], BKh[:, c, 64:128], I64, [("r_BKh", 0), "ident"], kBj, j % 2 == 1)
            for b2, (Bj, kBj) in enumerate(((B0, kB0), (B1, kB1))):
                S.op("act", lambda e, b2=b2, Bj=Bj: e.activation(TK[:, 2 * b2:2 * b2 + 2, :], Bj[0:64, 0:512].rearrange("p (a b) -> p a b", a=2), AF.Copy),
                     reads=[kBj], writes=["r_TK"])
                yield
            for j in range(NB):
                c = c0 + j
                cs = slice(c * 64, c * 64 + 64)
                mm(B2[0:64, j * 65:j * 65 + 64], rw["gdm"][:, cs], rw["glu"][:, hs], ["r_gdm", "r_glu"], kB2, False)
                mm(B2[0:64, j * 65 + 64:j * 65 + 65], rkr[:, cs], rw["ones"][:, 0:1], [("r_rkr", 0), "r_ones"], kB2, j == NB - 1)
            S.op("act", lambda e: e.activation(TG[:], B2[0:64, 0:NB * 65].rearrange("p (a b) -> p a b", a=NB), AF.Copy), reads=[kB2], writes=["r_TG"])
            yield
            for j in range(NB):
                mm(B2[0:64, j * 64:(j + 1) * 64], AAm[:, j, 128:192], TK[:, j, 0:64], ["r_AAm", "r_TK"], kB2, j == NB - 1)
            S.op("dve", lambda e: e.tensor_copy(Z[:, 0, :, 64:128], B2[0:64, 0:256].rearrange("p (a b) -> p a b", a=NB)), reads=[kB2], writes=[("r_Z", 0)])
            yield
            S.op("dve", lambda e: e.tensor_copy(Z[:, 0, :, 0:64], TK[:, :, 64:128]), reads=["r_TK"], writes=[("r_Z", 0)])
            yield
            S.op("dve", lambda e: e.tensor_tensor(Tt[:, 1], I64.unsqueeze(1).to_broadcast([64, NB, 64]), AAm[:, :, 0:64], ALU.add),
                 reads=["ident", "r_AAm"], writes=[("r_T", 1)])
            yield
            for k in range(6):
                Nk = (lambda j: AAm[:, j, 0:64]) if k == 0 else (lambda j, k=k: NPg[:, k % 2, j, 0:64])
                Ntk = (lambda j: Nt[:, j, :]) if k == 0 else (lambda j, k=k: NPg[:, k % 2, j, 64:128])
                rdk = ["r_AAm", "r_Nt"] if k == 0 else [("r_NP", k % 2)]
                if k < 5:
                    for j in range(NB):
                        if k < 4:
                            mm(B1[0:64, j * 128:j * 128 + 64], Ntk(j), Nk(j), rdk, kB1, False)
                        mm(B1[0:64, j * 128 + 64:(j + 1) * 128], Nk(j), Ntk(j), rdk, kB1, j == NB - 1)
                    S.op("act", lambda e, k=k: e.activation(NPg[:, (k + 1) % 2], B1[0:64, 0:512].rearrange("p (a b) -> p a b", a=NB), AF.Copy),
                         reads=[kB1], writes=[("r_NP", (k + 1) % 2)])
                    yield
                if k >= 1:
                    ti, to = k % 2, (k + 1) % 2
                    for j in range(NB):
                        mm(B0[0:64, j * 64:(j + 1) * 64], Ntk(j), Tt[:, ti, j, :], rdk + [("r_T", ti)], kB0, j == NB - 1)
                    S.op("dve", lambda e, ti=ti, to=to: e.tensor_tensor(Tt[:, to], B0[0:64, 0:NB * 64].rearrange("p (a b) -> p a b", a=NB), Tt[:, ti], ALU.add),
                         reads=[kB0, ("r_T", ti)], writes=[("r_T", to)])
                    yield
            for j in range(NB):
                mm(B0[0:64, j * 128:(j + 1) * 128], Tt[:, 0, j, :], Z[:, 0, j, :], [("r_T", 0), ("r_Z", 0)], kB0, j == NB - 1)
            S.op("act", lambda e: e.activation(Z[:, 1], B0[0:64, 0:512].rearrange("p (a b) -> p a b", a=NB), AF.Copy), reads=[kB0], writes=[("r_Z", 1)])
            yield
            Zf = Z[:, 1]
            zk = [("r_Z", 1)]
            for j in range(NB):
                Ah, ul = Zf[:, j, 0:64], Zf[:, j, 64:128]
                vt, bh, kh = TK[:, j, 0:64], TK[:, j, 128:192], TK[:, j, 192:256]
                mm(B0[0:64, 256 + j * 64:256 + (j + 1) * 64], Ah, bh, zk + ["r_TK"], kB0, False)
                mm(B1[0:64, j * 64:(j + 1) * 64], bh, ul, zk + ["r_TK"], kB1, False, start=True, stop=False)
                mm(B1[0:64, j * 64:(j + 1) * 64], kh, vt, ["r_TK"], kB1, False, start=False, stop=True)
                mm(B1[0:64, 256 + j * 64:256 + (j + 1) * 64], Ah, AAm[:, j, 64:128], zk + ["r_AAm"], kB1, j == NB - 1)
            v4 = lambda ap: ap.rearrange("p (a b) -> p a b", a=NB)
            S.op("dve", lambda e: e.tensor_tensor(Mm[:], I64.unsqueeze(1).to_broadcast([64, NB, 64]),
                                                  E1v[:, c0:c0 + NB, 63:64].to_broadcast([64, NB, 64]), ALU.mult),
                 reads=["ident", ("r_E1", 0)], writes=["r_M"])
            yield
            S.op("dve", lambda e: e.tensor_tensor(Mm[:], Mm[:], v4(B0[0:64, 256:512]), ALU.add), reads=[kB0, "r_M"], writes=["r_M"])
            yield
            S.op("act", lambda e: e.activation(Sl[:], v4(B1[0:64, 0:256]), AF.Copy), reads=[kB1], writes=["r_Sl"])
            yield
            S.op("dve", lambda e: e.tensor_tensor(Rf[:], v4(B1[0:64, 256:512]), AR[:, c0:c0 + NB, 64:128], ALU.add), reads=[kB1] + rd, writes=["r_Rf"])
            yield
            for j in range(NB):
                yo = B0[0:64, j * 64:(j + 1) * 64]
                mm(yo, Rf[:, j, :], Sst[:], ["r_Rf", ("r_S", h)], kB0, False, start=True, stop=False)
                mm(yo, AAm[:, j, 64:128], Zf[:, j, 64:128], zk + ["r_AAm"], kB0, False, start=False, stop=False)
                mm(yo, AAm[:, j, 192:256], TK[:, j, 0:64], ["r_AAm", "r_TK"], kB0, False, start=False, stop=True)
                mm(B2[0:64, 0:64], Mm[:, j, :], Sst[:], ["r_M", ("r_S", h)], kB2, True)
                S.op("dve", lambda e, j=j: e.tensor_tensor(Sst[:], B2[0:64, 0:64], Sl[:, j, :], ALU.add), reads=[kB2, "r_Sl"], writes=[("r_S", h)])
                yield
            S.op("act", lambda e: e.activation(y[:], v4(B0[0:64, 0:256]), AF.Copy), reads=[kB0], writes=["r_y"])
            yield
            b3 = lambda ap: ap.unsqueeze(2).to_broadcast([64, NB, 64])
            dv = lambda fn, r_, w_: S.op("dve", fn, reads=r_, writes=w_)
            dv(lambda e: e.tensor_reduce(sst[:, 0, :], y[:], AX.X, ALU.add), ["r_y"], ["r_sst"])
            yield
            dv(lambda e: e.tensor_tensor(ysq[:], y[:], y[:], ALU.mult), ["r_y"], ["r_ysq"])
            yield
            dv(lambda e: e.tensor_reduce(sst[:, 1, :], ysq[:], AX.X, ALU.add), ["r_ysq"], ["r_sst"])
            yield
            dv(lambda e: e.tensor_scalar(sst[:, 2, :], sst[:, 0, :], 1.0 / 64, None, ALU.mult), ["r_sst"], ["r_sst"])
            yield
            dv(lambda e: e.tensor_tensor(sst[:, 3, :], sst[:, 2, :], sst[:, 2, :], ALU.mult), ["r_sst"], ["r_sst"])
            yield
            dv(lambda e: e.scalar_tensor_tensor(sst[:, 4, :], sst[:, 1, :], 1.0 / 64, sst[:, 3, :], ALU.mult, ALU.subtract), ["r_sst"], ["r_sst"])
            yield
            dv(lambda e: e.tensor_scalar(sst[:, 4, :], sst[:, 4, :], GN_EPS, None, ALU.add), ["r_sst"], ["r_sst"])
            yield
            S.op("act", lambda e: e.activation(sst[:, 5, :], sst[:, 4, :], AF.Ln), reads=["r_sst"], writes=["r_sst"])
            yield
            S.op("act", lambda e: e.activation(sst[:, 5, :], sst[:, 5, :], AF.Exp, scale=-0.5), reads=["r_sst"], writes=["r_sst"])
            yield
            dv(lambda e: e.tensor_tensor(yt[:], y[:], b3(sst[:, 2, :]), ALU.subtract), ["r_y", "r_sst"], ["r_yt"])
            yield
            dv(lambda e: e.tensor_tensor(yt[:], yt[:], b3(sst[:, 5, :]), ALU.mult), ["r_yt", "r_sst"], ["r_yt"])
            yield
            dv(lambda e: e.tensor_tensor(yt[:], yt[:], rw["gnw"][:, hs].unsqueeze(1).to_broadcast([64, NB, 64]), ALU.mult), ["r_yt", "r_gn"], ["r_yt"])
            yield
            dv(lambda e: e.tensor_tensor(yt[:], yt[:], rw["gnb"][:, hs].unsqueeze(1).to_broadcast([64, NB, 64]), ALU.add), ["r_yt", "r_gn"], ["r_yt"])
            yield
            dv(lambda e: e.tensor_tensor(ysq[:], TK[:, :, 0:64], TG[:, :, 64:65].to_broadcast([64, NB, 64]), ALU.mult), ["r_TK", "r_TG"], ["r_ysq"])
            yield
            dv(lambda e: e.tensor_tensor(yt[:], yt[:], ysq[:], ALU.add), ["r_yt", "r_ysq"], ["r_yt"])
            yield
            dv(lambda e: e.tensor_tensor(yt[:], yt[:], TG[:, :, 0:64], ALU.mult), ["r_yt", "r_TG"], ["r_yt"])
            yield
            for j in range(NB):
                mm(B1[0:64, j * 64:(j + 1) * 64], yt[:, j, :], I64, ["r_yt", "ident"], kB1, j == NB - 1)
            S.op("act", lambda e, c0=c0: e.activation(ya[:, c0 * 64:(c0 + NB) * 64], B1[0:64, 0:NB * 64], AF.Copy), reads=[kB1], writes=[("r_ya", h)])
            yield
        if "ycT" in dr:
            S.dma("sp", dr["ycT"][64 * h:64 * h + 64, tsl], ya[:], reads=[("r_ya", h)], writes=[("ya_out", g, h)])


def build_fused_nc():
    Tm = 8192
    nc = bass.Bass("TRN2", target_bir_lowering=False)
    dr = {}

    def inp(name, shape, dt=F32):
        dr[name] = nc.dram_tensor(name, list(shape), dt, kind="ExternalInput").ap()
    inp("xT", [1024, Tm])
    inp("w_sel", [1024, NCOL])
    inp("ident", [128, 128])
    inp("biasT", [128, 2, 68])
    inp("cmask", [4, 128, 512], BF16)
    inp("epat", [2, 34, Tm], BF16)
    inp("qpos", [2, Tm], BF16)
    inp("esel", [64, 4, 1024], BF16)
    rwkv_inputs(inp)
    inp("x_tok", [2048, 1024])
    inp("w_out", [1024, 1024])
    for nm in ("ln1_g", "ln1_b", "ln2_g", "ln2_b"):
        inp(nm, [1024])
    inp("w_route", [1024, 36])
    inp("b_route", [36])
    inp("w1", [32, 1024, 256])
    inp("w3", [32, 1024, 256])
    inp("w2", [32, 256, 1024])
    dr["out"] = nc.dram_tensor("out", [2048, 1024], F32, kind="ExternalOutput").ap()
    rs_in = [nc.dram_tensor("rs_in%d" % q, [4096, 512], BF16).ap() for q in range(4)]
    rs_out = [nc.dram_tensor("rs_out%d" % q, [1024, 512], BF16).ap() for q in range(4)]
    dr["rs_in"] = rs_in
    with ExitStack() as es0:
        S = Sched(nc, es0)

        def on_group_done(g):
            if g < 12:
                return
            q = g - 12
            S.coll(lambda e: e.collective_compute("ReduceScatter", ALU.add, replica_groups=[[0, 1, 2, 3], [4, 5, 6, 7]],
                                                  ins=[rs_in[q]], outs=[rs_out[q]]),
                   reads=[("rsin", gg, kc) for gg in (q, 4 + q, 8 + q, 12 + q) for kc in range(8)], writes=[("rs_out", q)])
        dr["on_group_done"] = on_group_done
        with ExitStack() as es1:
            mixer_phase(S, nc, es1, Tm, dr, True, True, fused=True)
            S.barrier(include_coll=False)
            S.replay()
        dr["ycat_q"] = rs_out
        with ExitStack() as es2:
            token_phase(S, nc, es2, 2048, dr)
            S.replay()
    return nc


def kernel(**inputs):
    d = {k: np.ascontiguousarray(np.asarray(v)) for k, v in inputs.items()}
    Tm = 8192
    nc = build_fused_nc()
    w_route = np.ascontiguousarray(np.concatenate([d["w_group"], d["w_expert"]], 1))
    b_route = np.ascontiguousarray(np.concatenate([d["b_group"], d["b_expert"]], 0))
    common = dict(w_out=d["w_out"], ln1_g=d["ln1_g"], ln1_b=d["ln1_b"], ln2_g=d["ln2_g"], ln2_b=d["ln2_b"],
                  w_route=w_route, b_route=b_route, w1=d["w1_exp"].reshape(32, 1024, 256),
                  w3=d["w3_exp"].reshape(32, 1024, 256), w2=d["w2_exp"].reshape(32, 256, 1024))
    maps = []
    for c in range(8):
        b, hg = c // 4, c % 4
        m, _ = mixer_inputs(d, c, Tm)
        m.update(rwkv_host_inputs(d, c))
        m.update(common)
        esel = np.zeros((64, 4, 1024), np.float32)
        i = np.arange(64)
        for si, base in enumerate((hg * 128, hg * 128 + 64, 512 + hg * 128, 512 + hg * 128 + 64)):
            esel[i, si, base + i] = 1.0
        m["esel"] = esel.astype(ml_dtypes.bfloat16)
        off = hg * 2048
        m["x_tok"] = np.ascontiguousarray(d["x"][b, off:off + 2048, :])
        maps.append(m)
    res = run_bass_kernel_spmd(nc, maps, core_ids=list(range(8)))
    out = np.concatenate([r["out"] for r in res.results], 0).reshape(2, Tm, 1024)
    return out.astype(np.float32)
```

```python
import numpy as np
import ml_dtypes
from contextlib import ExitStack
import concourse.bass as bass
import concourse.mybir as mybir
from concourse.bass_utils import run_bass_kernel_spmd

F32 = mybir.dt.float32
BF16 = mybir.dt.bfloat16
ALU = mybir.AluOpType
AF = mybir.ActivationFunctionType
AX = mybir.AxisListType

D = 1024
ALPHA = float(2.0 ** 0.25)
LN_EPS = 1e-5


class _Rec:
    def __init__(self):
        self.call = None

    def __getattr__(self, name):
        def f(*a, **k):
            self.call = (name, a, k)
            return self
        return f


class Sched:
    SELF_SYNC = True

    def __init__(self, nc, es, nlanes=10):
        self.nc = nc
        self.E = {}
        self.src = {}
        for n in ("pe", "act", "dve", "pool", "sp"):
            sem = es.enter_context(nc.semaphore("sem_" + n))
            self.E[n] = dict(name=n, sem=sem, cnt=0, prog=[], waited={})
            self.src[n] = (sem, 1)
        self.lanes = {}
        for q in ("sp", "pool"):
            L = []
            for i in range(nlanes):
                lid = "L%s%d" % (q, i)
                sem = es.enter_context(nc.semaphore("sem_" + lid))
                self.src[lid] = (sem, 16)
                L.append(dict(id=lid, cnt=0))
            self.lanes[q] = dict(lanes=L, nxt=0)
        self.B = {}
        csem = es.enter_context(nc.semaphore("sem_coll"))
        self.src["coll"] = (csem, 1)
        self.ncoll = 0

    def coll(self, fn, reads=(), writes=()):
        e = self.E["pool"]
        self._emit_waits(e, self._deps(reads, writes))
        self.ncoll += 1
        r = _Rec()
        fn(r)
        e["prog"].append(("coll", r.call))
        self._mark(("coll", self.ncoll), reads, writes)

    def barrier(self, include_coll=True):
        for e in self.E.values():
            deps = {}
            for n, o in self.E.items():
                if n != e["name"] and o["cnt"] > 0:
                    deps[n] = o["cnt"]
            for q in self.lanes.values():
                for lane in q["lanes"]:
                    if lane["cnt"] > 0:
                        deps[lane["id"]] = lane["cnt"]
            if self.ncoll > 0 and include_coll:
                deps["coll"] = self.ncoll
            self._emit_waits(e, deps)

    @staticmethod
    def is_psum(k):
        return isinstance(k, tuple) and isinstance(k[0], str) and k[0].startswith("P")

    def _buf(self, k):
        b = self.B.get(k)
        if b is None:
            b = self.B[k] = dict(w=None, r={})
        return b

    def _deps(self, reads, writes):
        deps = {}

        def add(t):
            if t is None:
                return
            s, n = t
            if deps.get(s, 0) < n:
                deps[s] = n
        for k in reads:
            b = self._buf(k)
            add(b["w"])
            if self.is_psum(k):
                for s, n in b["r"].items():
                    add((s, n))
        for k in writes:
            b = self._buf(k)
            add(b["w"])
            for s, n in b["r"].items():
                add((s, n))
        return deps

    def _emit_waits(self, e, deps):
        for s, n in deps.items():
            if s == e["name"]:
                if s == "pe" or not self.SELF_SYNC or n > e["cnt"]:
                    continue
            if e["waited"].get(s, 0) < n:
                e["prog"].append(("wait", s, n))
                e["waited"][s] = n

    def _mark(self, t, reads, writes):
        s, n = t
        for k in reads:
            b = self._buf(k)
            if b["r"].get(s, 0) < n:
                b["r"][s] = n
        for k in writes:
            b = self._buf(k)
            b["w"] = t
            b["r"] = {}

    def op(self, eng, fn, reads=(), writes=(), inc=True):
        e = self.E[eng]
        self._emit_waits(e, self._deps(reads, writes))
        t = (eng, e["cnt"] + 1)
        if inc:
            e["cnt"] += 1
        r = _Rec()
        fn(r)
        e["prog"].append(("inst", r.call, inc))
        self._mark(t, reads, writes)

    def dma(self, q, out, in_, reads=(), writes=()):
        e = self.E[q]
        self._emit_waits(e, self._deps(reads, writes))
        LL = self.lanes[q]
        lane = LL["lanes"][LL["nxt"]]
        LL["nxt"] = (LL["nxt"] + 1) % len(LL["lanes"])
        if lane["cnt"] > 0 and e["waited"].get(lane["id"], 0) < lane["cnt"]:
            e["prog"].append(("wait", lane["id"], lane["cnt"]))
            e["waited"][lane["id"]] = lane["cnt"]
        lane["cnt"] += 1
        t = (lane["id"], lane["cnt"])
        e["prog"].append(("dma", out, in_, lane["id"]))
        self._mark(t, reads, writes)

    def finish(self, keys):
        e = self.E["sp"]
        self._emit_waits(e, self._deps(keys, ()))

    def replay(self):
        nc = self.nc
        with nc.Block() as block:
            def run(name):
                def f(eng):
                    mysem = self.E[name]["sem"]
                    start = self.E[name].get("done", 0)
                    self.E[name]["done"] = len(self.E[name]["prog"])
                    for it in self.E[name]["prog"][start:]:
                        if it[0] == "wait":
                            sem, sc = self.src[it[1]]
                            eng.wait_ge(sem, it[2] * sc)
                        elif it[0] == "inst":
                            name_, a_, k_ = it[1]
                            ins = getattr(eng, name_)(*a_, **k_)
                            if it[2]:
                                ins.then_inc(mysem, 1)
                        elif it[0] == "coll":
                            name_, a_, k_ = it[1]
                            getattr(eng, name_)(*a_, **k_).then_inc(self.src["coll"][0], 1)
                        else:
                            sem, _ = self.src[it[3]]
                            eng.dma_start(out=it[1], in_=it[2]).then_inc(sem, 16)
                return f
            block.tensor(run("pe"))
            block.scalar(run("act"))
            block.vector(run("dve"))
            block.gpsimd(run("pool"))
            block.sync(run("sp"))


class _Cut(Exception):
    pass


def token_phase(S, nc, es, T, dr, NE=32, do_route=True):
    import os
    CUT = int(os.environ.get("TOKEN_CUT", "0"))

    def cut(k, keys):
        if CUT == k:
            S.finish(keys)
            raise _Cut()

    NT = T // 128
    NG = T // 512

    def sb(name, shape, dt):
        return es.enter_context(nc.sbuf_tensor(name, shape, dt))

    P = [es.enter_context(nc.psum_tensor("tP%d" % i, [128, 1024], F32)) for i in range(4)]

    ident = sb("t_ident", [128, 128], F32)
    ones1 = sb("t_ones1", [1, 128], F32)
    wout = sb("t_wout", [128, 8, 1024], BF16)
    lnp = sb("t_lnp", [128, 2, 1024], F32)
    stg0 = sb("t_stg0", [128, 2048], F32)
    stg = None
    wr = sb("t_wr", [128, 8, 36], F32)
    br = sb("t_br", [1, 36], F32)
    ycF = sb("t_ycT", [128, max(8 * T, 16384)], BF16)
    ycT = ycF[:, 0:8 * T].rearrange("p (a b) -> p a b", a=8)
    h1T = sb("t_h1T", [128, 8, T], BF16)
    acc = sb("t_acc", [128, NT, 1024], F32)
    xb = [sb("t_xb%d" % i, [128, 1024], F32) for i in range(2)]
    hb1 = sb("t_hb", [128, 1024], F32)
    hb = [hb1, hb1]
    h1b1 = sb("t_h1b", [128, 1024], F32)
    h1b = [h1b1, h1b1]
    hTf = sb("t_hTf", [128, 8, 128], F32)
    st6 = sb("t_st6", [128, 2, 6], F32)
    mv = sb("t_mv", [128, 2], F32)
    sm = sb("t_sm", [128, 8], F32)
    rlog = sb("t_rlog", [128, NT, 36], F32)
    gates = sb("t_gates", [128, NT, 32], F32)
    rt = sb("t_rt", [128, 12, NT, 8], F32)
    w1b = [ycF[:, (0 + i) * 2048:(1 + i) * 2048].rearrange("p (a b) -> p a b", a=8) for i in range(2)]
    w3b = [ycF[:, (2 + i) * 2048:(3 + i) * 2048].rearrange("p (a b) -> p a b", a=8) for i in range(2)]
    w2b = [ycF[:, (4 + i) * 2048:(5 + i) * 2048].rearrange("p (a b) -> p a b", a=2) for i in range(2)]
    ssb = [sb("t_ssb%d" % i, [128, 512], F32) for i in range(2)]
    hidT = [sb("t_hidT%d" % i, [128, 2, 512], BF16) for i in range(2)]
    ob = xb

    S.dma("sp", ident[:], dr["ident"], writes=["ident"])
    S.op("pool", lambda e: e.memset(ones1[:], 1.0), writes=["ones1"])
    for i in range(4):
        S.dma("sp", stg0[:].rearrange("p (a b) -> p a b", a=2),
              dr["w_out"].rearrange("(kc p) n -> p kc n", p=128)[:, 2 * i:2 * i + 2, :], writes=["stg0"])
        S.op("pool", lambda e, i=i: e.tensor_copy(wout[:, 2 * i, :], stg0[:, 0:1024]), reads=["stg0"], writes=[("wout", 2 * i)])
        S.op("dve", lambda e, i=i: e.tensor_copy(wout[:, 2 * i + 1, :], stg0[:, 1024:2048]), reads=["stg0"], writes=[("wout", 2 * i + 1)])
    for i, nm in enumerate(("ln1_g", "ln1_b")):
        S.dma("sp", lnp[:, i, :], dr[nm].partition_broadcast(128), writes=["lnp"])
    S.dma("sp", wr[:], dr["w_route"].rearrange("(kc p) n -> p kc n", p=128), writes=["wr"])
    S.dma("sp", br[:], dr["b_route"].unsqueeze(0), writes=["br"])
    def load_ycT(q):
        if "ycat_q" in dr:
            S.dma("sp", ycT[:, :, q * 512:(q + 1) * 512], dr["ycat_q"][q].rearrange("(kc p) t -> p kc t", p=128),
                  reads=[("rs_out", q)], writes=[("ycT", q)])
            return
        S.dma("sp", ycT[:, :, q * 512:(q + 1) * 512],
              dr["ycatT"].rearrange("(kc p) t -> p kc t", p=128)[:, :, q * 512:(q + 1) * 512],
              reads=dr.get("ycat_keys", []), writes=[("ycT", q)])

    stg = [stg0[:], ycF[:, 12288:16384].bitcast(F32)]
    pc = [0]

    def load_piece(dst, src, a, dkey, extra):
        k = pc[0] % 2
        pc[0] += 1
        S.dma("sp", stg[k].rearrange("p (a b) -> p a b", a=a), src, writes=["stg%d" % k] + (extra if k == 1 else []))
        S.op("pool", lambda e, k=k: e.tensor_copy(dst, stg[k].rearrange("p (a b) -> p a b", a=a)),
             reads=["stg%d" % k], writes=[dkey] + extra)

    def load_expert(e):
        s = e % 2
        yk = [("ycT", q) for q in range(NG)] if e < 2 else []
        load_piece(w1b[s], dr["w1"][e].rearrange("(kc p) f -> p kc f", p=128), 8, ("w1b", s), yk)
        load_piece(w3b[s], dr["w3"][e].rearrange("(kc p) f -> p kc f", p=128), 8, ("w3b", s), yk)
        load_piece(w2b[s], dr["w2"][e].rearrange("(fc p) d -> p fc d", p=128), 2, ("w2b", s), yk)

    def layer_norm(src_ap, srck, gi, dst_ap, dstk, tmp_ap, tmpk, eps=LN_EPS):
        for h in range(2):
            S.op("dve", lambda e, h=h: e.bn_stats(st6[:, h, :], src_ap[:, h * 512:(h + 1) * 512]),
                 reads=[srck], writes=["st6"])
        S.op("dve", lambda e: e.bn_aggr(mv[:], st6[:].rearrange("p a b -> p (a b)")), reads=["st6"], writes=["mv"])
        S.op("dve", lambda e: e.tensor_scalar(sm[:, 0:1], mv[:, 1:2], eps, None, ALU.add),
             reads=["mv"], writes=["sm0"])
        S.op("act", lambda e: e.activation(sm[:, 1:2], sm[:, 0:1], AF.Sqrt), reads=["sm0"], writes=["sm1"])
        S.op("dve", lambda e: e.reciprocal(sm[:, 2:3], sm[:, 1:2]), reads=["sm1"], writes=["sm2"])
        S.op("dve", lambda e: e.tensor_scalar(sm[:, 3:4], mv[:, 0:1], sm[:, 2:3], -1.0, ALU.mult, ALU.mult),
             reads=["mv", "sm2"], writes=["sm3"])
        S.op("act", lambda e: e.activation(tmp_ap, src_ap, AF.Identity, bias=sm[:, 3:4], scale=sm[:, 2:3]),
             reads=[srck, "sm2", "sm3"], writes=[tmpk])
        S.op("dve", lambda e: e.tensor_tensor(tmp_ap, tmp_ap, lnp[:, gi, :], ALU.mult),
             reads=[tmpk, "lnp"], writes=[tmpk])
        S.op("pool", lambda e: e.tensor_tensor(dst_ap, tmp_ap, lnp[:, gi + 1, :], ALU.add),
             reads=[tmpk, "lnp"], writes=[dstk])

    cut(1, ['ident', 'wout', 'lnp', 'wr', 'br', ('ycT', 0), ('ycT', NG - 1)])
    def t1_outproj(t):
        s = t % 2
        if t % 4 == 0:
            load_ycT(t // 4)
        S.dma("sp", xb[s][:], dr["x_tok"][t * 128:(t + 1) * 128, :], writes=[("xbt", s)])
        for half in range(2):
            for kc in range(8):
                S.op("pe", lambda e, half=half, kc=kc: e.matmul(
                    P[s][:, half * 512:(half + 1) * 512], lhsT=ycT[:, kc, t * 128:(t + 1) * 128],
                    rhs=wout[:, kc, half * 512:(half + 1) * 512], start=(kc == 0), stop=(kc == 7)),
                    reads=[("ycT", t // 4), ("wout", kc)], writes=[("P", s)], inc=(kc == 7 and half == 1))

    def t1_norm(t):
        s = t % 2
        hbt = (hb1, h1b1)[s]
        hk = ("hb", s)
        S.op("dve", lambda e: e.scalar_tensor_tensor(hbt[:], xb[s][:], ALPHA, P[s][:], ALU.mult, ALU.add),
             reads=[("xbt", s), ("P", s)], writes=[hk])
        layer_norm(hbt[:], hk, 0, acc[:, t, :], ("acc", t), hbt[:], hk)

    def t1_route(t):
        for j in range(8):
            S.op("pe", lambda e, j=j: e.transpose(P[2][:, j * 128:(j + 1) * 128], acc[:, t, j * 128:(j + 1) * 128], ident[:]),
                 reads=[("acc", t), "ident"], writes=[("PA", 0), ("PB", 0)], inc=(j == 7))
        S.op("act", lambda e: e.activation(h1T[:, :, t * 128:(t + 1) * 128],
                                           P[2][:].rearrange("p (a b) -> p a b", a=8), AF.Copy),
             reads=[("PA", 0), ("PB", 0)], writes=[("h1T", t // 4)])
        S.op("dve", lambda e: e.tensor_copy(hTf[:], P[2][:].rearrange("p (a b) -> p a b", a=8)),
             reads=[("PA", 0), ("PB", 0)], writes=["hTf"])
        for kc in range(8):
            S.op("pe", lambda e, kc=kc: e.matmul(P[3][:, 0:36], lhsT=hTf[:, kc, :], rhs=wr[:, kc, :],
                                                 start=(kc == 0), stop=False),
                 reads=["hTf", "wr"], writes=[("PA", 1), ("PB", 1)], inc=False)
        S.op("pe", lambda e: e.matmul(P[3][:, 0:36], lhsT=ones1[:], rhs=br[:], start=False, stop=True),
             reads=["ones1", "br"], writes=[("PA", 1), ("PB", 1)])
        S.op("act", lambda e: e.activation(rlog[:, t, :], P[3][:, 0:36], AF.Copy),
             reads=[("PA", 1), ("PB", 1)], writes=["rlog"])

    t1_outproj(0)
    if NT > 1:
        t1_outproj(1)
    t1_norm(0)
    for t in range(NT):
        if t + 1 < NT:
            t1_norm(t + 1)
        if t + 2 < NT:
            t1_outproj(t + 2)
        t1_route(t)
    if not do_route:
        S.op('dve', lambda e: e.memset(gates[:], 0.03), writes=['gates'])
    gl = rlog[:, :, 0:4]
    _op = S.op
    if not do_route:
        S.op = lambda *a, **k: None
    R = lambda i, w=8: rt[:, i, :, 0:w]
    R1 = lambda i: rt[:, i, :, 0]
    bc = lambda ap, w: ap.unsqueeze(2).to_broadcast([128, NT, w])

    def dv(fn, reads, writes):
        S.op("dve", fn, reads=reads, writes=writes)
    dv(lambda e: e.tensor_reduce(R1(0), gl, AX.X, ALU.max), ["rlog"], ["r0"])
    dv(lambda e: e.tensor_tensor(R(1, 4), gl, bc(R1(0), 4), ALU.is_equal), ["rlog", "r0"], ["r1"])
    dv(lambda e: e.tensor_tensor(R(2, 4), gl, bc(R1(0), 4), ALU.subtract), ["rlog", "r0"], ["r2"])
    S.op("act", lambda e: e.activation(R(2, 4), R(2, 4), AF.Exp), reads=["r2"], writes=["r2"])
    dv(lambda e: e.tensor_reduce(R1(3), R(2, 4), AX.X, ALU.add), ["r2"], ["r3"])
    for g in range(4):
        oh = rt[:, 1, :, g]
        el = rlog[:, :, 4 + g * 8:12 + g * 8]
        if g == 0:
            dv(lambda e, oh=oh, el=el: e.tensor_tensor(R(4), el, bc(oh, 8), ALU.mult), ["rlog", "r1"], ["r4"])
        else:
            dv(lambda e, oh=oh, el=el: e.tensor_tensor(R(5), el, bc(oh, 8), ALU.mult), ["rlog", "r1"], ["r5"])
            dv(lambda e: e.tensor_tensor(R(4), R(4), R(5), ALU.add), ["r4", "r5"], ["r4"])
    dv(lambda e: e.tensor_reduce(R1(6), R(4), AX.X, ALU.max), ["r4"], ["r6"])
    dv(lambda e: e.tensor_tensor(R(7), R(4), bc(R1(6), 8), ALU.is_equal), ["r4", "r6"], ["r7"])
    dv(lambda e: e.scalar_tensor_tensor(R(8), R(7), -1e30, R(4), ALU.mult, ALU.add), ["r7", "r4"], ["r8"])
    dv(lambda e: e.tensor_reduce(R1(9), R(8), AX.X, ALU.max), ["r8"], ["r9"])
    dv(lambda e: e.tensor_tensor(R(5), R(4), bc(R1(6), 8), ALU.subtract), ["r4", "r6"], ["r5"])
    S.op("act", lambda e: e.activation(R(5), R(5), AF.Exp), reads=["r5"], writes=["r5"])
    dv(lambda e: e.tensor_tensor(R(7), R(4), bc(R1(9), 8), ALU.is_ge), ["r4", "r9"], ["r7"])
    dv(lambda e: e.tensor_tensor(R(5), R(5), R(7), ALU.mult), ["r5", "r7"], ["r5"])
    dv(lambda e: e.tensor_reduce(R1(10), R(5), AX.X, ALU.add), ["r5"], ["r10"])
    dv(lambda e: e.tensor_tensor(R1(10), R1(10), R1(3), ALU.mult), ["r10", "r3"], ["r10"])
    dv(lambda e: e.tensor_scalar(R1(10), R1(10), ALPHA, None, ALU.mult), ["r10"], ["r10"])
    dv(lambda e: e.reciprocal(R1(11), R1(10)), ["r10"], ["r11"])
    dv(lambda e: e.tensor_tensor(R(5), R(5), bc(R1(11), 8), ALU.mult), ["r5", "r11"], ["r5"])
    for g in range(4):
        oh = rt[:, 1, :, g]
        dv(lambda e, oh=oh, g=g: e.tensor_tensor(gates[:, :, g * 8:(g + 1) * 8], R(5), bc(oh, 8), ALU.mult),
           ["r5", "r1"], ["gates"])

    S.op = _op
    if NE > 0:
        load_expert(0)
    if NE > 1:
        load_expert(1)
    PA = [P[2][:, 0:512], P[3][:, 0:512]]
    PB = [P[2][:, 512:1024], P[3][:, 512:1024]]
    it = 0
    pend = None
    for ex in range(NE):
        s = ex % 2
        for tg in range(NG):
            hs = (ex * NG + tg) % 2
            for fc in range(2):
                u = it % 2
                it += 1
                for kc in range(8):
                    S.op("pe", lambda e, kc=kc, fc=fc, tg=tg, s=s, u=u: e.matmul(
                        PA[u], lhsT=w1b[s][:, kc, fc * 128:(fc + 1) * 128], rhs=h1T[:, kc, tg * 512:(tg + 1) * 512],
                        start=(kc == 0), stop=(kc == 7)),
                        reads=[("w1b", s), ("h1T", tg)], writes=[("PA", u)], inc=(kc == 7))
                for kc in range(8):
                    S.op("pe", lambda e, kc=kc, fc=fc, tg=tg, s=s, u=u: e.matmul(
                        PB[u], lhsT=w3b[s][:, kc, fc * 128:(fc + 1) * 128], rhs=h1T[:, kc, tg * 512:(tg + 1) * 512],
                        start=(kc == 0), stop=(kc == 7)),
                        reads=[("w3b", s), ("h1T", tg)], writes=[("PB", u)], inc=(kc == 7))
                S.op("act", lambda e, u=u: e.activation(ssb[u][:], PA[u], AF.Silu),
                     reads=[("PA", u)], writes=[("ssb", u)])
                S.op("dve", lambda e, u=u, hs=hs, fc=fc: e.tensor_tensor(hidT[hs][:, fc, :], PB[u], ssb[u][:], ALU.mult),
                     reads=[("PB", u), ("ssb", u)], writes=[("hidT", hs)])
                if pend is not None:
                    pend(fc)

            def second(part, ex=ex, tg=tg, s=s, hs=hs):
                for tt in range(2 * part, 2 * part + 2):
                    t = tg * 4 + tt
                    o = t % 2
                    for half in range(2):
                        for fc in range(2):
                            S.op("pe", lambda e, half=half, fc=fc, tt=tt, o=o: e.matmul(
                                P[o][:, half * 512:(half + 1) * 512], lhsT=hidT[hs][:, fc, tt * 128:(tt + 1) * 128],
                                rhs=w2b[s][:, fc, half * 512:(half + 1) * 512], start=(fc == 0), stop=(fc == 1)),
                                reads=[("hidT", hs), ("w2b", s)], writes=[("P", o)], inc=(half == 1 and fc == 1))
                    S.op("dve", lambda e, t=t, o=o: e.scalar_tensor_tensor(
                        acc[:, t, :], P[o][:], gates[:, t, ex:ex + 1], acc[:, t, :], ALU.mult, ALU.add),
                        reads=[("P", o), "gates", ("acc", t)], writes=[("acc", t)])
                if part == 1 and tg == NG - 1 and ex + 2 < NE:
                    load_expert(ex + 2)
            pend = second
    if pend is not None:
        pend(0)
        pend(1)
    for i, nm in enumerate(("ln2_g", "ln2_b")):
        S.dma("sp", lnp[:, i, :], dr[nm].partition_broadcast(128), writes=["lnp"])
    for t in range(NT):
        s = t % 2
        layer_norm(acc[:, t, :], ("acc", t), 0, ob[s][:], ("xbt", s), acc[:, t, :], ("acc", t), eps=LN_EPS / (ALPHA * ALPHA))
        S.dma("sp", dr["out"][t * 128:(t + 1) * 128, :], ob[s][:], reads=[("xbt", s)], writes=[("out", t)])
    S.finish([("out", t) for t in range(NT)])


def build_token_nc(T=2048, NE=32, do_route=True):
    nc = bass.Bass("TRN2", target_bir_lowering=False)
    dr = {}

    def inp(name, shape, dt=F32):
        dr[name] = nc.dram_tensor(name, list(shape), dt, kind="ExternalInput").ap()
    inp("ycatT", [1024, T], BF16)
    inp("x_tok", [T, 1024])
    inp("w_out", [1024, 1024])
    for nm in ("ln1_g", "ln1_b", "ln2_g", "ln2_b"):
        inp(nm, [1024])
    inp("w_route", [1024, 36])
    inp("b_route", [36])
    inp("w1", [32, 1024, 256])
    inp("w3", [32, 1024, 256])
    inp("w2", [32, 256, 1024])
    inp("ident", [128, 128])
    dr["out"] = nc.dram_tensor("out", [T, 1024], F32, kind="ExternalOutput").ap()
    with ExitStack() as es:
        S = Sched(nc, es)
        try:
            token_phase(S, nc, es, T, dr, NE, do_route)
        except _Cut:
            pass
        S.replay()
    return nc


NCOL = 928
G_R, G_K, G_V, G_L1, G_GD, G_Q, G_MK, G_MV = 0, 128, 256, 384, 448, 544, 672, 800
MNEG = -30000.0


def mixer_phase(S, nc, es, Tm, dr, do_rwkv=True, do_moba=True, fused=False):
    NGm = Tm // 512
    NKT = Tm // 128

    def sb(name, shape, dt):
        return es.enter_context(nc.sbuf_tensor(name, shape, dt))

    def ps(name, shape, dt=F32):
        return es.enter_context(nc.psum_tensor(name, shape, dt))

    PI = [ps("mPI%d" % i, [128, 512]) for i in range(2)]
    PS = [ps("mPS%d" % i, [128, 512]) for i in range(2)]
    PO = ps("mPO", [128, 512])
    PM = ps("mPM", [128, 512])
    PR = [ps("mPR%d" % i, [128, 512]) for i in range(2)]

    ident = sb("m_ident", [128, 128], F32)
    identb = sb("m_identb", [128, 128], BF16)
    wb = sb("m_wb", [128, 8, NCOL], BF16)
    xs0 = sb("m_xs", [128, 4, 512], F32)
    xs = [xs0, xs0]
    xb0 = sb("m_xb", [128, 8, 512], BF16)
    xb = [xb0, xb0]
    QT = [sb("m_QT%d" % h, [98, 512], BF16) for h in range(2)]
    KT = [sb("m_KT%d" % h, [98, Tm], BF16) for h in range(2)]
    VA = sb("m_VA", [128, NKT, 2, 65], BF16)
    qf = [sb("m_qf%d" % h, [64, 512], F32) for h in range(2)]
    kmean = [sb("m_km%d" % h, [64, 32], F32) for h in range(2)]
    biasT = sb("m_biasT", [128, 2, 68], F32)
    cm = sb("m_cm", [128, 4, 512], BF16)
    est = xs0[:].rearrange("p a b -> p (a b)")[:, 0:2048]
    wst = xs0[:].rearrange("p a b -> p (a b)")[:, 0:NCOL]
    PT = [sb("m_PT%d" % i, [128, 512], BF16) for i in range(2)]
    gsel = sb("m_gsel", [128, 4, 32], F32)
    top8 = sb("m_top8", [128, 4, 8], F32)
    mbp = sb("m_mbp", [128, 4, 96], F32)
    osb = sb("m_osb", [65, 512], F32)
    rcp = sb("m_rcp", [65, 512], F32)
    ones65 = sb("m_ones65", [65, 64], F32)
    ybs = [sb("m_yb%d" % h, [64, 512], BF16) for h in range(2 if fused else 1)]
    if fused:
        Esel = sb("m_Esel", [64, 4, 1024], BF16)
        pstg = [sb("m_pstg%d" % i, [128, 512], BF16) for i in range(2)]

    S.dma("sp", ident[:], dr["ident"], writes=["ident"])
    S.op("dve", lambda e: e.tensor_copy(identb[:], ident[:]), reads=["ident"], writes=["identb"])
    S.dma("sp", biasT[:], dr["biasT"], writes=["biasT"])
    S.op("dve", lambda e: e.memset(ones65[:], 1.0), writes=["ones65"])
    S.op("dve", lambda e: e.memset(mbp[:], 0.0), writes=[("mbp", i) for i in range(4)])
    S.op("pool", lambda e: e.memset(VA[:], 1.0), writes=["VA"])
    for h in range(2):
        S.op("dve", lambda e, h=h: e.memset(kmean[h][:], 0.0), writes=[("kmean", h)])
    wst2 = [xs0[:].rearrange("p a b -> p (a b)")[:, 0:NCOL], xs0[:].rearrange("p a b -> p (a b)")[:, 1024:1024 + NCOL]]
    for kc in range(8):
        S.dma("sp", wst2[kc % 2], dr["w_sel"][kc * 128:(kc + 1) * 128, :], writes=[("wst", kc % 2)])
        S.op("pool" if kc % 2 == 0 else "dve", lambda e, kc=kc: e.tensor_copy(wb[:, kc, :], wst2[kc % 2]),
             reads=[("wst", kc % 2)], writes=[("wb", kc)])

    rw = rwkv_setup(S, nc, es, dr, sb) if do_rwkv else None

    pic = [0]
    NY = [None]
    pending_place = [None]

    def inproj_group(g, col0, M, consume):
        s = g % 2
        u = pic[0] % 2
        pic[0] += 1
        for kc in range(8):
            S.op("pe", lambda e, kc=kc, u=u, s=s: e.matmul(PI[u][0:M, :], lhsT=wb[:, kc, col0:col0 + M], rhs=xb[s][:, kc, :],
                                                          start=(kc == 0), stop=(kc == 7)),
                 reads=[("wb", kc), "xb"], writes=[("PI", u)], inc=(kc == 7))
        consume(PI[u], ("PI", u))

    def load_x(gg):
        for half in range(2):
            S.dma("sp", xs0[:], dr["xT"].rearrange("(kc p) t -> p kc t", p=128)[:, 4 * half:4 * half + 4, gg * 512:(gg + 1) * 512],
                  writes=["xs", ("wst", 0), ("wst", 1)])
            S.op("pool", lambda e, half=half: e.tensor_copy(xb0[:, 4 * half:4 * half + 4, :], xs0[:]),
                 reads=["xs"], writes=["xb"])
    load_x(0)
    S.dma("sp", cm[:], dr["cmask"].rearrange("j p t -> p j t"), writes=["cm"])
    for h in range(2):
        S.dma("sp", KT[h][64:98, :], dr["epat"][h], writes=[("KTe", h)])
        S.dma("sp", QT[h][96:98, :], dr["qpos"][:, 0:512], writes=[("QTp", h)])
    if fused:
        S.dma("sp", Esel[:], dr["esel"], writes=["Esel"])
    for g in range(NGm):
        s = g % 2
        tsl = slice(g * 512, (g + 1) * 512)
        if do_moba:
            for h in range(2):
                def cq(P, pk, h=h):
                    S.op("act", lambda e: e.activation(QT[h][0:64, :], P[0:64, :], AF.Copy), reads=[pk], writes=[("QTq", h)])
                    S.op("dve", lambda e: e.tensor_copy(qf[h][:], P[0:64, :]), reads=[pk], writes=[("qf", h)])
                inproj_group(g, G_Q + 64 * h, 64, cq)

                def ck(P, pk, h=h):
                    S.op("act", lambda e: e.activation(KT[h][0:64, tsl], P[0:64, :], AF.Copy), reads=[pk], writes=[("KTk", h)])
                    S.op("dve", lambda e: e.tensor_reduce(kmean[h][:, 2 * g:2 * g + 2], P[0:64, :].rearrange("p (a b) -> p a b", a=2), AX.X, ALU.add),
                         reads=[pk], writes=[("kmean", h)])
                inproj_group(g, G_MK + 64 * h, 64, ck)
            for tt in range(4):
                u = pic[0] % 2
                pic[0] += 1
                kt = g * 4 + tt
                for kc in range(8):
                    S.op("pe", lambda e, kc=kc, u=u, s=s, tt=tt: e.matmul(PI[u][:, 0:128], lhsT=xb[s][:, kc, tt * 128:(tt + 1) * 128],
                                                                         rhs=wb[:, kc, G_MV:G_MV + 128], start=(kc == 0), stop=(kc == 7)),
                         reads=[("wb", kc), "xb"], writes=[("PI", u)], inc=(kc == 7))
                S.op("act", lambda e, u=u, kt=kt: e.activation(VA[:, kt, :, 0:64], PI[u][:, 0:128].rearrange("p (a b) -> p a b", a=2), AF.Copy),
                     reads=[("PI", u)], writes=["VA"])
        adv = lambda: None
        gen = None
        if do_rwkv:
            rwkv_inproj(S, nc, rw, g, inproj_group, dr, ident)
            if NY[0] is None:
                class _Null:
                    op = staticmethod(lambda *a, **k: None)
                    dma = staticmethod(lambda *a, **k: None)
                NY[0] = sum(1 for _ in rwkv_compute(_Null, nc, rw, g, dr, PR, PI[0], ("PI", 0), ident))
            gen = rwkv_compute(S, nc, rw, g, dr, PR, PI[0], ("PI", 0), ident)
            n_it = 2 * (4 * g + 4) + 8
            per = -(-NY[0] // n_it)

            def adv(gen=gen, per=per):
                for _ in range(per):
                    try:
                        next(gen)
                    except StopIteration:
                        return
        if pending_place[0] is not None:
            pending_place[0]()
            pending_place[0] = None
        pf = []
        if g + 1 < NGm:
            def x_dma(half, gg=g + 1):
                S.dma("sp", xs0[:], dr["xT"].rearrange("(kc p) t -> p kc t", p=128)[:, 4 * half:4 * half + 4, gg * 512:(gg + 1) * 512],
                      writes=["xs"])

            def x_cast(half):
                S.op("act", lambda e: e.activation(xb0[:, 4 * half:4 * half + 4, :], xs0[:], AF.Copy), reads=["xs"], writes=["xb"])
            x_dma(0)
            pf = [lambda: (x_cast(0), x_dma(1)), lambda: x_cast(1)]
        if not do_moba:
            for _ in gen:
                pass
            while pf:
                pf.pop(0)()
            continue
        for h in range(2):
            for cq4 in range(4):
                blk = (g * 4 + cq4) // 2
                if blk > 0:
                    S.op("pe", lambda e, h=h, cq4=cq4, blk=blk: e.matmul(PM[:, cq4 * 32:cq4 * 32 + blk], lhsT=qf[h][:, cq4 * 128:(cq4 + 1) * 128],
                                                                       rhs=kmean[h][:, 0:blk], start=True, stop=True),
                         reads=[("qf", h), ("kmean", h)], writes=[("PM", 0)])
            adv()
            for cq4 in range(4):
                blk = (g * 4 + cq4) // 2
                mk, gk, tk = ("mbp", cq4), ("gsel", cq4), ("top8", cq4)
                mb_, gs_, t8_ = mbp[:, cq4, :], gsel[:, cq4, :], top8[:, cq4, :]
                S.op("dve", lambda e, mb_=mb_: e.memset(mb_[:, 64:96], MNEG), writes=[mk])
                if blk > 3:
                    S.op("dve", lambda e, gs_=gs_: e.memset(gs_, -1e30), writes=[gk])
                    S.op("dve", lambda e, gs_=gs_, cq4=cq4, blk=blk: e.tensor_copy(gs_[:, 0:blk], PM[:, cq4 * 32:cq4 * 32 + blk]),
                         reads=[("PM", 0)], writes=[gk])
                    S.op("dve", lambda e, gs_=gs_, t8_=t8_: e.max(t8_, gs_), reads=[gk], writes=[tk])
                    S.op("dve", lambda e, mb_=mb_, gs_=gs_, t8_=t8_, blk=blk: e.tensor_scalar(mb_[:, 64:64 + blk], gs_[:, 0:blk], t8_[:, 2:3], -MNEG,
                                                                                   ALU.is_ge, ALU.mult),
                         reads=[gk, tk], writes=[mk])
                    S.op("dve", lambda e, mb_=mb_, blk=blk: e.tensor_scalar(mb_[:, 64:64 + blk], mb_[:, 64:64 + blk], MNEG, None, ALU.add),
                         reads=[mk], writes=[mk])
                elif blk > 0:
                    S.op("dve", lambda e, mb_=mb_, blk=blk: e.memset(mb_[:, 64:64 + blk], 0.0), writes=[mk])
                S.op("dve", lambda e, mb_=mb_, blk=blk: e.memset(mb_[:, 64 + blk:65 + blk], 0.0), writes=[mk])
                adv()
            for cq4 in range(4):
                S.op("pe", lambda e, cq4=cq4: e.matmul(PO[0:96, cq4 * 128:(cq4 + 1) * 128],
                                                       lhsT=mbp[:, cq4, :], rhs=ident[:], start=True, stop=True),
                     reads=[("mbp", cq4), "ident"], writes=[("PO", 0)])
            S.op("act", lambda e, h=h: e.activation(QT[h][64:96, :], PO[64:96, :], AF.Copy), reads=[("PO", 0)], writes=[("QTm", h)])
            if pf:
                pf.pop(0)()
            nkt = 4 * g + 4

            def emit_st(kt, h=h):
                u = kt % 2
                dl = kt - 4 * g
                S.op("pe", lambda e: e.matmul(PS[u][:], lhsT=KT[h][0:98, kt * 128:(kt + 1) * 128], rhs=QT[h][0:98, :],
                                              start=True, stop=(dl < 0)),
                     reads=[("KTk", h), ("KTe", h), ("QTq", h), ("QTm", h), ("QTp", h)], writes=[("PS", u)], inc=(dl < 0))
                if dl >= 0:
                    S.op("pe", lambda e: e.matmul(PS[u][:], lhsT=identb[:], rhs=cm[:, dl, :], start=False, stop=True),
                         reads=["identb", "cm"], writes=[("PS", u)])
            emit_st(0)
            for kt in range(nkt):
                u = kt % 2
                dl = kt - 4 * g
                if kt + 1 < nkt:
                    emit_st(kt + 1)
                S.op("act", lambda e, h=h, u=u, dl=dl: e.activation(PT[u][:], PS[u][:], AF.Exp, bias=biasT[:, h, dl + 64:dl + 65], scale=0.125),
                     reads=[("PS", u), "biasT"], writes=[("PT", u)])
                adv()
                S.op("pe", lambda e, h=h, kt=kt, u=u, nkt=nkt: e.matmul(PO[0:65, :], lhsT=VA[:, kt, h, :], rhs=PT[u][:],
                                                                       start=(kt == 0), stop=(kt == nkt - 1)),
                     reads=["VA", ("PT", u)], writes=[("PO", 0)])
            S.op("act", lambda e: e.activation(rcp[64:65, :], PO[64:65, :], AF.Ln), reads=[("PO", 0)], writes=["rcp"])
            S.op("act", lambda e: e.activation(rcp[64:65, :], rcp[64:65, :], AF.Exp, scale=-1.0), reads=["rcp"], writes=["rcp"])
            S.op("act", lambda e: e.activation(osb[0:64, :], PO[0:64, :], AF.Copy), reads=[("PO", 0)], writes=["osb"])
            adv()
            adv()
            S.op("pe", lambda e: e.matmul(PM[0:64, :], lhsT=ones65[64:65, :], rhs=rcp[64:65, :], start=True, stop=True),
                 reads=["ones65", "rcp"], writes=[("PM", 0)])
            yb = ybs[h if fused else 0]
            ybk = ("yb", h if fused else 0)
            S.op("dve", lambda e: e.tensor_tensor(yb[:], osb[0:64, :], PM[0:64, :], ALU.mult), reads=["osb", ("PM", 0)], writes=[ybk])
            if not fused:
                S.dma("sp", dr["ycT"][128 + 64 * h:192 + 64 * h, tsl], yb[:], reads=[ybk], writes=[("ycT_out", g, h)])
        if gen is not None:
            for _ in gen:
                pass
        while pf:
            pf.pop(0)()
        if fused:
            def place(g=g):
                seg = g // 4
                srcs = [(rw["ya", 0], ("r_ya", 0)), (rw["ya", 1], ("r_ya", 1)), (ybs[0], ("yb", 0)), (ybs[1], ("yb", 1))]
                for kc in range(8):
                    u = pic[0] % 2
                    pic[0] += 1
                    for si, (yt_, yk_) in enumerate(srcs):
                        S.op("pe", lambda e, si=si, yt_=yt_, u=u, kc=kc: e.matmul(PI[u][:, :], lhsT=Esel[:, si, kc * 128:(kc + 1) * 128], rhs=yt_[:],
                                                                                 start=(si == 0), stop=(si == 3)),
                             reads=["Esel", yk_], writes=[("PI", u)], inc=(si == 3))
                    st = pstg[kc % 2]
                    S.op("act" if kc % 2 == 0 else "dve",
                         (lambda e, st=st, u=u: e.activation(st[:], PI[u][:, :], AF.Copy)) if kc % 2 == 0 else
                         (lambda e, st=st, u=u: e.tensor_copy(st[:], PI[u][:, :])),
                         reads=[("PI", u)], writes=[("pstg", kc % 2)])
                    S.dma("sp", dr["rs_in"][g % 4][seg * 1024 + kc * 128:seg * 1024 + (kc + 1) * 128, :], st[:],
                          reads=[("pstg", kc % 2)], writes=[("rsin", g, kc)])
                if "on_group_done" in dr:
                    dr["on_group_done"](g)
            if g + 1 < NGm:
                pending_place[0] = place
            else:
                place()
    outs = []
    for g in range(NGm):
        if fused:
            outs += [("rsin", g, kc) for kc in range(8)]
            continue
        for h in range(2):
            if do_moba:
                outs.append(("ycT_out", g, h))
            if do_rwkv:
                outs.append(("ya_out", g, h))
    if not fused:
        S.finish(outs)
    return outs


def build_mixer_nc(Tm=8192, do_rwkv=True, do_moba=True):
    nc = bass.Bass("TRN2", target_bir_lowering=False)
    dr = {}

    def inp(name, shape, dt=F32):
        dr[name] = nc.dram_tensor(name, list(shape), dt, kind="ExternalInput").ap()
    inp("xT", [1024, Tm])
    inp("w_sel", [1024, NCOL])
    inp("ident", [128, 128])
    inp("biasT", [128, 2, 68])
    inp("cmask", [4, 128, 512], BF16)
    inp("epat", [2, 34, Tm], BF16)
    inp("qpos", [2, Tm], BF16)
    rwkv_inputs(inp)
    dr["ycT"] = nc.dram_tensor("ycT", [256, Tm], BF16, kind="ExternalOutput").ap()
    with ExitStack() as es:
        S = Sched(nc, es)
        mixer_phase(S, nc, es, Tm, dr, do_rwkv, do_moba)
        S.replay()
    return nc


def rwkv_inputs(inp):
    pass


def mixer_consts(hg, Tm):
    heads = [2 * hg, 2 * hg + 1]
    slopes = [2.0 ** (-(h + 1)) for h in heads]
    p = np.arange(128, dtype=np.float32)[:, None]
    dl = (np.arange(68, dtype=np.float32) - 64)[None, :]
    biasT = np.stack([sl * (dl * 128 + p) for sl in slopes], 1).astype(np.float32)
    k = np.arange(128)[:, None]
    q = np.arange(512)[None, :]
    cmask = np.stack([np.where(j * 128 + k <= q, 0.0, MNEG) for j in range(4)], 0).astype(np.float32)
    epat = np.zeros((2, 34, Tm), np.float32)
    for n in range(min(32, Tm // 256)):
        epat[:, n, n * 256:(n + 1) * 256] = 1.0
    for i, sl in enumerate(slopes):
        epat[i, 32, :] = -8.0 * sl * 64
        epat[i, 33, :] = -8.0 * sl
    t = np.arange(Tm) % 512
    qpos = np.stack([t // 64, t % 64], 0).astype(np.float32)
    bf = ml_dtypes.bfloat16
    return dict(biasT=biasT, cmask=cmask.astype(bf), epat=epat.astype(bf), qpos=qpos.astype(bf), ident=np.eye(128, dtype=np.float32))


def mixer_inputs(d, c, Tm=8192):
    b, hg = c // 4, c % 4
    w_in = d["w_in"]
    cols = []
    for base in (0, 512, 1024):
        cols.append(np.arange(base + hg * 128, base + hg * 128 + 128))
    cols.append(np.arange(1536, 1696))
    for base in (1696, 1696 + 512, 1696 + 1024):
        cols.append(np.arange(base + hg * 128, base + hg * 128 + 128))
    cols = np.concatenate(cols)
    m = dict(mixer_consts(hg, Tm))
    m["xT"] = np.ascontiguousarray(d["x"][b, :Tm, :].T)
    m["w_sel"] = np.ascontiguousarray(w_in[:, cols])
    return m, cols


LAM = 0.6065306597126334
GN_EPS = 64e-5


def rwkv_inputs(inp):
    inp("mu_cols", [128, 8])
    inp("pcols", [64, 2, 5])
    inp("wlu", [32, 128])
    inp("alu", [32, 128])
    inp("glu", [96, 128])
    inp("gnw", [128])
    inp("gnb", [128])
    inp("rmask", [64, 384])


def rwkv_host_inputs(d, c):
    b, hg = c // 4, c % 4
    sl = slice(hg * 128, hg * 128 + 128)
    mu = d["mu_shift"]
    mu_cols = np.zeros((128, 8), np.float32)
    for i, base in enumerate((0, 512, 1024)):
        for h in range(2):
            mu_cols[0:64, 2 * i + h] = mu[base + hg * 128 + h * 64: base + hg * 128 + h * 64 + 64]
    mu_cols[0:64, 6] = mu[1536:1600]
    mu_cols[0:96, 7] = mu[1600:1696]
    pcols = np.zeros((64, 2, 5), np.float32)
    for h in range(2):
        s2 = slice(hg * 128 + h * 64, hg * 128 + h * 64 + 64)
        pcols[:, h, 0] = d["w0"][s2]
        pcols[:, h, 1] = d["a0"][s2]
        pcols[:, h, 2] = d["k_k"][s2]
        pcols[:, h, 3] = d["k_a"][s2]
        pcols[:, h, 4] = d["r_k"].reshape(-1)[s2]
    s_ = np.arange(64)[:, None]
    t_ = np.arange(64)[None, :]
    Ms = (s_ < t_).astype(np.float32)
    Mi = (s_ <= t_).astype(np.float32)
    rmask = np.concatenate([Ms, Mi, Ms, Mi, Ms.T, Ms.T], 1).astype(np.float32)[:, :384]
    return dict(mu_cols=mu_cols, pcols=pcols, wlu=np.ascontiguousarray(d["w_lora_up"][:, sl]),
                alu=np.ascontiguousarray(d["a_lora_up"][:, sl]), glu=np.ascontiguousarray(d["g_lora_up"][:, sl]),
                gnw=np.ascontiguousarray(d["gn_w"][sl]), gnb=np.ascontiguousarray(d["gn_b"][sl]), rmask=rmask)


def rwkv_setup(S, nc, es, dr, sb):
    rw = {}
    for nm, shp in (("mu", [128, 8]), ("pc", [64, 2, 5]), ("omk", [64, 2]), ("wlu", [32, 128]), ("alu", [64, 128]), ("glu", [96, 128]),
                    ("gnw", [64, 128]), ("gnb", [64, 128]), ("rmask", [64, 384]), ("ones", [64, 64]),
                    ("l1m", [64, 512]), ("gdm", [96, 512]), ("tmp", [96, 512])):
        rw[nm] = sb("r_" + nm, shp, F32)
    rw["tw"] = rw["l1m"]
    rw["gs"] = rw["gdm"]
    rw["praw"] = sb("r_praw", [96, 513], F32)
    rw["last"] = sb("r_last", [96, 8], F32)
    for nm in ("sg", "asig", "kk", "kkn", "kmod", "E1", "E3", "rkr"):
        rw[nm, 0] = rw[nm, 1] = sb("r_%s" % nm, [64, 512], F32)
    rw["cum", 0] = rw["cum", 1] = rw["kk", 0]
    rw["E2", 0] = rw["E2", 1] = rw["sg", 0]
    for nm in ("AR", "BK", "BKh"):
        rw[nm, 0] = rw[nm, 1] = sb("r_%s" % nm, [64, 8, 128], F32)
    for h in range(2):
        for nm in ("rm", "km", "vm"):
            rw[nm, h] = sb("r_%s%d" % (nm, h), [64, 512], F32)
        rw["S", h] = sb("r_S%d" % h, [64, 64], F32)
        rw["ya", h] = sb("r_ya%d" % h, [64, 512], BF16)
    for nm, shp in (("AAm", [64, 4, 256]), ("Nt", [64, 4, 64]), ("NPg", [64, 2, 4, 128]), ("TK", [64, 4, 256]), ("TG", [64, 4, 65]),
                    ("Z", [64, 2, 4, 128]), ("Tt", [64, 2, 4, 64]), ("Afm", [64, 4, 64]), ("M", [64, 4, 64]), ("Sl", [64, 4, 64]), ("Rf", [64, 4, 64]),
                    ("y", [64, 4, 64]), ("yt", [64, 4, 64]), ("ysq", [64, 4, 64]), ("sst", [64, 6, 4])):
        rw[nm] = sb("r_" + nm, shp, F32)
    S.dma("sp", rw["mu"][:], dr["mu_cols"], writes=["r_mu"])
    S.dma("sp", rw["pc"][:], dr["pcols"], writes=["r_pc"])
    S.dma("sp", rw["wlu"][:], dr["wlu"], writes=["r_wlu"])
    S.dma("sp", rw["alu"][32:64, :], dr["alu"], writes=["r_alu"])
    S.dma("sp", rw["glu"][:], dr["glu"], writes=["r_glu"])
    S.dma("sp", rw["gnw"][:], dr["gnw"].partition_broadcast(64), writes=["r_gn"])
    S.dma("sp", rw["gnb"][:], dr["gnb"].partition_broadcast(64), writes=["r_gn"])
    S.dma("sp", rw["rmask"][:], dr["rmask"], writes=["r_rmask"])
    S.op("dve", lambda e: e.memset(rw["ones"][:], 1.0), writes=["r_ones"])
    S.op("dve", lambda e: e.tensor_scalar(rw["omk"][:], rw["pc"][:, :, 3], -1.0, 1.0, ALU.mult, ALU.add), reads=["r_pc"], writes=["r_omk"])
    S.op("pool", lambda e: e.memset(rw["last"][:], 0.0), writes=["r_last"])
    for h in range(2):
        S.op("pool", lambda e, h=h: e.memset(rw["S", h][:], 0.0), writes=[("r_S", h)])
    return rw


def rwkv_inproj(S, nc, rw, g, inproj_group, dr, ident):
    tsl = slice(g * 512, (g + 1) * 512)
    I64 = ident[0:64, 0:64]
    mu = rw["mu"]
    tmp = rw["tmp"]

    def shift_mix(gi, M, dst, dkey):
        praw = rw["praw"]
        last = rw["last"]

        def consume(P, pk):
            S.op("act", lambda e: e.activation(praw[0:M, 1:513], P[0:M, :], AF.Copy), reads=[pk], writes=["r_praw"])
            S.op("act", lambda e: e.activation(praw[0:M, 0:1], last[0:M, gi:gi + 1], AF.Copy), reads=["r_last"], writes=["r_praw"])
            S.op("dve", lambda e: e.tensor_tensor(tmp[0:M, :], praw[0:M, 0:512], praw[0:M, 1:513], ALU.subtract),
                 reads=["r_praw"], writes=["r_tmp"])
            S.op("dve", lambda e: e.scalar_tensor_tensor(dst[0:M, :], tmp[0:M, :], mu[0:M, gi:gi + 1], praw[0:M, 1:513], ALU.mult, ALU.add),
                 reads=["r_tmp", "r_mu", "r_praw"], writes=[dkey])
            S.op("act", lambda e: e.activation(last[0:M, gi:gi + 1], praw[0:M, 512:513], AF.Copy), reads=["r_praw"], writes=["r_last"])
        return consume
    for h in range(2):
        inproj_group(g, G_R + 64 * h, 64, shift_mix(0 + h, 64, rw["rm", h], ("r_rm", h)))
        inproj_group(g, G_K + 64 * h, 64, shift_mix(2 + h, 64, rw["km", h], ("r_km", h)))
        inproj_group(g, G_V + 64 * h, 64, shift_mix(4 + h, 64, rw["vm", h], ("r_vm", h)))
    inproj_group(g, G_L1, 64, shift_mix(6, 64, rw["l1m"], "r_l1m"))
    inproj_group(g, G_GD, 96, shift_mix(7, 96, rw["gdm"], "r_gdm"))


def rwkv_compute(S, nc, rw, g, dr, PR, PM, kPM, ident):
    tsl = slice(g * 512, (g + 1) * 512)
    I64 = ident[0:64, 0:64]
    tmp = rw["tmp"]
    S.op("act", lambda e: e.activation(rw["l1m"][0:32, :], rw["l1m"][0:32, :], AF.Tanh), reads=["r_l1m"], writes=["r_l1m"])
    yield
    S.op("act", lambda e: e.activation(rw["gdm"][:], rw["gdm"][:], AF.Sigmoid), reads=["r_gdm"], writes=["r_gdm"])
    yield
    pc = rw["pc"]
    for h in range(2):
        hs = slice(h * 64, h * 64 + 64)
        rm, km, vm, sg, asig, kk, kkn, kmod, cum, E1, E2, E3, rkr = [rw[n, h] for n in
                                                                      ("rm", "km", "vm", "sg", "asig", "kk", "kkn", "kmod", "cum", "E1", "E2", "E3", "rkr")]
        AR, BK, BKh, Sst, ya = rw["AR", h], rw["BK", h], rw["BKh", h], rw["S", h], rw["ya", h]
        P0, P1 = PR[0], PR[1]
        k0, k1 = ("PR", 0), ("PR", 1)
        S.op("pe", lambda e: e.matmul(P0[0:64, :], lhsT=rw["wlu"][:, hs], rhs=rw["l1m"][0:32, :], start=True, stop=True),
             reads=["r_wlu", "r_l1m"], writes=[k0])
        S.op("act", lambda e: e.activation(sg[:], P0[0:64, :], AF.Sigmoid, bias=pc[:, h, 0:1]), reads=[k0, "r_pc"], writes=[("r_sg", 0)])
        yield
        S.op("pe", lambda e: e.matmul(P1[0:64, :], lhsT=rw["alu"][32:64, hs], rhs=rw["l1m"][32:64, :], start=True, stop=True),
             reads=["r_alu", "r_l1m"], writes=[k1])
        S.op("act", lambda e: e.activation(asig[:], P1[0:64, :], AF.Sigmoid, bias=pc[:, h, 1:2]), reads=[k1, "r_pc"], writes=[("r_asig", 0)])
        yield
        S.op("dve", lambda e: e.tensor_scalar(kk[:], km[:], pc[:, h, 2:3], None, ALU.mult), reads=[("r_km", h), "r_pc"], writes=[("r_kk", 0)])
        yield
        S.op("dve", lambda e: e.tensor_tensor(tmp[0:64, :], kk[:], kk[:], ALU.mult), reads=[("r_kk", 0)], writes=["r_tmp"])
        yield
        S.op("pe", lambda e: e.matmul(P0[0:64, :], lhsT=rw["ones"][:], rhs=tmp[0:64, :], start=True, stop=True),
             reads=["r_ones", "r_tmp"], writes=[k0])
        S.op("dve", lambda e: e.tensor_scalar(tmp[0:64, :], P0[0:64, :], 1e-18, None, ALU.max), reads=[k0], writes=["r_tmp"])
        yield
        S.op("act", lambda e: e.activation(tmp[0:64, :], tmp[0:64, :], AF.Ln), reads=["r_tmp"], writes=["r_tmp"])
        yield
        S.op("act", lambda e: e.activation(tmp[0:64, :], tmp[0:64, :], AF.Exp, scale=-0.5), reads=["r_tmp"], writes=["r_tmp"])
        yield
        S.op("dve", lambda e: e.tensor_tensor(kkn[:], kk[:], tmp[0:64, :], ALU.mult), reads=[("r_kk", 0), "r_tmp"], writes=[("r_kkn", 0)])
        yield
        S.op("dve", lambda e: e.tensor_scalar(tmp[0:64, :], asig[:], pc[:, h, 3:4], rw["omk"][:, h:h + 1], ALU.mult, ALU.add),
             reads=[("r_asig", 0), "r_pc", "r_omk"], writes=["r_tmp"])
        yield
        S.op("dve", lambda e: e.tensor_tensor(kmod[:], km[:], tmp[0:64, :], ALU.mult), reads=[("r_km", h), "r_tmp"], writes=[("r_kmod", 0)])
        yield
        S.op("dve", lambda e: e.scalar_tensor_tensor(rkr[:], rm[:], pc[:, h, 4:5], kmod[:], ALU.mult, ALU.mult),
             reads=[("r_rm", h), ("r_kmod", 0), "r_pc"], writes=[("r_rkr", 0)])
        yield
        for c in range(8):
            cs = slice(c * 64, c * 64 + 64)
            S.op("dve", lambda e, cs=cs: e.tensor_tensor_scan(cum[:, cs], rw["ones"][:], sg[:, cs], 0.0, ALU.mult, ALU.add),
                 reads=[("r_sg", 0), "r_ones"], writes=[("r_kk", 0)])
            yield
        S.op("act", lambda e: e.activation(E1[:], cum[:], AF.Exp, scale=-LAM), reads=[("r_kk", 0)], writes=[("r_E1", 0)])
        yield
        S.op("dve", lambda e: e.tensor_tensor(tmp[0:64, :], cum[:], sg[:], ALU.subtract), reads=[("r_kk", 0), ("r_sg", 0)], writes=["r_tmp"])
        yield
        S.op("act", lambda e: e.activation(E3[:], tmp[0:64, :], AF.Exp, scale=-LAM), reads=["r_tmp"], writes=[("r_E3", 0)])
        yield
        S.op("act", lambda e: e.activation(E2[:], cum[:], AF.Exp, scale=LAM), reads=[("r_kk", 0)], writes=[("r_sg", 0)])
        yield
        v3 = lambda ap: ap.rearrange("p (c t) -> p c t", c=8)
        S.op("dve", lambda e: e.scalar_tensor_tensor(AR[:, :, 0:64], v3(kkn[:]), -1.0, v3(E3[:]), ALU.mult, ALU.mult),
             reads=[("r_kkn", 0), ("r_E3", 0)], writes=[("r_AR", 0)])
        yield
        S.op("dve", lambda e: e.tensor_tensor(AR[:, :, 64:128], v3(rm[:]), v3(E1[:]), ALU.mult),
             reads=[("r_rm", h), ("r_E1", 0)], writes=[("r_AR", 0)])
        yield
        S.op("dve", lambda e: e.tensor_tensor(tmp[0:64, :], kkn[:], asig[:], ALU.mult), reads=[("r_kkn", 0), ("r_asig", 0)], writes=["r_tmp"])
        yield
        S.op("dve", lambda e: e.tensor_tensor(BK[:, :, 0:64], v3(tmp[0:64, :]), v3(E2[:]), ALU.mult), reads=["r_tmp", ("r_sg", 0)], writes=[("r_BK", 0)])
        yield
        S.op("dve", lambda e: e.tensor_tensor(BK[:, :, 64:128], v3(kmod[:]), v3(E2[:]), ALU.mult), reads=[("r_kmod", 0), ("r_sg", 0)], writes=[("r_BK", 0)])
        yield
        S.op("dve", lambda e: e.tensor_tensor(BKh[:], BK[:], v3(E1[:])[:, :, 63:64].to_broadcast([64, 8, 128]), ALU.mult),
             reads=[("r_BK", 0), ("r_E1", 0)], writes=[("r_BKh", 0)])
        yield
        Tt = rw["Tt"]
        AAm, Nt, NPg, TK, TG, Z, Afm, Mm, Sl, Rf, y, yt, ysq, sst = [rw[n] for n in
                                                                       ("AAm", "Nt", "NPg", "TK", "TG", "Z", "Afm", "M", "Sl", "Rf", "y", "yt", "ysq", "sst")]
        rmask = rw["rmask"]
        B0, B1, B2 = PR[0], PR[1], PM
        kB0, kB1, kB2 = ("PR", 0), ("PR", 1), kPM
        NB = 4
        E1v = v3(E1[:])
        rd = [("r_AR", 0), ("r_BK", 0)]

        def mm(out, lhsT, rhs, reads, wk, inc, start=True, stop=True):
            S.op("pe", lambda e: e.matmul(out, lhsT=lhsT, rhs=rhs, start=start, stop=stop), reads=reads, writes=[wk], inc=inc)
        for hb in range(2):
            c0 = hb * NB
            banks = [(B0, kB0), (B0, kB0), (B1, kB1), (B1, kB1)]
            for j in range(NB):
                c = c0 + j
                Bj, kBj = banks[j]
                off = (j % 2) * 256
                mm(Bj[0:64, off:off + 128], BK[:, c, 0:64], AR[:, c, :], rd, kBj, False)
                mm(Bj[0:64, off + 128:off + 256], BK[:, c, 64:128], AR[:, c, :], rd, kBj, j % 2 == 1)
            for b2, (Bj, kBj) in enumerate(((B0, kB0), (B1, kB1))):
                S.op("dve", lambda e, b2=b2, Bj=Bj: e.tensor_tensor(AAm[:, 2 * b2:2 * b2 + 2, :], Bj[0:64, 0:512].rearrange("p (a b) -> p a b", a=2),
                                                                  rmask[:, 0:256].unsqueeze(1).to_broadcast([64, 2, 256]), ALU.mult),
                     reads=[kBj, "r_rmask"], writes=["r_AAm"])
                yield
            for j in range(NB):
                c = c0 + j
                mm(B2[0:64, j * 64:(j + 1) * 64], AR[:, c, 0:64], BK[:, c, 0:64], rd, kB2, j == NB - 1)
            S.op("dve", lambda e: e.tensor_tensor(Nt[:], B2[0:64, 0:256].rearrange("p (a b) -> p a b", a=NB),
                                                  rmask[:, 256:320].unsqueeze(1).to_broadcast([64, NB, 64]), ALU.mult),
                 reads=[kB2, "r_rmask"], writes=["r_Nt"])
            yield
            for j in range(NB):
                c = c0 + j
                cs = slice(c * 64, c * 64 + 64)
                Bj, kBj = banks[j]
                off = (j % 2) * 256
                mm(Bj[0:64, off:off + 64], vm[:, cs], I64, [("r_vm", h), "ident"], kBj, False)
                mm(Bj[0:64, off + 64:off + 128], AR[:, c, 0:64], I64, rd + ["ident"], kBj, False)
                mm(Bj[0:64, off + 128:off + 192], BKh[:, c, 0:64], I64, [("r_BKh", 0), "ident"], kBj, False)
                mm(Bj[0:64, off + 192:off + 256], BKh[:, c, 64:128], I64, [("r_BKh", 0), "ident"], kBj, j % 2 == 1)
            for b2, (Bj, kBj) in enumerate(((B0, kB0), (B1, kB1))):
                S.op("act", lambda e, b2=b2, Bj=Bj: e.activation(TK[:, 2 * b2:2 * b2 + 2, :], Bj[0:64, 0:512].rearrange("p (a b) -> p a b", a=2), AF.Copy),
                     reads=[kBj], writes=["r_TK"])
                yield
            for j in range(NB):
                c = c0 + j
                cs = slice(c * 64, c * 64 + 64)
                mm(B2[0:64, j * 65:j * 65 + 64], rw["gdm"][:, cs], rw["glu"][:, hs], ["r_gdm", "r_glu"], kB2, False)
                mm(B2[0:64, j * 65 + 64:j * 65 + 65], rkr[:, cs], rw["ones"][:, 0:1], [("r_rkr", 0), "r_ones"], kB2, j == NB - 1)
            S.op("act", lambda e: e.activation(TG[:], B2[0:64, 0:NB * 65].rearrange("p (a b) -> p a b", a=NB), AF.Copy), reads=[kB2], writes=["r_TG"])
            yield
            for j in range(NB):
                mm(B2[0:64, j * 64:(j + 1) * 64], AAm[:, j, 128:192], TK[:, j, 0:64], ["r_AAm", "r_TK"], kB2, j == NB - 1)
            S.op("dve", lambda e: e.tensor_copy(Z[:, 0, :, 64:128], B2[0:64, 0:256].rearrange("p (a b) -> p a b", a=NB)), reads=[kB2], writes=[("r_Z", 0)])
            yield
            S.op("dve", lambda e: e.tensor_copy(Z[:, 0, :, 0:64], TK[:, :, 64:128]), reads=["r_TK"], writes=[("r_Z", 0)])
            yield
            S.op("dve", lambda e: e.tensor_tensor(Tt[:, 1], I64.unsqueeze(1).to_broadcast([64, NB, 64]), AAm[:, :, 0:64], ALU.add),
                 reads=["ident", "r_AAm"], writes=[("r_T", 1)])
            yield
            for k in range(6):
                Nk = (lambda j: AAm[:, j, 0:64]) if k == 0 else (lambda j, k=k: NPg[:, k % 2, j, 0:64])
                Ntk = (lambda j: Nt[:, j, :]) if k == 0 else (lambda j, k=k: NPg[:, k % 2, j, 64:128])
                rdk = ["r_AAm", "r_Nt"] if k == 0 else [("r_NP", k % 2)]
                if k < 5:
                    for j in range(NB):
                        if k < 4:
                            mm(B1[0:64, j * 128:j * 128 + 64], Ntk(j), Nk(j), rdk, kB1, False)
                        mm(B1[0:64, j * 128 + 64:(j + 1) * 128], Nk(j), Ntk(j), rdk, kB1, j == NB - 1)
                    S.op("act", lambda e, k=k: e.activation(NPg[:, (k + 1) % 2], B1[0:64, 0:512].rearrange("p (a b) -> p a b", a=NB), AF.Copy),
                         reads=[kB1], writes=[("r_NP", (k + 1) % 2)])
                    yield
                if k >= 1:
                    ti, to = k % 2, (k + 1) % 2
                    for j in range(NB):
                        mm(B0[0:64, j * 64:(j + 1) * 64], Ntk(j), Tt[:, ti, j, :], rdk + [("r_T", ti)], kB0, j == NB - 1)
                    S.op("dve", lambda e, ti=ti, to=to: e.tensor_tensor(Tt[:, to], B0[0:64, 0:NB * 64].rearrange("p (a b) -> p a b", a=NB), Tt[:, ti], ALU.add),
                         reads=[kB0, ("r_T", ti)], writes=[("r_T", to)])
                    yield
            for j in range(NB):
                mm(B0[0:64, j * 128:(j + 1) * 128], Tt[:, 0, j, :], Z[:, 0, j, :], [("r_T", 0), ("r_Z", 0)], kB0, j == NB - 1)
            S.op("act", lambda e: e.activation(Z[:, 1], B0[0:64, 0:512].rearrange("p (a b) -> p a b", a=NB), AF.Copy), reads=[kB0], writes=[("r_Z", 1)])
            yield
            Zf = Z[:, 1]
            zk = [("r_Z", 1)]
            for j in range(NB):
                Ah, ul = Zf[:, j, 0:64], Zf[:, j, 64:128]
                vt, bh, kh = TK[:, j, 0:64], TK[:, j, 128:192], TK[:, j, 192:256]
                mm(B0[0:64, 256 + j * 64:256 + (j + 1) * 64], Ah, bh, zk + ["r_TK"], kB0, False)
                mm(B1[0:64, j * 64:(j + 1) * 64], bh, ul, zk + ["r_TK"], kB1, False, start=True, stop=False)
                mm(B1[0:64, j * 64:(j + 1) * 64], kh, vt, ["r_TK"], kB1, False, start=False, stop=True)
                mm(B1[0:64, 256 + j * 64:256 + (j + 1) * 64], Ah, AAm[:, j, 64:128], zk + ["r_AAm"], kB1, j == NB - 1)
            v4 = lambda ap: ap.rearrange("p (a b) -> p a b", a=NB)
            S.op("dve", lambda e: e.tensor_tensor(Mm[:], I64.unsqueeze(1).to_broadcast([64, NB, 64]),
                                                  E1v[:, c0:c0 + NB, 63:64].to_broadcast([64, NB, 64]), ALU.mult),
                 reads=["ident", ("r_E1", 0)], writes=["r_M"])
            yield
            S.op("dve", lambda e: e.tensor_tensor(Mm[:], Mm[:], v4(B0[0:64, 256:512]), ALU.add), reads=[kB0, "r_M"], writes=["r_M"])
            yield
            S.op("act", lambda e: e.activation(Sl[:], v4(B1[0:64, 0:256]), AF.Copy), reads=[kB1], writes=["r_Sl"])
            yield
            S.op("dve", lambda e: e.tensor_tensor(Rf[:], v4(B1[0:64, 256:512]), AR[:, c0:c0 + NB, 64:128], ALU.add), reads=[kB1] + rd, writes=["r_Rf"])
            yield
            for j in range(NB):
                yo = B0[0:64, j * 64:(j + 1) * 64]
                mm(yo, Rf[:, j, :], Sst[:], ["r_Rf", ("r_S", h)], kB0, False, start=True, stop=False)
                mm(yo, AAm[:, j, 64:128], Zf[:, j, 64:128], zk + ["r_AAm"], kB0, False, start=False, stop=False)
                mm(yo, AAm[:, j, 192:256], TK[:, j, 0:64], ["r_AAm", "r_TK"], kB0, False, start=False, stop=True)
                mm(B2[0:64, 0:64], Mm[:, j, :], Sst[:], ["r_M", ("r_S", h)], kB2, True)
                S.op("dve", lambda e, j=j: e.tensor_tensor(Sst[:], B2[0:64, 0:64], Sl[:, j, :], ALU.add), reads=[kB2, "r_Sl"], writes=[("r_S", h)])
                yield
            S.op("act", lambda e: e.activation(y[:], v4(B0[0:64, 0:256]), AF.Copy), reads=[kB0], writes=["r_y"])
            yield
            b3 = lambda ap: ap.unsqueeze(2).to_broadcast([64, NB, 64])
            dv = lambda fn, r_, w_: S.op("dve", fn, reads=r_, writes=w_)
            dv(lambda e: e.tensor_reduce(sst[:, 0, :], y[:], AX.X, ALU.add), ["r_y"], ["r_sst"])
            yield
            dv(lambda e: e.tensor_tensor(ysq[:], y[:], y[:], ALU.mult), ["r_y"], ["r_ysq"])
            yield
            dv(lambda e: e.tensor_reduce(sst[:, 1, :], ysq[:], AX.X, ALU.add), ["r_ysq"], ["r_sst"])
            yield
            dv(lambda e: e.tensor_scalar(sst[:, 2, :], sst[:, 0, :], 1.0 / 64, None, ALU.mult), ["r_sst"], ["r_sst"])
            yield
            dv(lambda e: e.tensor_tensor(sst[:, 3, :], sst[:, 2, :], sst[:, 2, :], ALU.mult), ["r_sst"], ["r_sst"])
            yield
            dv(lambda e: e.scalar_tensor_tensor(sst[:, 4, :], sst[:, 1, :], 1.0 / 64, sst[:, 3, :], ALU.mult, ALU.subtract), ["r_sst"], ["r_sst"])
            yield
            dv(lambda e: e.tensor_scalar(sst[:, 4, :], sst[:, 4, :], GN_EPS, None, ALU.add), ["r_sst"], ["r_sst"])
            yield
            S.op("act", lambda e: e.activation(sst[:, 5, :], sst[:, 4, :], AF.Ln), reads=["r_sst"], writes=["r_sst"])
            yield
            S.op("act", lambda e: e.activation(sst[:, 5, :], sst[:, 5, :], AF.Exp, scale=-0.5), reads=["r_sst"], writes=["r_sst"])
            yield
            dv(lambda e: e.tensor_tensor(yt[:], y[:], b3(sst[:, 2, :]), ALU.subtract), ["r_y", "r_sst"], ["r_yt"])
            yield
            dv(lambda e: e.tensor_tensor(yt[:], yt[:], b3(sst[:, 5, :]), ALU.mult), ["r_yt", "r_sst"], ["r_yt"])
            yield
            dv(lambda e: e.tensor_tensor(yt[:], yt[:], rw["gnw"][:, hs].unsqueeze(1).to_broadcast([64, NB, 64]), ALU.mult), ["r_yt", "r_gn"], ["r_yt"])
            yield
            dv(lambda e: e.tensor_tensor(yt[:], yt[:], rw["gnb"][:, hs].unsqueeze(1).to_broadcast([64, NB, 64]), ALU.add), ["r_yt", "r_gn"], ["r_yt"])
            yield
            dv(lambda e: e.tensor_tensor(ysq[:], TK[:, :, 0:64], TG[:, :, 64:65].to_broadcast([64, NB, 64]), ALU.mult), ["r_TK", "r_TG"], ["r_ysq"])
            yield
            dv(lambda e: e.tensor_tensor(yt[:], yt[:], ysq[:], ALU.add), ["r_yt", "r_ysq"], ["r_yt"])
            yield
            dv(lambda e: e.tensor_tensor(yt[:], yt[:], TG[:, :, 0:64], ALU.mult), ["r_yt", "r_TG"], ["r_yt"])
            yield
            for j in range(NB):
                mm(B1[0:64, j * 64:(j + 1) * 64], yt[:, j, :], I64, ["r_yt", "ident"], kB1, j == NB - 1)
            S.op("act", lambda e, c0=c0: e.activation(ya[:, c0 * 64:(c0 + NB) * 64], B1[0:64, 0:NB * 64], AF.Copy), reads=[kB1], writes=[("r_ya", h)])
            yield
        if "ycT" in dr:
            S.dma("sp", dr["ycT"][64 * h:64 * h + 64, tsl], ya[:], reads=[("r_ya", h)], writes=[("ya_out", g, h)])


def build_fused_nc():
    Tm = 8192
    nc = bass.Bass("TRN2", target_bir_lowering=False)
    dr = {}

    def inp(name, shape, dt=F32):
        dr[name] = nc.dram_tensor(name, list(shape), dt, kind="ExternalInput").ap()
    inp("xT", [1024, Tm])
    inp("w_sel", [1024, NCOL])
    inp("ident", [128, 128])
    inp("biasT", [128, 2, 68])
    inp("cmask", [4, 128, 512], BF16)
    inp("epat", [2, 34, Tm], BF16)
    inp("qpos", [2, Tm], BF16)
    inp("esel", [64, 4, 1024], BF16)
    rwkv_inputs(inp)
    inp("x_tok", [2048, 1024])
    inp("w_out", [1024, 1024])
    for nm in ("ln1_g", "ln1_b", "ln2_g", "ln2_b"):
        inp(nm, [1024])
    inp("w_route", [1024, 36])
    inp("b_route", [36])
    inp("w1", [32, 1024, 256])
    inp("w3", [32, 1024, 256])
    inp("w2", [32, 256, 1024])
    dr["out"] = nc.dram_tensor("out", [2048, 1024], F32, kind="ExternalOutput").ap()
    rs_in = [nc.dram_tensor("rs_in%d" % q, [4096, 512], BF16).ap() for q in range(4)]
    rs_out = [nc.dram_tensor("rs_out%d" % q, [1024, 512], BF16).ap() for q in range(4)]
    dr["rs_in"] = rs_in
    with ExitStack() as es0:
        S = Sched(nc, es0)

        def on_group_done(g):
            if g < 12:
                return
            q = g - 12
            S.coll(lambda e: e.collective_compute("ReduceScatter", ALU.add, replica_groups=[[0, 1, 2, 3], [4, 5, 6, 7]],
                                                  ins=[rs_in[q]], outs=[rs_out[q]]),
                   reads=[("rsin", gg, kc) for gg in (q, 4 + q, 8 + q, 12 + q) for kc in range(8)], writes=[("rs_out", q)])
        dr["on_group_done"] = on_group_done
        with ExitStack() as es1:
            mixer_phase(S, nc, es1, Tm, dr, True, True, fused=True)
            S.barrier(include_coll=False)
            S.replay()
        dr["ycat_q"] = rs_out
        with ExitStack() as es2:
            token_phase(S, nc, es2, 2048, dr)
            S.replay()
    return nc


def kernel(**inputs):
    d = {k: np.ascontiguousarray(np.asarray(v)) for k, v in inputs.items()}
    Tm = 8192
    nc = build_fused_nc()
    w_route = np.ascontiguousarray(np.concatenate([d["w_group"], d["w_expert"]], 1))
    b_route = np.ascontiguousarray(np.concatenate([d["b_group"], d["b_expert"]], 0))
    common = dict(w_out=d["w_out"], ln1_g=d["ln1_g"], ln1_b=d["ln1_b"], ln2_g=d["ln2_g"], ln2_b=d["ln2_b"],
                  w_route=w_route, b_route=b_route, w1=d["w1_exp"].reshape(32, 1024, 256),
                  w3=d["w3_exp"].reshape(32, 1024, 256), w2=d["w2_exp"].reshape(32, 256, 1024))
    maps = []
    for c in range(8):
        b, hg = c // 4, c % 4
        m, _ = mixer_inputs(d, c, Tm)
        m.update(rwkv_host_inputs(d, c))
        m.update(common)
        esel = np.zeros((64, 4, 1024), np.float32)
        i = np.arange(64)
        for si, base in enumerate((hg * 128, hg * 128 + 64, 512 + hg * 128, 512 + hg * 128 + 64)):
            esel[i, si, base + i] = 1.0
        m["esel"] = esel.astype(ml_dtypes.bfloat16)
        off = hg * 2048
        m["x_tok"] = np.ascontiguousarray(d["x"][b, off:off + 2048, :])
        maps.append(m)
    res = run_bass_kernel_spmd(nc, maps, core_ids=list(range(8)))
    out = np.concatenate([r["out"] for r in res.results], 0).reshape(2, Tm, 1024)
    return out.astype(np.float32)
```

```python
import numpy as np
import ml_dtypes
from contextlib import ExitStack
import concourse.bass as bass
import concourse.mybir as mybir
from concourse.bass_utils import run_bass_kernel_spmd

F32 = mybir.dt.float32
BF16 = mybir.dt.bfloat16
ALU = mybir.AluOpType
AF = mybir.ActivationFunctionType
AX = mybir.AxisListType

D = 1024
ALPHA = float(2.0 ** 0.25)
LN_EPS = 1e-5


class _Rec:
    def __init__(self):
        self.call = None

    def __getattr__(self, name):
        def f(*a, **k):
            self.call = (name, a, k)
            return self
        return f


class Sched:
    SELF_SYNC = True

    def __init__(self, nc, es, nlanes=16):
        self.nc = nc
        self.E = {}
        self.src = {}
        for n in ("pe", "act", "dve", "pool", "sp"):
            sem = es.enter_context(nc.semaphore("sem_" + n))
            self.E[n] = dict(name=n, sem=sem, cnt=0, prog=[], waited={})
            self.src[n] = (sem, 1)
        self.lanes = {}
        for q in ("sp", "pool"):
            L = []
            for i in range(nlanes):
                lid = "L%s%d" % (q, i)
                sem = es.enter_context(nc.semaphore("sem_" + lid))
                self.src[lid] = (sem, 16)
                L.append(dict(id=lid, cnt=0))
            self.lanes[q] = dict(lanes=L, nxt=0)
        self.B = {}
        csem = es.enter_context(nc.semaphore("sem_coll"))
        self.src["coll"] = (csem, 1)
        self.ncoll = 0

    def coll(self, fn, reads=(), writes=()):
        e = self.E["pool"]
        self._emit_waits(e, self._deps(reads, writes))
        self.ncoll += 1
        r = _Rec()
        fn(r)
        e["prog"].append(("coll", r.call))
        self._mark(("coll", self.ncoll), reads, writes)

    def barrier(self, include_coll=True):
        for e in self.E.values():
            deps = {}
            for n, o in self.E.items():
                if n != e["name"] and o["cnt"] > 0:
                    deps[n] = o["cnt"]
            for q in self.lanes.values():
                for lane in q["lanes"]:
                    if lane["cnt"] > 0:
                        deps[lane["id"]] = lane["cnt"]
            if self.ncoll > 0 and include_coll:
                deps["coll"] = self.ncoll
            self._emit_waits(e, deps)

    @staticmethod
    def is_psum(k):
        return isinstance(k, tuple) and isinstance(k[0], str) and k[0].startswith("P")

    def _buf(self, k):
        b = self.B.get(k)
        if b is None:
            b = self.B[k] = dict(w=None, r={})
        return b

    def _deps(self, reads, writes):
        deps = {}

        def add(t):
            if t is None:
                return
            s, n = t
            if deps.get(s, 0) < n:
                deps[s] = n
        for k in reads:
            b = self._buf(k)
            add(b["w"])
            if self.is_psum(k):
                for s, n in b["r"].items():
                    add((s, n))
        for k in writes:
            b = self._buf(k)
            add(b["w"])
            for s, n in b["r"].items():
                add((s, n))
        return deps

    def _emit_waits(self, e, deps):
        for s, n in deps.items():
            if s == e["name"]:
                if s == "pe" or not self.SELF_SYNC or n > e["cnt"]:
                    continue
            if e["waited"].get(s, 0) < n:
                e["prog"].append(("wait", s, n))
                e["waited"][s] = n

    def _mark(self, t, reads, writes):
        s, n = t
        for k in reads:
            b = self._buf(k)
            if b["r"].get(s, 0) < n:
                b["r"][s] = n
        for k in writes:
            b = self._buf(k)
            b["w"] = t
            b["r"] = {}

    def op(self, eng, fn, reads=(), writes=(), inc=True):
        e = self.E[eng]
        self._emit_waits(e, self._deps(reads, writes))
        t = (eng, e["cnt"] + 1)
        if inc:
            e["cnt"] += 1
        r = _Rec()
        fn(r)
        e["prog"].append(("inst", r.call, inc))
        self._mark(t, reads, writes)

    def dma(self, q, out, in_, reads=(), writes=()):
        e = self.E[q]
        self._emit_waits(e, self._deps(reads, writes))
        LL = self.lanes[q]
        lane = LL["lanes"][LL["nxt"]]
        LL["nxt"] = (LL["nxt"] + 1) % len(LL["lanes"])
        if lane["cnt"] > 0 and e["waited"].get(lane["id"], 0) < lane["cnt"]:
            e["prog"].append(("wait", lane["id"], lane["cnt"]))
            e["waited"][lane["id"]] = lane["cnt"]
        lane["cnt"] += 1
        t = (lane["id"], lane["cnt"])
        e["prog"].append(("dma", out, in_, lane["id"]))
        self._mark(t, reads, writes)

    def finish(self, keys):
        e = self.E["sp"]
        self._emit_waits(e, self._deps(keys, ()))

    def replay(self):
        nc = self.nc
        with nc.Block() as block:
            def run(name):
                def f(eng):
                    mysem = self.E[name]["sem"]
                    start = self.E[name].get("done", 0)
                    self.E[name]["done"] = len(self.E[name]["prog"])
                    for it in self.E[name]["prog"][start:]:
                        if it[0] == "wait":
                            sem, sc = self.src[it[1]]
                            eng.wait_ge(sem, it[2] * sc)
                        elif it[0] == "inst":
                            name_, a_, k_ = it[1]
                            ins = getattr(eng, name_)(*a_, **k_)
                            if it[2]:
                                ins.then_inc(mysem, 1)
                        elif it[0] == "coll":
                            name_, a_, k_ = it[1]
                            getattr(eng, name_)(*a_, **k_).then_inc(self.src["coll"][0], 1)
                        else:
                            sem, _ = self.src[it[3]]
                            eng.dma_start(out=it[1], in_=it[2]).then_inc(sem, 16)
                return f
            block.tensor(run("pe"))
            block.scalar(run("act"))
            block.vector(run("dve"))
            block.gpsimd(run("pool"))
            block.sync(run("sp"))


class _Cut(Exception):
    pass


def token_phase(S, nc, es, T, dr, NE=32, do_route=True):
    import os
    CUT = int(os.environ.get("TOKEN_CUT", "0"))

    def cut(k, keys):
        if CUT == k:
            S.finish(keys)
            raise _Cut()

    NT = T // 128
    NG = T // 512

    def sb(name, shape, dt):
        return es.enter_context(nc.sbuf_tensor(name, shape, dt))

    P = [es.enter_context(nc.psum_tensor("tP%d" % i, [128, 1024], F32)) for i in range(4)]

    ident = sb("t_ident", [128, 128], F32)
    ones1 = sb("t_ones1", [1, 128], F32)
    wout = sb("t_wout", [128, 8, 1024], BF16)
    lnp = sb("t_lnp", [128, 2, 1024], F32)
    stg0 = sb("t_stg0", [128, 2048], F32)
    stg = None
    wr = sb("t_wr", [128, 8, 36], F32)
    br = sb("t_br", [1, 36], F32)
    ycF = sb("t_ycT", [128, max(8 * T, 16384)], BF16)
    ycT = ycF[:, 0:8 * T].rearrange("p (a b) -> p a b", a=8)
    h1T = sb("t_h1T", [128, 8, T], BF16)
    acc = sb("t_acc", [128, NT, 1024], F32)
    xb = [sb("t_xb%d" % i, [128, 1024], F32) for i in range(2)]
    hb1 = sb("t_hb", [128, 1024], F32)
    hb = [hb1, hb1]
    h1b1 = sb("t_h1b", [128, 1024], F32)
    h1b = [h1b1, h1b1]
    hTf = sb("t_hTf", [128, 8, 128], F32)
    st6 = sb("t_st6", [128, 2, 6], F32)
    mv = sb("t_mv", [128, 2], F32)
    sm = sb("t_sm", [128, 8], F32)
    rlog = sb("t_rlog", [128, NT, 36], F32)
    gates = sb("t_gates", [128, NT, 32], F32)
    rt = sb("t_rt", [128, 12, NT, 8], F32)
    w1b = [ycF[:, (0 + i) * 2048:(1 + i) * 2048].rearrange("p (a b) -> p a b", a=8) for i in range(2)]
    w3b = [ycF[:, (2 + i) * 2048:(3 + i) * 2048].rearrange("p (a b) -> p a b", a=8) for i in range(2)]
    w2b = [ycF[:, (4 + i) * 2048:(5 + i) * 2048].rearrange("p (a b) -> p a b", a=2) for i in range(2)]
    ssb = [sb("t_ssb%d" % i, [128, 512], F32) for i in range(2)]
    hidT = [sb("t_hidT%d" % i, [128, 2, 512], BF16) for i in range(2)]
    ob = xb

    S.dma("sp", ident[:], dr["ident"], writes=["ident"])
    S.op("pool", lambda e: e.memset(ones1[:], 1.0), writes=["ones1"])
    for i in range(4):
        S.dma("sp", stg0[:].rearrange("p (a b) -> p a b", a=2),
              dr["w_out"].rearrange("(kc p) n -> p kc n", p=128)[:, 2 * i:2 * i + 2, :], writes=["stg0"])
        S.op("pool", lambda e, i=i: e.tensor_copy(wout[:, 2 * i, :], stg0[:, 0:1024]), reads=["stg0"], writes=[("wout", 2 * i)])
        S.op("dve", lambda e, i=i: e.tensor_copy(wout[:, 2 * i + 1, :], stg0[:, 1024:2048]), reads=["stg0"], writes=[("wout", 2 * i + 1)])
    for i, nm in enumerate(("ln1_g", "ln1_b")):
        S.dma("sp", lnp[:, i, :], dr[nm].partition_broadcast(128), writes=["lnp"])
    S.dma("sp", wr[:], dr["w_route"].rearrange("(kc p) n -> p kc n", p=128), writes=["wr"])
    S.dma("sp", br[:], dr["b_route"].unsqueeze(0), writes=["br"])
    def load_ycT(q):
        if "ycat_q" in dr:
            S.dma("sp", ycT[:, :, q * 512:(q + 1) * 512], dr["ycat_q"][q].rearrange("(kc p) t -> p kc t", p=128),
                  reads=[("rs_out", q)], writes=[("ycT", q)])
            return
        S.dma("sp", ycT[:, :, q * 512:(q + 1) * 512],
              dr["ycatT"].rearrange("(kc p) t -> p kc t", p=128)[:, :, q * 512:(q + 1) * 512],
              reads=dr.get("ycat_keys", []), writes=[("ycT", q)])

    stg = [stg0[:], ycF[:, 12288:16384].bitcast(F32)]
    pc = [0]

    def load_piece(dst, src, a, dkey, extra):
        k = pc[0] % 2
        pc[0] += 1
        S.dma("sp", stg[k].rearrange("p (a b) -> p a b", a=a), src, writes=["stg%d" % k] + (extra if k == 1 else []))
        S.op("pool", lambda e, k=k: e.tensor_copy(dst, stg[k].rearrange("p (a b) -> p a b", a=a)),
             reads=["stg%d" % k], writes=[dkey] + extra)

    def load_expert(e):
        s = e % 2
        yk = [("ycT", q) for q in range(NG)] if e < 2 else []
        load_piece(w1b[s], dr["w1"][e].rearrange("(kc p) f -> p kc f", p=128), 8, ("w1b", s), yk)
        load_piece(w3b[s], dr["w3"][e].rearrange("(kc p) f -> p kc f", p=128), 8, ("w3b", s), yk)
        load_piece(w2b[s], dr["w2"][e].rearrange("(fc p) d -> p fc d", p=128), 2, ("w2b", s), yk)

    def layer_norm(src_ap, srck, gi, dst_ap, dstk, tmp_ap, tmpk, eps=LN_EPS):
        for h in range(2):
            S.op("dve", lambda e, h=h: e.bn_stats(st6[:, h, :], src_ap[:, h * 512:(h + 1) * 512]),
                 reads=[srck], writes=["st6"])
        S.op("dve", lambda e: e.bn_aggr(mv[:], st6[:].rearrange("p a b -> p (a b)")), reads=["st6"], writes=["mv"])
        S.op("dve", lambda e: e.tensor_scalar(sm[:, 0:1], mv[:, 1:2], eps, None, ALU.add),
             reads=["mv"], writes=["sm0"])
        S.op("act", lambda e: e.activation(sm[:, 1:2], sm[:, 0:1], AF.Sqrt), reads=["sm0"], writes=["sm1"])
        S.op("dve", lambda e: e.reciprocal(sm[:, 2:3], sm[:, 1:2]), reads=["sm1"], writes=["sm2"])
        S.op("dve", lambda e: e.tensor_scalar(sm[:, 3:4], mv[:, 0:1], sm[:, 2:3], -1.0, ALU.mult, ALU.mult),
             reads=["mv", "sm2"], writes=["sm3"])
        S.op("act", lambda e: e.activation(tmp_ap, src_ap, AF.Identity, bias=sm[:, 3:4], scale=sm[:, 2:3]),
             reads=[srck, "sm2", "sm3"], writes=[tmpk])
        S.op("dve", lambda e: e.tensor_tensor(tmp_ap, tmp_ap, lnp[:, gi, :], ALU.mult),
             reads=[tmpk, "lnp"], writes=[tmpk])
        S.op("pool", lambda e: e.tensor_tensor(dst_ap, tmp_ap, lnp[:, gi + 1, :], ALU.add),
             reads=[tmpk, "lnp"], writes=[dstk])

    cut(1, ['ident', 'wout', 'lnp', 'wr', 'br', ('ycT', 0), ('ycT', NG - 1)])
    def t1_outproj(t):
        s = t % 2
        if t % 4 == 0:
            load_ycT(t // 4)
        S.dma("sp", xb[s][:], dr["x_tok"][t * 128:(t + 1) * 128, :], writes=[("xbt", s)])
        for half in range(2):
            for kc in range(8):
                S.op("pe", lambda e, half=half, kc=kc: e.matmul(
                    P[s][:, half * 512:(half + 1) * 512], lhsT=ycT[:, kc, t * 128:(t + 1) * 128],
                    rhs=wout[:, kc, half * 512:(half + 1) * 512], start=(kc == 0), stop=(kc == 7)),
                    reads=[("ycT", t // 4), ("wout", kc)], writes=[("P", s)], inc=(kc == 7 and half == 1))

    def t1_norm(t):
        s = t % 2
        hbt = (hb1, h1b1)[s]
        hk = ("hb", s)
        S.op("dve", lambda e: e.scalar_tensor_tensor(hbt[:], xb[s][:], ALPHA, P[s][:], ALU.mult, ALU.add),
             reads=[("xbt", s), ("P", s)], writes=[hk])
        layer_norm(hbt[:], hk, 0, acc[:, t, :], ("acc", t), hbt[:], hk)

    def t1_route(t):
        for j in range(8):
            S.op("pe", lambda e, j=j: e.transpose(P[2][:, j * 128:(j + 1) * 128], acc[:, t, j * 128:(j + 1) * 128], ident[:]),
                 reads=[("acc", t), "ident"], writes=[("PA", 0), ("PB", 0)], inc=(j == 7))
        S.op("act", lambda e: e.activation(h1T[:, :, t * 128:(t + 1) * 128],
                                           P[2][:].rearrange("p (a b) -> p a b", a=8), AF.Copy),
             reads=[("PA", 0), ("PB", 0)], writes=[("h1T", t // 4)])
        S.op("dve", lambda e: e.tensor_copy(hTf[:], P[2][:].rearrange("p (a b) -> p a b", a=8)),
             reads=[("PA", 0), ("PB", 0)], writes=["hTf"])
        for kc in range(8):
            S.op("pe", lambda e, kc=kc: e.matmul(P[3][:, 0:36], lhsT=hTf[:, kc, :], rhs=wr[:, kc, :],
                                                 start=(kc == 0), stop=False),
                 reads=["hTf", "wr"], writes=[("PA", 1), ("PB", 1)], inc=False)
        S.op("pe", lambda e: e.matmul(P[3][:, 0:36], lhsT=ones1[:], rhs=br[:], start=False, stop=True),
             reads=["ones1", "br"], writes=[("PA", 1), ("PB", 1)])
        S.op("act", lambda e: e.activation(rlog[:, t, :], P[3][:, 0:36], AF.Copy),
             reads=[("PA", 1), ("PB", 1)], writes=["rlog"])

    t1_outproj(0)
    if NT > 1:
        t1_outproj(1)
    t1_norm(0)
    for t in range(NT):
        if t + 1 < NT:
            t1_norm(t + 1)
        if t + 2 < NT:
            t1_outproj(t + 2)
        t1_route(t)
    if not do_route:
        S.op('dve', lambda e: e.memset(gates[:], 0.03), writes=['gates'])
    gl = rlog[:, :, 0:4]
    _op = S.op
    if not do_route:
        S.op = lambda *a, **k: None
    R = lambda i, w=8: rt[:, i, :, 0:w]
    R1 = lambda i: rt[:, i, :, 0]
    bc = lambda ap, w: ap.unsqueeze(2).to_broadcast([128, NT, w])

    def dv(fn, reads, writes):
        S.op("dve", fn, reads=reads, writes=writes)
    dv(lambda e: e.tensor_reduce(R1(0), gl, AX.X, ALU.max), ["rlog"], ["r0"])
    dv(lambda e: e.tensor_tensor(R(1, 4), gl, bc(R1(0), 4), ALU.is_equal), ["rlog", "r0"], ["r1"])
    dv(lambda e: e.tensor_tensor(R(2, 4), gl, bc(R1(0), 4), ALU.subtract), ["rlog", "r0"], ["r2"])
    S.op("act", lambda e: e.activation(R(2, 4), R(2, 4), AF.Exp), reads=["r2"], writes=["r2"])
    dv(lambda e: e.tensor_reduce(R1(3), R(2, 4), AX.X, ALU.add), ["r2"], ["r3"])
    for g in range(4):
        oh = rt[:, 1, :, g]
        el = rlog[:, :, 4 + g * 8:12 + g * 8]
        if g == 0:
            dv(lambda e, oh=oh, el=el: e.tensor_tensor(R(4), el, bc(oh, 8), ALU.mult), ["rlog", "r1"], ["r4"])
        else:
            dv(lambda e, oh=oh, el=el: e.tensor_tensor(R(5), el, bc(oh, 8), ALU.mult), ["rlog", "r1"], ["r5"])
            dv(lambda e: e.tensor_tensor(R(4), R(4), R(5), ALU.add), ["r4", "r5"], ["r4"])
    dv(lambda e: e.tensor_reduce(R1(6), R(4), AX.X, ALU.max), ["r4"], ["r6"])
    dv(lambda e: e.tensor_tensor(R(7), R(4), bc(R1(6), 8), ALU.is_equal), ["r4", "r6"], ["r7"])
    dv(lambda e: e.scalar_tensor_tensor(R(8), R(7), -1e30, R(4), ALU.mult, ALU.add), ["r7", "r4"], ["r8"])
    dv(lambda e: e.tensor_reduce(R1(9), R(8), AX.X, ALU.max), ["r8"], ["r9"])
    dv(lambda e: e.tensor_tensor(R(5), R(4), bc(R1(6), 8), ALU.subtract), ["r4", "r6"], ["r5"])
    S.op("act", lambda e: e.activation(R(5), R(5), AF.Exp), reads=["r5"], writes=["r5"])
    dv(lambda e: e.tensor_tensor(R(7), R(4), bc(R1(9), 8), ALU.is_ge), ["r4", "r9"], ["r7"])
    dv(lambda e: e.tensor_tensor(R(5), R(5), R(7), ALU.mult), ["r5", "r7"], ["r5"])
    dv(lambda e: e.tensor_reduce(R1(10), R(5), AX.X, ALU.add), ["r5"], ["r10"])
    dv(lambda e: e.tensor_tensor(R1(10), R1(10), R1(3), ALU.mult), ["r10", "r3"], ["r10"])
    dv(lambda e: e.tensor_scalar(R1(10), R1(10), ALPHA, None, ALU.mult), ["r10"], ["r10"])
    dv(lambda e: e.reciprocal(R1(11), R1(10)), ["r10"], ["r11"])
    dv(lambda e: e.tensor_tensor(R(5), R(5), bc(R1(11), 8), ALU.mult), ["r5", "r11"], ["r5"])
    for g in range(4):
        oh = rt[:, 1, :, g]
        dv(lambda e, oh=oh, g=g: e.tensor_tensor(gates[:, :, g * 8:(g + 1) * 8], R(5), bc(oh, 8), ALU.mult),
           ["r5", "r1"], ["gates"])

    S.op = _op
    if NE > 0:
        load_expert(0)
    if NE > 1:
        load_expert(1)
    PA = [P[2][:, 0:512], P[3][:, 0:512]]
    PB = [P[2][:, 512:1024], P[3][:, 512:1024]]
    it = 0
    pend = None
    for ex in range(NE):
        s = ex % 2
        for tg in range(NG):
            hs = (ex * NG + tg) % 2
            for fc in range(2):
                u = it % 2
                it += 1
                for kc in range(8):
                    S.op("pe", lambda e, kc=kc, fc=fc, tg=tg, s=s, u=u: e.matmul(
                        PA[u], lhsT=w1b[s][:, kc, fc * 128:(fc + 1) * 128], rhs=h1T[:, kc, tg * 512:(tg + 1) * 512],
                        start=(kc == 0), stop=(kc == 7)),
                        reads=[("w1b", s), ("h1T", tg)], writes=[("PA", u)], inc=(kc == 7))
                for kc in range(8):
                    S.op("pe", lambda e, kc=kc, fc=fc, tg=tg, s=s, u=u: e.matmul(
                        PB[u], lhsT=w3b[s][:, kc, fc * 128:(fc + 1) * 128], rhs=h1T[:, kc, tg * 512:(tg + 1) * 512],
                        start=(kc == 0), stop=(kc == 7)),
                        reads=[("w3b", s), ("h1T", tg)], writes=[("PB", u)], inc=(kc == 7))
                S.op("act", lambda e, u=u: e.activation(ssb[u][:], PA[u], AF.Silu),
                     reads=[("PA", u)], writes=[("ssb", u)])
                S.op("dve", lambda e, u=u, hs=hs, fc=fc: e.tensor_tensor(hidT[hs][:, fc, :], PB[u], ssb[u][:], ALU.mult),
                     reads=[("PB", u), ("ssb", u)], writes=[("hidT", hs)])
                if pend is not None:
                    pend(fc)

            def second(part, ex=ex, tg=tg, s=s, hs=hs):
                for tt in range(2 * part, 2 * part + 2):
                    t = tg * 4 + tt
                    o = t % 2
                    for half in range(2):
                        for fc in range(2):
                            S.op("pe", lambda e, half=half, fc=fc, tt=tt, o=o: e.matmul(
                                P[o][:, half * 512:(half + 1) * 512], lhsT=hidT[hs][:, fc, tt * 128:(tt + 1) * 128],
                                rhs=w2b[s][:, fc, half * 512:(half + 1) * 512], start=(fc == 0), stop=(fc == 1)),
                                reads=[("hidT", hs), ("w2b", s)], writes=[("P", o)], inc=(half == 1 and fc == 1))
                    S.op("dve", lambda e, t=t, o=o: e.scalar_tensor_tensor(
                        acc[:, t, :], P[o][:], gates[:, t, ex:ex + 1], acc[:, t, :], ALU.mult, ALU.add),
                        reads=[("P", o), "gates", ("acc", t)], writes=[("acc", t)])
                if part == 1 and tg == NG - 1 and ex + 2 < NE:
                    load_expert(ex + 2)
            pend = second
    if pend is not None:
        pend(0)
        pend(1)
    for i, nm in enumerate(("ln2_g", "ln2_b")):
        S.dma("sp", lnp[:, i, :], dr[nm].partition_broadcast(128), writes=["lnp"])
    for t in range(NT):
        s = t % 2
        layer_norm(acc[:, t, :], ("acc", t), 0, ob[s][:], ("xbt", s), acc[:, t, :], ("acc", t), eps=LN_EPS / (ALPHA * ALPHA))
        S.dma("sp", dr["out"][t * 128:(t + 1) * 128, :], ob[s][:], reads=[("xbt", s)], writes=[("out", t)])
    S.finish([("out", t) for t in range(NT)])


def build_token_nc(T=2048, NE=32, do_route=True):
    nc = bass.Bass("TRN2", target_bir_lowering=False)
    dr = {}

    def inp(name, shape, dt=F32):
        dr[name] = nc.dram_tensor(name, list(shape), dt, kind="ExternalInput").ap()
    inp("ycatT", [1024, T], BF16)
    inp("x_tok", [T, 1024])
    inp("w_out", [1024, 1024])
    for nm in ("ln1_g", "ln1_b", "ln2_g", "ln2_b"):
        inp(nm, [1024])
    inp("w_route", [1024, 36])
    inp("b_route", [36])
    inp("w1", [32, 1024, 256])
    inp("w3", [32, 1024, 256])
    inp("w2", [32, 256, 1024])
    inp("ident", [128, 128])
    dr["out"] = nc.dram_tensor("out", [T, 1024], F32, kind="ExternalOutput").ap()
    with ExitStack() as es:
        S = Sched(nc, es)
        try:
            token_phase(S, nc, es, T, dr, NE, do_route)
        except _Cut:
            pass
        S.replay()
    return nc


NCOL = 928
G_R, G_K, G_V, G_L1, G_GD, G_Q, G_MK, G_MV = 0, 128, 256, 384, 448, 544, 672, 800
MNEG = -30000.0


def mixer_phase(S, nc, es, Tm, dr, do_rwkv=True, do_moba=True, fused=False):
    NGm = Tm // 512
    NKT = Tm // 128

    def sb(name, shape, dt):
        return es.enter_context(nc.sbuf_tensor(name, shape, dt))

    def ps(name, shape, dt=F32):
        return es.enter_context(nc.psum_tensor(name, shape, dt))

    PI = [ps("mPI%d" % i, [128, 512]) for i in range(2)]
    PS = [ps("mPS%d" % i, [128, 512]) for i in range(2)]
    PO = ps("mPO", [128, 512])
    PM = ps("mPM", [128, 512])
    PR = [ps("mPR%d" % i, [128, 512]) for i in range(2)]

    ident = sb("m_ident", [128, 128], F32)
    identb = sb("m_identb", [128, 128], BF16)
    wb = sb("m_wb", [128, 8, NCOL], BF16)
    xs0 = sb("m_xs", [128, 4, 512], F32)
    xs = [xs0, xs0]
    xb0 = sb("m_xb", [128, 8, 512], BF16)
    xb = [xb0, xb0]
    QT = [sb("m_QT%d" % h, [98, 512], BF16) for h in range(2)]
    KT = [sb("m_KT%d" % h, [98, Tm], BF16) for h in range(2)]
    VA = sb("m_VA", [128, NKT, 2, 65], BF16)
    qf = [sb("m_qf%d" % h, [64, 512], F32) for h in range(2)]
    kmean = [sb("m_km%d" % h, [64, 32], F32) for h in range(2)]
    biasT = sb("m_biasT", [128, 2, 68], F32)
    cm = sb("m_cm", [128, 4, 512], BF16)
    est = xs0[:].rearrange("p a b -> p (a b)")[:, 0:2048]
    wst = xs0[:].rearrange("p a b -> p (a b)")[:, 0:NCOL]
    PT = [sb("m_PT%d" % i, [128, 512], BF16) for i in range(2)]
    gsel = sb("m_gsel", [128, 4, 32], F32)
    top8 = sb("m_top8", [128, 4, 8], F32)
    mbp = sb("m_mbp", [128, 4, 96], F32)
    osb = sb("m_osb", [65, 512], F32)
    rcp = sb("m_rcp", [65, 512], F32)
    ones65 = sb("m_ones65", [65, 64], F32)
    ybs = [sb("m_yb%d" % h, [64, 512], BF16) for h in range(2 if fused else 1)]
    if fused:
        Esel = sb("m_Esel", [64, 4, 1024], BF16)
        pstg = [sb("m_pstg%d" % i, [128, 512], BF16) for i in range(2)]

    S.dma("sp", ident[:], dr["ident"], writes=["ident"])
    S.op("dve", lambda e: e.tensor_copy(identb[:], ident[:]), reads=["ident"], writes=["identb"])
    S.dma("sp", biasT[:], dr["biasT"], writes=["biasT"])
    S.op("dve", lambda e: e.memset(ones65[:], 1.0), writes=["ones65"])
    S.op("dve", lambda e: e.memset(mbp[:], 0.0), writes=[("mbp", i) for i in range(4)])
    S.op("pool", lambda e: e.memset(VA[:], 1.0), writes=["VA"])
    for h in range(2):
        S.op("dve", lambda e, h=h: e.memset(kmean[h][:], 0.0), writes=[("kmean", h)])
    wst2 = [xs0[:].rearrange("p a b -> p (a b)")[:, 0:NCOL], xs0[:].rearrange("p a b -> p (a b)")[:, 1024:1024 + NCOL]]
    for kc in range(8):
        S.dma("sp", wst2[kc % 2], dr["w_sel"][kc * 128:(kc + 1) * 128, :], writes=[("wst", kc % 2)])
        S.op("pool" if kc % 2 == 0 else "dve", lambda e, kc=kc: e.tensor_copy(wb[:, kc, :], wst2[kc % 2]),
             reads=[("wst", kc % 2)], writes=[("wb", kc)])

    rw = rwkv_setup(S, nc, es, dr, sb) if do_rwkv else None

    pic = [0]
    NY = [None]
    pending_place = [None]

    def inproj_group(g, col0, M, consume):
        s = g % 2
        u = pic[0] % 2
        pic[0] += 1
        for kc in range(8):
            S.op("pe", lambda e, kc=kc, u=u, s=s: e.matmul(PI[u][0:M, :], lhsT=wb[:, kc, col0:col0 + M], rhs=xb[s][:, kc, :],
                                                          start=(kc == 0), stop=(kc == 7)),
                 reads=[("wb", kc), "xb"], writes=[("PI", u)], inc=(kc == 7))
        consume(PI[u], ("PI", u))

    def load_x(gg):
        for half in range(2):
            S.dma("sp", xs0[:], dr["xT"].rearrange("(kc p) t -> p kc t", p=128)[:, 4 * half:4 * half + 4, gg * 512:(gg + 1) * 512],
                  writes=["xs", ("wst", 0), ("wst", 1)])
            S.op("pool", lambda e, half=half: e.tensor_copy(xb0[:, 4 * half:4 * half + 4, :], xs0[:]),
                 reads=["xs"], writes=["xb"])
    load_x(0)
    S.dma("sp", cm[:], dr["cmask"].rearrange("j p t -> p j t"), writes=["cm"])
    for h in range(2):
        S.dma("sp", KT[h][64:98, :], dr["epat"][h], writes=[("KTe", h)])
        S.dma("sp", QT[h][96:98, :], dr["qpos"][:, 0:512], writes=[("QTp", h)])
    if fused:
        S.dma("sp", Esel[:], dr["esel"], writes=["Esel"])
    for g in range(NGm):
        s = g % 2
        tsl = slice(g * 512, (g + 1) * 512)
        if do_moba:
            for h in range(2):
                def cq(P, pk, h=h):
                    S.op("act", lambda e: e.activation(QT[h][0:64, :], P[0:64, :], AF.Copy), reads=[pk], writes=[("QTq", h)])
                    S.op("dve", lambda e: e.tensor_copy(qf[h][:], P[0:64, :]), reads=[pk], writes=[("qf", h)])
                inproj_group(g, G_Q + 64 * h, 64, cq)

                def ck(P, pk, h=h):
                    S.op("act", lambda e: e.activation(KT[h][0:64, tsl], P[0:64, :], AF.Copy), reads=[pk], writes=[("KTk", h)])
                    S.op("dve", lambda e: e.tensor_reduce(kmean[h][:, 2 * g:2 * g + 2], P[0:64, :].rearrange("p (a b) -> p a b", a=2), AX.X, ALU.add),
                         reads=[pk], writes=[("kmean", h)])
                inproj_group(g, G_MK + 64 * h, 64, ck)
            for tt in range(4):
                u = pic[0] % 2
                pic[0] += 1
                kt = g * 4 + tt
                for kc in range(8):
                    S.op("pe", lambda e, kc=kc, u=u, s=s, tt=tt: e.matmul(PI[u][:, 0:128], lhsT=xb[s][:, kc, tt * 128:(tt + 1) * 128],
                                                                         rhs=wb[:, kc, G_MV:G_MV + 128], start=(kc == 0), stop=(kc == 7)),
                         reads=[("wb", kc), "xb"], writes=[("PI", u)], inc=(kc == 7))
                S.op("act", lambda e, u=u, kt=kt: e.activation(VA[:, kt, :, 0:64], PI[u][:, 0:128].rearrange("p (a b) -> p a b", a=2), AF.Copy),
                     reads=[("PI", u)], writes=["VA"])
        adv = lambda: None
        gen = None
        if do_rwkv:
            rwkv_inproj(S, nc, rw, g, inproj_group, dr, ident)
            if NY[0] is None:
                class _Null:
                    op = staticmethod(lambda *a, **k: None)
                    dma = staticmethod(lambda *a, **k: None)
                NY[0] = sum(1 for _ in rwkv_compute(_Null, nc, rw, g, dr, PR, PI[0], ("PI", 0), ident))
            gen = rwkv_compute(S, nc, rw, g, dr, PR, PI[0], ("PI", 0), ident)
            n_it = 2 * (4 * g + 4) + 8
            per = -(-NY[0] // n_it)

            def adv(gen=gen, per=per):
                for _ in range(per):
                    try:
                        next(gen)
                    except StopIteration:
                        return
        if pending_place[0] is not None:
            pending_place[0]()
            pending_place[0] = None
        pf = []
        if g + 1 < NGm:
            def x_dma(half, gg=g + 1):
                S.dma("sp", xs0[:], dr["xT"].rearrange("(kc p) t -> p kc t", p=128)[:, 4 * half:4 * half + 4, gg * 512:(gg + 1) * 512],
                      writes=["xs"])

            def x_cast(half):
                S.op("act", lambda e: e.activation(xb0[:, 4 * half:4 * half + 4, :], xs0[:], AF.Copy), reads=["xs"], writes=["xb"])
            x_dma(0)
            pf = [lambda: (x_cast(0), x_dma(1)), lambda: x_cast(1)]
        if not do_moba:
            for _ in gen:
                pass
            while pf:
                pf.pop(0)()
            continue
        for h in range(2):
            for cq4 in range(4):
                blk = (g * 4 + cq4) // 2
                if blk > 0:
                    S.op("pe", lambda e, h=h, cq4=cq4, blk=blk: e.matmul(PM[:, cq4 * 32:cq4 * 32 + blk], lhsT=qf[h][:, cq4 * 128:(cq4 + 1) * 128],
                                                                       rhs=kmean[h][:, 0:blk], start=True, stop=True),
                         reads=[("qf", h), ("kmean", h)], writes=[("PM", 0)])
            adv()
            for cq4 in range(4):
                blk = (g * 4 + cq4) // 2
                mk, gk, tk = ("mbp", cq4), ("gsel", cq4), ("top8", cq4)
                mb_, gs_, t8_ = mbp[:, cq4, :], gsel[:, cq4, :], top8[:, cq4, :]
                S.op("dve", lambda e, mb_=mb_: e.memset(mb_[:, 64:96], MNEG), writes=[mk])
                if blk > 3:
                    S.op("dve", lambda e, gs_=gs_: e.memset(gs_, -1e30), writes=[gk])
                    S.op("dve", lambda e, gs_=gs_, cq4=cq4, blk=blk: e.tensor_copy(gs_[:, 0:blk], PM[:, cq4 * 32:cq4 * 32 + blk]),
                         reads=[("PM", 0)], writes=[gk])
                    S.op("dve", lambda e, gs_=gs_, t8_=t8_: e.max(t8_, gs_), reads=[gk], writes=[tk])
                    S.op("dve", lambda e, mb_=mb_, gs_=gs_, t8_=t8_, blk=blk: e.tensor_scalar(mb_[:, 64:64 + blk], gs_[:, 0:blk], t8_[:, 2:3], -MNEG,
                                                                                   ALU.is_ge, ALU.mult),
                         reads=[gk, tk], writes=[mk])
                    S.op("dve", lambda e, mb_=mb_, blk=blk: e.tensor_scalar(mb_[:, 64:64 + blk], mb_[:, 64:64 + blk], MNEG, None, ALU.add),
                         reads=[mk], writes=[mk])
                elif blk > 0:
                    S.op("dve", lambda e, mb_=mb_, blk=blk: e.memset(mb_[:, 64:64 + blk], 0.0), writes=[mk])
                S.op("dve", lambda e, mb_=mb_, blk=blk: e.memset(mb_[:, 64 + blk:65 + blk], 0.0), writes=[mk])
                adv()
            for cq4 in range(4):
                S.op("pe", lambda e, cq4=cq4: e.matmul(PO[0:96, cq4 * 128:(cq4 + 1) * 128],
                                                       lhsT=mbp[:, cq4, :], rhs=ident[:], start=True, stop=True),
                     reads=[("mbp", cq4), "ident"], writes=[("PO", 0)])
            S.op("act", lambda e, h=h: e.activation(QT[h][64:96, :], PO[64:96, :], AF.Copy), reads=[("PO", 0)], writes=[("QTm", h)])
            if pf:
                pf.pop(0)()
            nkt = 4 * g + 4

            def emit_st(kt, h=h):
                u = kt % 2
                dl = kt - 4 * g
                S.op("pe", lambda e: e.matmul(PS[u][:], lhsT=KT[h][0:98, kt * 128:(kt + 1) * 128], rhs=QT[h][0:98, :],
                                              start=True, stop=(dl < 0)),
                     reads=[("KTk", h), ("KTe", h), ("QTq", h), ("QTm", h), ("QTp", h)], writes=[("PS", u)], inc=(dl < 0))
                if dl >= 0:
                    S.op("pe", lambda e: e.matmul(PS[u][:], lhsT=identb[:], rhs=cm[:, dl, :], start=False, stop=True),
                         reads=["identb", "cm"], writes=[("PS", u)])
            emit_st(0)
            for kt in range(nkt):
                u = kt % 2
                dl = kt - 4 * g
                if kt + 1 < nkt:
                    emit_st(kt + 1)
                S.op("act", lambda e, h=h, u=u, dl=dl: e.activation(PT[u][:], PS[u][:], AF.Exp, bias=biasT[:, h, dl + 64:dl + 65], scale=0.125),
                     reads=[("PS", u), "biasT"], writes=[("PT", u)])
                adv()
                S.op("pe", lambda e, h=h, kt=kt, u=u, nkt=nkt: e.matmul(PO[0:65, :], lhsT=VA[:, kt, h, :], rhs=PT[u][:],
                                                                       start=(kt == 0), stop=(kt == nkt - 1)),
                     reads=["VA", ("PT", u)], writes=[("PO", 0)])
            S.op("act", lambda e: e.activation(rcp[64:65, :], PO[64:65, :], AF.Ln), reads=[("PO", 0)], writes=["rcp"])
            S.op("act", lambda e: e.activation(rcp[64:65, :], rcp[64:65, :], AF.Exp, scale=-1.0), reads=["rcp"], writes=["rcp"])
            S.op("act", lambda e: e.activation(osb[0:64, :], PO[0:64, :], AF.Copy), reads=[("PO", 0)], writes=["osb"])
            adv()
            adv()
            S.op("pe", lambda e: e.matmul(PM[0:64, :], lhsT=ones65[64:65, :], rhs=rcp[64:65, :], start=True, stop=True),
                 reads=["ones65", "rcp"], writes=[("PM", 0)])
            yb = ybs[h if fused else 0]
            ybk = ("yb", h if fused else 0)
            S.op("dve", lambda e: e.tensor_tensor(yb[:], osb[0:64, :], PM[0:64, :], ALU.mult), reads=["osb", ("PM", 0)], writes=[ybk])
            if not fused:
                S.dma("sp", dr["ycT"][128 + 64 * h:192 + 64 * h, tsl], yb[:], reads=[ybk], writes=[("ycT_out", g, h)])
        if gen is not None:
            for _ in gen:
                pass
        while pf:
            pf.pop(0)()
        if fused:
            def place(g=g):
                seg = g // 4
                srcs = [(rw["ya", 0], ("r_ya", 0)), (rw["ya", 1], ("r_ya", 1)), (ybs[0], ("yb", 0)), (ybs[1], ("yb", 1))]
                for kc in range(8):
                    u = pic[0] % 2
                    pic[0] += 1
                    for si, (yt_, yk_) in enumerate(srcs):
                        S.op("pe", lambda e, si=si, yt_=yt_, u=u, kc=kc: e.matmul(PI[u][:, :], lhsT=Esel[:, si, kc * 128:(kc + 1) * 128], rhs=yt_[:],
                                                                                 start=(si == 0), stop=(si == 3)),
                             reads=["Esel", yk_], writes=[("PI", u)], inc=(si == 3))
                    st = pstg[kc % 2]
                    S.op("act" if kc % 2 == 0 else "dve",
                         (lambda e, st=st, u=u: e.activation(st[:], PI[u][:, :], AF.Copy)) if kc % 2 == 0 else
                         (lambda e, st=st, u=u: e.tensor_copy(st[:], PI[u][:, :])),
                         reads=[("PI", u)], writes=[("pstg", kc % 2)])
                    S.dma("sp", dr["rs_in"][g % 4][seg * 1024 + kc * 128:seg * 1024 + (kc + 1) * 128, :], st[:],
                          reads=[("pstg", kc % 2)], writes=[("rsin", g, kc)])
                if "on_group_done" in dr:
                    dr["on_group_done"](g)
            if g + 1 < NGm:
                pending_place[0] = place
            else:
                place()
    outs = []
    for g in range(NGm):
        if fused:
            outs += [("rsin", g, kc) for kc in range(8)]
            continue
        for h in range(2):
            if do_moba:
                outs.append(("ycT_out", g, h))
            if do_rwkv:
                outs.append(("ya_out", g, h))
    if not fused:
        S.finish(outs)
    return outs


def build_mixer_nc(Tm=8192, do_rwkv=True, do_moba=True):
    nc = bass.Bass("TRN2", target_bir_lowering=False)
    dr = {}

    def inp(name, shape, dt=F32):
        dr[name] = nc.dram_tensor(name, list(shape), dt, kind="ExternalInput").ap()
    inp("xT", [1024, Tm])
    inp("w_sel", [1024, NCOL])
    inp("ident", [128, 128])
    inp("biasT", [128, 2, 68])
    inp("cmask", [4, 128, 512], BF16)
    inp("epat", [2, 34, Tm], BF16)
    inp("qpos", [2, Tm], BF16)
    rwkv_inputs(inp)
    dr["ycT"] = nc.dram_tensor("ycT", [256, Tm], BF16, kind="ExternalOutput").ap()
    with ExitStack() as es:
        S = Sched(nc, es)
        mixer_phase(S, nc, es, Tm, dr, do_rwkv, do_moba)
        S.replay()
    return nc


def rwkv_inputs(inp):
    pass


def mixer_consts(hg, Tm):
    heads = [2 * hg, 2 * hg + 1]
    slopes = [2.0 ** (-(h + 1)) for h in heads]
    p = np.arange(128, dtype=np.float32)[:, None]
    dl = (np.arange(68, dtype=np.float32) - 64)[None, :]
    biasT = np.stack([sl * (dl * 128 + p) for sl in slopes], 1).astype(np.float32)
    k = np.arange(128)[:, None]
    q = np.arange(512)[None, :]
    cmask = np.stack([np.where(j * 128 + k <= q, 0.0, MNEG) for j in range(4)], 0).astype(np.float32)
    epat = np.zeros((2, 34, Tm), np.float32)
    for n in range(min(32, Tm // 256)):
        epat[:, n, n * 256:(n + 1) * 256] = 1.0
    for i, sl in enumerate(slopes):
        epat[i, 32, :] = -8.0 * sl * 64
        epat[i, 33, :] = -8.0 * sl
    t = np.arange(Tm) % 512
    qpos = np.stack([t // 64, t % 64], 0).astype(np.float32)
    bf = ml_dtypes.bfloat16
    return dict(biasT=biasT, cmask=cmask.astype(bf), epat=epat.astype(bf), qpos=qpos.astype(bf), ident=np.eye(128, dtype=np.float32))


def mixer_inputs(d, c, Tm=8192):
    b, hg = c // 4, c % 4
    w_in = d["w_in"]
    cols = []
    for base in (0, 512, 1024):
        cols.append(np.arange(base + hg * 128, base + hg * 128 + 128))
    cols.append(np.arange(1536, 1696))
    for base in (1696, 1696 + 512, 1696 + 1024):
        cols.append(np.arange(base + hg * 128, base + hg * 128 + 128))
    cols = np.concatenate(cols)
    m = dict(mixer_consts(hg, Tm))
    m["xT"] = np.ascontiguousarray(d["x"][b, :Tm, :].T)
    m["w_sel"] = np.ascontiguousarray(w_in[:, cols])
    return m, cols


LAM = 0.6065306597126334
GN_EPS = 64e-5


def rwkv_inputs(inp):
    inp("mu_cols", [128, 8])
    inp("pcols", [64, 2, 5])
    inp("wlu", [32, 128])
    inp("alu", [32, 128])
    inp("glu", [96, 128])
    inp("gnw", [128])
    inp("gnb", [128])
    inp("rmask", [64, 384])


def rwkv_host_inputs(d, c):
    b, hg = c // 4, c % 4
    sl = slice(hg * 128, hg * 128 + 128)
    mu = d["mu_shift"]
    mu_cols = np.zeros((128, 8), np.float32)
    for i, base in enumerate((0, 512, 1024)):
        for h in range(2):
            mu_cols[0:64, 2 * i + h] = mu[base + hg * 128 + h * 64: base + hg * 128 + h * 64 + 64]
    mu_cols[0:64, 6] = mu[1536:1600]
    mu_cols[0:96, 7] = mu[1600:1696]
    pcols = np.zeros((64, 2, 5), np.float32)
    for h in range(2):
        s2 = slice(hg * 128 + h * 64, hg * 128 + h * 64 + 64)
        pcols[:, h, 0] = d["w0"][s2]
        pcols[:, h, 1] = d["a0"][s2]
        pcols[:, h, 2] = d["k_k"][s2]
        pcols[:, h, 3] = d["k_a"][s2]
        pcols[:, h, 4] = d["r_k"].reshape(-1)[s2]
    s_ = np.arange(64)[:, None]
    t_ = np.arange(64)[None, :]
    Ms = (s_ < t_).astype(np.float32)
    Mi = (s_ <= t_).astype(np.float32)
    rmask = np.concatenate([Ms, Mi, Ms, Mi, Ms.T, Ms.T], 1).astype(np.float32)[:, :384]
    return dict(mu_cols=mu_cols, pcols=pcols, wlu=np.ascontiguousarray(d["w_lora_up"][:, sl]),
                alu=np.ascontiguousarray(d["a_lora_up"][:, sl]), glu=np.ascontiguousarray(d["g_lora_up"][:, sl]),
                gnw=np.ascontiguousarray(d["gn_w"][sl]), gnb=np.ascontiguousarray(d["gn_b"][sl]), rmask=rmask)


def rwkv_setup(S, nc, es, dr, sb):
    rw = {}
    for nm, shp in (("mu", [128, 8]), ("pc", [64, 2, 5]), ("omk", [64, 2]), ("wlu", [32, 128]), ("alu", [64, 128]), ("glu", [96, 128]),
                    ("gnw", [64, 128]), ("gnb", [64, 128]), ("rmask", [64, 384]), ("ones", [64, 64]),
                    ("l1m", [64, 512]), ("gdm", [96, 512]), ("tmp", [96, 512])):
        rw[nm] = sb("r_" + nm, shp, F32)
    rw["tw"] = rw["l1m"]
    rw["gs"] = rw["gdm"]
    rw["praw"] = sb("r_praw", [96, 513], F32)
    rw["last"] = sb("r_last", [96, 8], F32)
    for nm in ("sg", "asig", "kk", "kkn", "kmod", "E1", "E3", "rkr"):
        rw[nm, 0] = rw[nm, 1] = sb("r_%s" % nm, [64, 512], F32)
    rw["cum", 0] = rw["cum", 1] = rw["kk", 0]
    rw["E2", 0] = rw["E2", 1] = rw["sg", 0]
    for nm in ("AR", "BK", "BKh"):
        rw[nm, 0] = rw[nm, 1] = sb("r_%s" % nm, [64, 8, 128], F32)
    for h in range(2):
        for nm in ("rm", "km", "vm"):
            rw[nm, h] = sb("r_%s%d" % (nm, h), [64, 512], F32)
        rw["S", h] = sb("r_S%d" % h, [64, 64], F32)
        rw["ya", h] = sb("r_ya%d" % h, [64, 512], BF16)
    for nm, shp in (("AAm", [64, 4, 256]), ("Nt", [64, 4, 64]), ("NPg", [64, 2, 4, 128]), ("TK", [64, 4, 256]), ("TG", [64, 4, 65]),
                    ("Z", [64, 2, 4, 128]), ("Tt", [64, 2, 4, 64]), ("Afm", [64, 4, 64]), ("M", [64, 4, 64]), ("Sl", [64, 4, 64]), ("Rf", [64, 4, 64]),
                    ("y", [64, 4, 64]), ("yt", [64, 4, 64]), ("ysq", [64, 4, 64]), ("sst", [64, 6, 4])):
        rw[nm] = sb("r_" + nm, shp, F32)
    S.dma("sp", rw["mu"][:], dr["mu_cols"], writes=["r_mu"])
    S.dma("sp", rw["pc"][:], dr["pcols"], writes=["r_pc"])
    S.dma("sp", rw["wlu"][:], dr["wlu"], writes=["r_wlu"])
    S.dma("sp", rw["alu"][32:64, :], dr["alu"], writes=["r_alu"])
    S.dma("sp", rw["glu"][:], dr["glu"], writes=["r_glu"])
    S.dma("sp", rw["gnw"][:], dr["gnw"].partition_broadcast(64), writes=["r_gn"])
    S.dma("sp", rw["gnb"][:], dr["gnb"].partition_broadcast(64), writes=["r_gn"])
    S.dma("sp", rw["rmask"][:], dr["rmask"], writes=["r_rmask"])
    S.op("dve", lambda e: e.memset(rw["ones"][:], 1.0), writes=["r_ones"])
    S.op("dve", lambda e: e.tensor_scalar(rw["omk"][:], rw["pc"][:, :, 3], -1.0, 1.0, ALU.mult, ALU.add), reads=["r_pc"], writes=["r_omk"])
    S.op("pool", lambda e: e.memset(rw["last"][:], 0.0), writes=["r_last"])
    for h in range(2):
        S.op("pool", lambda e, h=h: e.memset(rw["S", h][:], 0.0), writes=[("r_S", h)])
    return rw


def rwkv_inproj(S, nc, rw, g, inproj_group, dr, ident):
    tsl = slice(g * 512, (g + 1) * 512)
    I64 = ident[0:64, 0:64]
    mu = rw["mu"]
    tmp = rw["tmp"]

    def shift_mix(gi, M, dst, dkey):
        praw = rw["praw"]
        last = rw["last"]

        def consume(P, pk):
            S.op("act", lambda e: e.activation(praw[0:M, 1:513], P[0:M, :], AF.Copy), reads=[pk], writes=["r_praw"])
            S.op("act", lambda e: e.activation(praw[0:M, 0:1], last[0:M, gi:gi + 1], AF.Copy), reads=["r_last"], writes=["r_praw"])
            S.op("dve", lambda e: e.tensor_tensor(tmp[0:M, :], praw[0:M, 0:512], praw[0:M, 1:513], ALU.subtract),
                 reads=["r_praw"], writes=["r_tmp"])
            S.op("dve", lambda e: e.scalar_tensor_tensor(dst[0:M, :], tmp[0:M, :], mu[0:M, gi:gi + 1], praw[0:M, 1:513], ALU.mult, ALU.add),
                 reads=["r_tmp", "r_mu", "r_praw"], writes=[dkey])
            S.op("act", lambda e: e.activation(last[0:M, gi:gi + 1], praw[0:M, 512:513], AF.Copy), reads=["r_praw"], writes=["r_last"])
        return consume
    for h in range(2):
        inproj_group(g, G_R + 64 * h, 64, shift_mix(0 + h, 64, rw["rm", h], ("r_rm", h)))
        inproj_group(g, G_K + 64 * h, 64, shift_mix(2 + h, 64, rw["km", h], ("r_km", h)))
        inproj_group(g, G_V + 64 * h, 64, shift_mix(4 + h, 64, rw["vm", h], ("r_vm", h)))
    inproj_group(g, G_L1, 64, shift_mix(6, 64, rw["l1m"], "r_l1m"))
    inproj_group(g, G_GD, 96, shift_mix(7, 96, rw["gdm"], "r_gdm"))


def rwkv_compute(S, nc, rw, g, dr, PR, PM, kPM, ident):
    tsl = slice(g * 512, (g + 1) * 512)
    I64 = ident[0:64, 0:64]
    tmp = rw["tmp"]
    S.op("act", lambda e: e.activation(rw["l1m"][0:32, :], rw["l1m"][0:32, :], AF.Tanh), reads=["r_l1m"], writes=["r_l1m"])
    yield
    S.op("act", lambda e: e.activation(rw["gdm"][:], rw["gdm"][:], AF.Sigmoid), reads=["r_gdm"], writes=["r_gdm"])
    yield
    pc = rw["pc"]
    for h in range(2):
        hs = slice(h * 64, h * 64 + 64)
        rm, km, vm, sg, asig, kk, kkn, kmod, cum, E1, E2, E3, rkr = [rw[n, h] for n in
                                                                      ("rm", "km", "vm", "sg", "asig", "kk", "kkn", "kmod", "cum", "E1", "E2", "E3", "rkr")]
        AR, BK, BKh, Sst, ya = rw["AR", h], rw["BK", h], rw["BKh", h], rw["S", h], rw["ya", h]
        P0, P1 = PR[0], PR[1]
        k0, k1 = ("PR", 0), ("PR", 1)
        S.op("pe", lambda e: e.matmul(P0[0:64, :], lhsT=rw["wlu"][:, hs], rhs=rw["l1m"][0:32, :], start=True, stop=True),
             reads=["r_wlu", "r_l1m"], writes=[k0])
        S.op("act", lambda e: e.activation(sg[:], P0[0:64, :], AF.Sigmoid, bias=pc[:, h, 0:1]), reads=[k0, "r_pc"], writes=[("r_sg", 0)])
        yield
        S.op("pe", lambda e: e.matmul(P1[0:64, :], lhsT=rw["alu"][32:64, hs], rhs=rw["l1m"][32:64, :], start=True, stop=True),
             reads=["r_alu", "r_l1m"], writes=[k1])
        S.op("act", lambda e: e.activation(asig[:], P1[0:64, :], AF.Sigmoid, bias=pc[:, h, 1:2]), reads=[k1, "r_pc"], writes=[("r_asig", 0)])
        yield
        S.op("dve", lambda e: e.tensor_scalar(kk[:], km[:], pc[:, h, 2:3], None, ALU.mult), reads=[("r_km", h), "r_pc"], writes=[("r_kk", 0)])
        yield
        S.op("dve", lambda e: e.tensor_tensor(tmp[0:64, :], kk[:], kk[:], ALU.mult), reads=[("r_kk", 0)], writes=["r_tmp"])
        yield
        S.op("pe", lambda e: e.matmul(P0[0:64, :], lhsT=rw["ones"][:], rhs=tmp[0:64, :], start=True, stop=True),
             reads=["r_ones", "r_tmp"], writes=[k0])
        S.op("dve", lambda e: e.tensor_scalar(tmp[0:64, :], P0[0:64, :], 1e-18, None, ALU.max), reads=[k0], writes=["r_tmp"])
        yield
        S.op("act", lambda e: e.activation(tmp[0:64, :], tmp[0:64, :], AF.Ln), reads=["r_tmp"], writes=["r_tmp"])
        yield
        S.op("act", lambda e: e.activation(tmp[0:64, :], tmp[0:64, :], AF.Exp, scale=-0.5), reads=["r_tmp"], writes=["r_tmp"])
        yield
        S.op("dve", lambda e: e.tensor_tensor(kkn[:], kk[:], tmp[0:64, :], ALU.mult), reads=[("r_kk", 0), "r_tmp"], writes=[("r_kkn", 0)])
        yield
        S.op("dve", lambda e: e.tensor_scalar(tmp[0:64, :], asig[:], pc[:, h, 3:4], rw["omk"][:, h:h + 1], ALU.mult, ALU.add),
             reads=[("r_asig", 0), "r_pc", "r_omk"], writes=["r_tmp"])
        yield
        S.op("dve", lambda e: e.tensor_tensor(kmod[:], km[:], tmp[0:64, :], ALU.mult), reads=[("r_km", h), "r_tmp"], writes=[("r_kmod", 0)])
        yield
        S.op("dve", lambda e: e.scalar_tensor_tensor(rkr[:], rm[:], pc[:, h, 4:5], kmod[:], ALU.mult, ALU.mult),
             reads=[("r_rm", h), ("r_kmod", 0), "r_pc"], writes=[("r_rkr", 0)])
        yield
        for c in range(8):
            cs = slice(c * 64, c * 64 + 64)
            S.op("dve", lambda e, cs=cs: e.tensor_tensor_scan(cum[:, cs], rw["ones"][:], sg[:, cs], 0.0, ALU.mult, ALU.add),
                 reads=[("r_sg", 0), "r_ones"], writes=[("r_kk", 0)])
            yield
        S.op("act", lambda e: e.activation(E1[:], cum[:], AF.Exp, scale=-LAM), reads=[("r_kk", 0)], writes=[("r_E1", 0)])
        yield
        S.op("dve", lambda e: e.tensor_tensor(tmp[0:64, :], cum[:], sg[:], ALU.subtract), reads=[("r_kk", 0), ("r_sg", 0)], writes=["r_tmp"])
        yield
        S.op("act", lambda e: e.activation(E3[:], tmp[0:64, :], AF.Exp, scale=-LAM), reads=["r_tmp"], writes=[("r_E3", 0)])
        yield
        S.op("act", lambda e: e.activation(E2[:], cum[:], AF.Exp, scale=LAM), reads=[("r_kk", 0)], writes=[("r_sg", 0)])
        yield
        v3 = lambda ap: ap.rearrange("p (c t) -> p c t", c=8)
        S.op("dve", lambda e: e.scalar_tensor_tensor(AR[:, :, 0:64], v3(kkn[:]), -1.0, v3(E3[:]), ALU.mult, ALU.mult),
             reads=[("r_kkn", 0), ("r_E3", 0)], writes=[("r_AR", 0)])
        yield
        S.op("dve", lambda e: e.tensor_tensor(AR[:, :, 64:128], v3(rm[:]), v3(E1[:]), ALU.mult),
             reads=[("r_rm", h), ("r_E1", 0)], writes=[("r_AR", 0)])
        yield
        S.op("dve", lambda e: e.tensor_tensor(tmp[0:64, :], kkn[:], asig[:], ALU.mult), reads=[("r_kkn", 0), ("r_asig", 0)], writes=["r_tmp"])
        yield
        S.op("dve", lambda e: e.tensor_tensor(BK[:, :, 0:64], v3(tmp[0:64, :]), v3(E2[:]), ALU.mult), reads=["r_tmp", ("r_sg", 0)], writes=[("r_BK", 0)])
        yield
        S.op("dve", lambda e: e.tensor_tensor(BK[:, :, 64:128], v3(kmod[:]), v3(E2[:]), ALU.mult), reads=[("r_kmod", 0), ("r_sg", 0)], writes=[("r_BK", 0)])
        yield
        S.op("dve", lambda e: e.tensor_tensor(BKh[:], BK[:], v3(E1[:])[:, :, 63:64].to_broadcast([64, 8, 128]), ALU.mult),
             reads=[("r_BK", 0), ("r_E1", 0)], writes=[("r_BKh", 0)])
        yield
        Tt = rw["Tt"]
        AAm, Nt, NPg, TK, TG, Z, Afm, Mm, Sl, Rf, y, yt, ysq, sst = [rw[n] for n in
                                                                       ("AAm", "Nt", "NPg", "TK", "TG", "Z", "Afm", "M", "Sl", "Rf", "y", "yt", "ysq", "sst")]
        rmask = rw["rmask"]
        B0, B1, B2 = PR[0], PR[1], PM
        kB0, kB1, kB2 = ("PR", 0), ("PR", 1), kPM
        NB = 4
        E1v = v3(E1[:])
        rd = [("r_AR", 0), ("r_BK", 0)]

        def mm(out, lhsT, rhs, reads, wk, inc, start=True, stop=True):
            S.op("pe", lambda e: e.matmul(out, lhsT=lhsT, rhs=rhs, start=start, stop=stop), reads=reads, writes=[wk], inc=inc)
        for hb in range(2):
            c0 = hb * NB
            banks = [(B0, kB0), (B0, kB0), (B1, kB1), (B1, kB1)]
            for j in range(NB):
                c = c0 + j
                Bj, kBj = banks[j]
                off = (j % 2) * 256
                mm(Bj[0:64, off:off + 128], BK[:, c, 0:64], AR[:, c, :], rd, kBj, False)
                mm(Bj[0:64, off + 128:off + 256], BK[:, c, 64:128], AR[:, c, :], rd, kBj, j % 2 == 1)
            for b2, (Bj, kBj) in enumerate(((B0, kB0), (B1, kB1))):
                S.op("dve", lambda e, b2=b2, Bj=Bj: e.tensor_tensor(AAm[:, 2 * b2:2 * b2 + 2, :], Bj[0:64, 0:512].rearrange("p (a b) -> p a b", a=2),
                                                                  rmask[:, 0:256].unsqueeze(1).to_broadcast([64, 2, 256]), ALU.mult),
                     reads=[kBj, "r_rmask"], writes=["r_AAm"])
                yield
            for j in range(NB):
                c = c0 + j
                mm(B2[0:64, j * 64:(j + 1) * 64], AR[:, c, 0:64], BK[:, c, 0:64], rd, kB2, j == NB - 1)
            S.op("dve", lambda e: e.tensor_tensor(Nt[:], B2[0:64, 0:256].rearrange("p (a b) -> p a b", a=NB),
                                                  rmask[:, 256:320].unsqueeze(1).to_broadcast([64, NB, 64]), ALU.mult),
                 reads=[kB2, "r_rmask"], writes=["r_Nt"])
            yield
            for j in range(NB):
                c = c0 + j
                cs = slice(c * 64, c * 64 + 64)
                Bj, kBj = banks[j]
                off = (j % 2) * 256
                mm(Bj[0:64, off:off + 64], vm[:, cs], I64, [("r_vm", h), "ident"], kBj, False)
                mm(Bj[0:64, off + 64:off + 128], AR[:, c, 0:64], I64, rd + ["ident"], kBj, False)
                mm(Bj[0:64, off + 128:off + 192], BKh[:, c, 0:64], I64, [("r_BKh", 0), "ident"], kBj, False)
                mm(Bj[0:64, off + 192:off + 256], BKh[:, c, 64:128], I64, [("r_BKh", 0), "ident"], kBj, j % 2 == 1)
            for b2, (Bj, kBj) in enumerate(((B0, kB0), (B1, kB1))):
                S.op("act", lambda e, b2=b2, Bj=Bj: e.activation(TK[:, 2 * b2:2 * b2 + 2, :], Bj[0:64, 0:512].rearrange("p (a b) -> p a b", a=2), AF.Copy),
                     reads=[kBj], writes=["r_TK"])
                yield
            for j in range(NB):
                c = c0 + j
                cs = slice(c * 64, c * 64 + 64)
                mm(B2[0:64, j * 65:j * 65 + 64], rw["gdm"][:, cs], rw["glu"][:, hs], ["r_gdm", "r_glu"], kB2, False)
                mm(B2[0:64, j * 65 + 64:j * 65 + 65], rkr[:, cs], rw["ones"][:, 0:1], [("r_rkr", 0), "r_ones"], kB2, j == NB - 1)
            S.op("act", lambda e: e.activation(TG[:], B2[0:64, 0:NB * 65].rearrange("p (a b) -> p a b", a=NB), AF.Copy), reads=[kB2], writes=["r_TG"])
            yield
            for j in range(NB):
                mm(B2[0:64, j * 64:(j + 1) * 64], AAm[:, j, 128:192], TK[:, j, 0:64], ["r_AAm", "r_TK"], kB2, j == NB - 1)
            S.op("dve", lambda e: e.tensor_copy(Z[:, 0, :, 64:128], B2[0:64, 0:256].rearrange("p (a b) -> p a b", a=NB)), reads=[kB2], writes=[("r_Z", 0)])
            yield
            S.op("dve", lambda e: e.tensor_copy(Z[:, 0, :, 0:64], TK[:, :, 64:128]), reads=["r_TK"], writes=[("r_Z", 0)])
            yield
            S.op("dve", lambda e: e.tensor_tensor(Tt[:, 1], I64.unsqueeze(1).to_broadcast([64, NB, 64]), AAm[:, :, 0:64], ALU.add),
                 reads=["ident", "r_AAm"], writes=[("r_T", 1)])
            yield
            for k in range(6):
                Nk = (lambda j: AAm[:, j, 0:64]) if k == 0 else (lambda j, k=k: NPg[:, k % 2, j, 0:64])
                Ntk = (lambda j: Nt[:, j, :]) if k == 0 else (lambda j, k=k: NPg[:, k % 2, j, 64:128])
                rdk = ["r_AAm", "r_Nt"] if k == 0 else [("r_NP", k % 2)]
                if k < 5:
                    for j in range(NB):
                        if k < 4:
                            mm(B1[0:64, j * 128:j * 128 + 64], Ntk(j), Nk(j), rdk, kB1, False)
                        mm(B1[0:64, j * 128 + 64:(j + 1) * 128], Nk(j), Ntk(j), rdk, kB1, j == NB - 1)
                    S.op("act", lambda e, k=k: e.activation(NPg[:, (k + 1) % 2], B1[0:64, 0:512].rearrange("p (a b) -> p a b", a=NB), AF.Copy),
                         reads=[kB1], writes=[("r_NP", (k + 1) % 2)])
                    yield
                if k >= 1:
                    ti, to = k % 2, (k + 1) % 2
                    for j in range(NB):
                        mm(B0[0:64, j * 64:(j + 1) * 64], Ntk(j), Tt[:, ti, j, :], rdk + [("r_T", ti)], kB0, j == NB - 1)
                    S.op("dve", lambda e, ti=ti, to=to: e.tensor_tensor(Tt[:, to], B0[0:64, 0:NB * 64].rearrange("p (a b) -> p a b", a=NB), Tt[:, ti], ALU.add),
                         reads=[kB0, ("r_T", ti)], writes=[("r_T", to)])
                    yield
            for j in range(NB):
                mm(B0[0:64, j * 128:(j + 1) * 128], Tt[:, 0, j, :], Z[:, 0, j, :], [("r_T", 0), ("r_Z", 0)], kB0, j == NB - 1)
            S.op("act", lambda e: e.activation(Z[:, 1], B0[0:64, 0:512].rearrange("p (a b) -> p a b", a=NB), AF.Copy), reads=[kB0], writes=[("r_Z", 1)])
            yield
            Zf = Z[:, 1]
            zk = [("r_Z", 1)]
            for j in range(NB):
                Ah, ul = Zf[:, j, 0:64], Zf[:, j, 64:128]
                vt, bh, kh = TK[:, j, 0:64], TK[:, j, 128:192], TK[:, j, 192:256]
                mm(B0[0:64, 256 + j * 64:256 + (j + 1) * 64], Ah, bh, zk + ["r_TK"], kB0, False)
                mm(B1[0:64, j * 64:(j + 1) * 64], bh, ul, zk + ["r_TK"], kB1, False, start=True, stop=False)
                mm(B1[0:64, j * 64:(j + 1) * 64], kh, vt, ["r_TK"], kB1, False, start=False, stop=True)
                mm(B1[0:64, 256 + j * 64:256 + (j + 1) * 64], Ah, AAm[:, j, 64:128], zk + ["r_AAm"], kB1, j == NB - 1)
            v4 = lambda ap: ap.rearrange("p (a b) -> p a b", a=NB)
            S.op("dve", lambda e: e.tensor_tensor(Mm[:], I64.unsqueeze(1).to_broadcast([64, NB, 64]),
                                                  E1v[:, c0:c0 + NB, 63:64].to_broadcast([64, NB, 64]), ALU.mult),
                 reads=["ident", ("r_E1", 0)], writes=["r_M"])
            yield
            S.op("dve", lambda e: e.tensor_tensor(Mm[:], Mm[:], v4(B0[0:64, 256:512]), ALU.add), reads=[kB0, "r_M"], writes=["r_M"])
            yield
            S.op("act", lambda e: e.activation(Sl[:], v4(B1[0:64, 0:256]), AF.Copy), reads=[kB1], writes=["r_Sl"])
            yield
            S.op("dve", lambda e: e.tensor_tensor(Rf[:], v4(B1[0:64, 256:512]), AR[:, c0:c0 + NB, 64:128], ALU.add), reads=[kB1] + rd, writes=["r_Rf"])
            yield
            for j in range(NB):
                yo = B0[0:64, j * 64:(j + 1) * 64]
                mm(yo, Rf[:, j, :], Sst[:], ["r_Rf", ("r_S", h)], kB0, False, start=True, stop=False)
                mm(yo, AAm[:, j, 64:128], Zf[:, j, 64:128], zk + ["r_AAm"], kB0, False, start=False, stop=False)
                mm(yo, AAm[:, j, 192:256], TK[:, j, 0:64], ["r_AAm", "r_TK"], kB0, False, start=False, stop=True)
                mm(B2[0:64, 0:64], Mm[:, j, :], Sst[:], ["r_M", ("r_S", h)], kB2, True)
                S.op("dve", lambda e, j=j: e.tensor_tensor(Sst[:], B2[0:64, 0:64], Sl[:, j, :], ALU.add), reads=[kB2, "r_Sl"], writes=[("r_S", h)])
                yield
            S.op("act", lambda e: e.activation(y[:], v4(B0[0:64, 0:256]), AF.Copy), reads=[kB0], writes=["r_y"])
            yield
            b3 = lambda ap: ap.unsqueeze(2).to_broadcast([64, NB, 64])
            dv = lambda fn, r_, w_: S.op("dve", fn, reads=r_, writes=w_)
            dv(lambda e: e.tensor_reduce(sst[:, 0, :], y[:], AX.X, ALU.add), ["r_y"], ["r_sst"])
            yield
            dv(lambda e: e.tensor_tensor(ysq[:], y[:], y[:], ALU.mult), ["r_y"], ["r_ysq"])
            yield
            dv(lambda e: e.tensor_reduce(sst[:, 1, :], ysq[:], AX.X, ALU.add), ["r_ysq"], ["r_sst"])
            yield
            dv(lambda e: e.tensor_scalar(sst[:, 2, :], sst[:, 0, :], 1.0 / 64, None, ALU.mult), ["r_sst"], ["r_sst"])
            yield
            dv(lambda e: e.tensor_tensor(sst[:, 3, :], sst[:, 2, :], sst[:, 2, :], ALU.mult), ["r_sst"], ["r_sst"])
            yield
            dv(lambda e: e.scalar_tensor_tensor(sst[:, 4, :], sst[:, 1, :], 1.0 / 64, sst[:, 3, :], ALU.mult, ALU.subtract), ["r_sst"], ["r_sst"])
            yield
            dv(lambda e: e.tensor_scalar(sst[:, 4, :], sst[:, 4, :], GN_EPS, None, ALU.add), ["r_sst"], ["r_sst"])
            yield
            S.op("act", lambda e: e.activation(sst[:, 5, :], sst[:, 4, :], AF.Ln), reads=["r_sst"], writes=["r_sst"])
            yield
            S.op("act", lambda e: e.activation(sst[:, 5, :], sst[:, 5, :], AF.Exp, scale=-0.5), reads=["r_sst"], writes=["r_sst"])
            yield
            dv(lambda e: e.tensor_tensor(yt[:], y[:], b3(sst[:, 2, :]), ALU.subtract), ["r_y", "r_sst"], ["r_yt"])
            yield
            dv(lambda e: e.tensor_tensor(yt[:], yt[:], b3(sst[:, 5, :]), ALU.mult), ["r_yt", "r_sst"], ["r_yt"])
            yield
            dv(lambda e: e.tensor_tensor(yt[:], yt[:], rw["gnw"][:, hs].unsqueeze(1).to_broadcast([64, NB, 64]), ALU.mult), ["r_yt", "r_gn"], ["r_yt"])
            yield
            dv(lambda e: e.tensor_tensor(yt[:], yt[:], rw["gnb"][:, hs].unsqueeze(1).to_broadcast([64, NB, 64]), ALU.add), ["r_yt", "r_gn"], ["r_yt"])
            yield
            dv(lambda e: e.tensor_tensor(ysq[:], TK[:, :, 0:64], TG[:, :, 64:65].to_broadcast([64, NB, 64]), ALU.mult), ["r_TK", "r_TG"], ["r_ysq"])
            yield
            dv(lambda e: e.tensor_tensor(yt[:], yt[:], ysq[:], ALU.add), ["r_yt", "r_ysq"], ["r_yt"])
            yield
            dv(lambda e: e.tensor_tensor(yt[:], yt[:], TG[:, :, 0:64], ALU.mult), ["r_yt", "r_TG"], ["r_yt"])
            yield
            for j in range(NB):
                mm(B1[0:64, j * 64:(j + 1) * 64], yt[:, j, :], I64, ["r_yt", "ident"], kB1, j == NB - 1)
            S.op("act", lambda e, c0=c0: e.activation(ya[:, c0 * 64:(c0 + NB) * 64], B1[0:64, 0:NB * 64], AF.Copy), reads=[kB1], writes=[("r_ya", h)])
            yield
        if "ycT" in dr:
            S.dma("sp", dr["ycT"][64 * h:64 * h + 64, tsl], ya[:], reads=[("r_ya", h)], writes=[("ya_out", g, h)])


def build_fused_nc():
    Tm = 8192
    nc = bass.Bass("TRN2", target_bir_lowering=False)
    dr = {}

    def inp(name, shape, dt=F32):
        dr[name] = nc.dram_tensor(name, list(shape), dt, kind="ExternalInput").ap()
    inp("xT", [1024, Tm])
    inp("w_sel", [1024, NCOL])
    inp("ident", [128, 128])
    inp("biasT", [128, 2, 68])
    inp("cmask", [4, 128, 512], BF16)
    inp("epat", [2, 34, Tm], BF16)
    inp("qpos", [2, Tm], BF16)
    inp("esel", [64, 4, 1024], BF16)
    rwkv_inputs(inp)
    inp("x_tok", [2048, 1024])
    inp("w_out", [1024, 1024])
    for nm in ("ln1_g", "ln1_b", "ln2_g", "ln2_b"):
        inp(nm, [1024])
    inp("w_route", [1024, 36])
    inp("b_route", [36])
    inp("w1", [32, 1024, 256])
    inp("w3", [32, 1024, 256])
    inp("w2", [32, 256, 1024])
    dr["out"] = nc.dram_tensor("out", [2048, 1024], F32, kind="ExternalOutput").ap()
    rs_in = [nc.dram_tensor("rs_in%d" % q, [4096, 512], BF16).ap() for q in range(4)]
    rs_out = [nc.dram_tensor("rs_out%d" % q, [1024, 512], BF16).ap() for q in range(4)]
    dr["rs_in"] = rs_in
    with ExitStack() as es0:
        S = Sched(nc, es0)

        def on_group_done(g):
            if g < 12:
                return
            q = g - 12
            S.coll(lambda e: e.collective_compute("ReduceScatter", ALU.add, replica_groups=[[0, 1, 2, 3], [4, 5, 6, 7]],
                                                  ins=[rs_in[q]], outs=[rs_out[q]]),
                   reads=[("rsin", gg, kc) for gg in (q, 4 + q, 8 + q, 12 + q) for kc in range(8)], writes=[("rs_out", q)])
        dr["on_group_done"] = on_group_done
        with ExitStack() as es1:
            mixer_phase(S, nc, es1, Tm, dr, True, True, fused=True)
            S.barrier(include_coll=False)
            S.replay()
        dr["ycat_q"] = rs_out
        with ExitStack() as es2:
            token_phase(S, nc, es2, 2048, dr)
            S.replay()
    return nc


def kernel(**inputs):
    d = {k: np.ascontiguousarray(np.asarray(v)) for k, v in inputs.items()}
    Tm = 8192
    nc = build_fused_nc()
    w_route = np.ascontiguousarray(np.concatenate([d["w_group"], d["w_expert"]], 1))
    b_route = np.ascontiguousarray(np.concatenate([d["b_group"], d["b_expert"]], 0))
    common = dict(w_out=d["w_out"], ln1_g=d["ln1_g"], ln1_b=d["ln1_b"], ln2_g=d["ln2_g"], ln2_b=d["ln2_b"],
                  w_route=w_route, b_route=b_route, w1=d["w1_exp"].reshape(32, 1024, 256),
                  w3=d["w3_exp"].reshape(32, 1024, 256), w2=d["w2_exp"].reshape(32, 256, 1024))
    maps = []
    for c in range(8):
        b, hg = c // 4, c % 4
        m, _ = mixer_inputs(d, c, Tm)
        m.update(rwkv_host_inputs(d, c))
        m.update(common)
        esel = np.zeros((64, 4, 1024), np.float32)
        i = np.arange(64)
        for si, base in enumerate((hg * 128, hg * 128 + 64, 512 + hg * 128, 512 + hg * 128 + 64)):
            esel[i, si, base + i] = 1.0
        m["esel"] = esel.astype(ml_dtypes.bfloat16)
        off = hg * 2048
        m["x_tok"] = np.ascontiguousarray(d["x"][b, off:off + 2048, :])
        maps.append(m)
    res = run_bass_kernel_spmd(nc, maps, core_ids=list(range(8)))
    out = np.concatenate([r["out"] for r in res.results], 0).reshape(2, Tm, 1024)
    return out.astype(np.float32)
```

```python
import numpy as np
import ml_dtypes
from contextlib import ExitStack
import concourse.bass as bass
import concourse.mybir as mybir
from concourse.bass_utils import run_bass_kernel_spmd

F32 = mybir.dt.float32
BF16 = mybir.dt.bfloat16
ALU = mybir.AluOpType
AF = mybir.ActivationFunctionType
AX = mybir.AxisListType

D = 1024
ALPHA = float(2.0 ** 0.25)
LN_EPS = 1e-5


class _Rec:
    def __init__(self):
        self.call = None

    def __getattr__(self, name):
        def f(*a, **k):
            self.call = (name, a, k)
            return self
        return f


class Sched:
    SELF_SYNC = True

    def __init__(self, nc, es, nlanes=10):
        self.nc = nc
        self.E = {}
        self.src = {}
        for n in ("pe", "act", "dve", "pool", "sp"):
            sem = es.enter_context(nc.semaphore("sem_" + n))
            self.E[n] = dict(name=n, sem=sem, cnt=0, prog=[], waited={})
            self.src[n] = (sem, 1)
        self.lanes = {}
        for q in ("sp", "pool"):
            L = []
            for i in range(nlanes):
                lid = "L%s%d" % (q, i)
                sem = es.enter_context(nc.semaphore("sem_" + lid))
                self.src[lid] = (sem, 16)
                L.append(dict(id=lid, cnt=0))
            self.lanes[q] = dict(lanes=L, nxt=0)
        self.B = {}
        csem = es.enter_context(nc.semaphore("sem_coll"))
        self.src["coll"] = (csem, 1)
        self.ncoll = 0

    def coll(self, fn, reads=(), writes=()):
        e = self.E["pool"]
        self._emit_waits(e, self._deps(reads, writes))
        self.ncoll += 1
        r = _Rec()
        fn(r)
        e["prog"].append(("coll", r.call))
        self._mark(("coll", self.ncoll), reads, writes)

    def barrier(self, include_coll=True):
        for e in self.E.values():
            deps = {}
            for n, o in self.E.items():
                if n != e["name"] and o["cnt"] > 0:
                    deps[n] = o["cnt"]
            for q in self.lanes.values():
                for lane in q["lanes"]:
                    if lane["cnt"] > 0:
                        deps[lane["id"]] = lane["cnt"]
            if self.ncoll > 0 and include_coll:
                deps["coll"] = self.ncoll
            self._emit_waits(e, deps)

    @staticmethod
    def is_psum(k):
        return isinstance(k, tuple) and isinstance(k[0], str) and k[0].startswith("P")

    def _buf(self, k):
        b = self.B.get(k)
        if b is None:
            b = self.B[k] = dict(w=None, r={})
        return b

    def _deps(self, reads, writes):
        deps = {}

        def add(t):
            if t is None:
                return
            s, n = t
            if deps.get(s, 0) < n:
                deps[s] = n
        for k in reads:
            b = self._buf(k)
            add(b["w"])
            if self.is_psum(k):
                for s, n in b["r"].items():
                    add((s, n))
        for k in writes:
            b = self._buf(k)
            add(b["w"])
            for s, n in b["r"].items():
                add((s, n))
        return deps

    def _emit_waits(self, e, deps):
        for s, n in deps.items():
            if s == e["name"]:
                if s == "pe" or not self.SELF_SYNC or n > e["cnt"]:
                    continue
            if e["waited"].get(s, 0) < n:
                e["prog"].append(("wait", s, n))
                e["waited"][s] = n

    def _mark(self, t, reads, writes):
        s, n = t
        for k in reads:
            b = self._buf(k)
            if b["r"].get(s, 0) < n:
                b["r"][s] = n
        for k in writes:
            b = self._buf(k)
            b["w"] = t
            b["r"] = {}

    def op(self, eng, fn, reads=(), writes=(), inc=True):
        e = self.E[eng]
        self._emit_waits(e, self._deps(reads, writes))
        t = (eng, e["cnt"] + 1)
        if inc:
            e["cnt"] += 1
        r = _Rec()
        fn(r)
        e["prog"].append(("inst", r.call, inc))
        self._mark(t, reads, writes)

    def dma(self, q, out, in_, reads=(), writes=()):
        e = self.E[q]
        self._emit_waits(e, self._deps(reads, writes))
        LL = self.lanes[q]
        lane = LL["lanes"][LL["nxt"]]
        LL["nxt"] = (LL["nxt"] + 1) % len(LL["lanes"])
        if lane["cnt"] > 0 and e["waited"].get(lane["id"], 0) < lane["cnt"]:
            e["prog"].append(("wait", lane["id"], lane["cnt"]))
            e["waited"][lane["id"]] = lane["cnt"]
        lane["cnt"] += 1
        t = (lane["id"], lane["cnt"])
        e["prog"].append(("dma", out, in_, lane["id"]))
        self._mark(t, reads, writes)

    def finish(self, keys):
        e = self.E["sp"]
        self._emit_waits(e, self._deps(keys, ()))

    def replay(self):
        nc = self.nc
        with nc.Block() as block:
            def run(name):
                def f(eng):
                    mysem = self.E[name]["sem"]
                    start = self.E[name].get("done", 0)
                    self.E[name]["done"] = len(self.E[name]["prog"])
                    for it in self.E[name]["prog"][start:]:
                        if it[0] == "wait":
                            sem, sc = self.src[it[1]]
                            eng.wait_ge(sem, it[2] * sc)
                        elif it[0] == "inst":
                            name_, a_, k_ = it[1]
                            ins = getattr(eng, name_)(*a_, **k_)
                            if it[2]:
                                ins.then_inc(mysem, 1)
                        elif it[0] == "coll":
                            name_, a_, k_ = it[1]
                            getattr(eng, name_)(*a_, **k_).then_inc(self.src["coll"][0], 1)
                        else:
                            sem, _ = self.src[it[3]]
                            eng.dma_start(out=it[1], in_=it[2]).then_inc(sem, 16)
                return f
            block.tensor(run("pe"))
            block.scalar(run("act"))
            block.vector(run("dve"))
            block.gpsimd(run("pool"))
            block.sync(run("sp"))


class _Cut(Exception):
    pass


def token_phase(S, nc, es, T, dr, NE=32, do_route=True):
    import os
    CUT = int(os.environ.get("TOKEN_CUT", "0"))

    def cut(k, keys):
        if CUT == k:
            S.finish(keys)
            raise _Cut()

    NT = T // 128
    NG = T // 512

    def sb(name, shape, dt):
        return es.enter_context(nc.sbuf_tensor(name, shape, dt))

    P = [es.enter_context(nc.psum_tensor("tP%d" % i, [128, 1024], F32)) for i in range(4)]

    ident = sb("t_ident", [128, 128], F32)
    ones1 = sb("t_ones1", [1, 128], F32)
    wout = sb("t_wout", [128, 8, 1024], BF16)
    lnp = sb("t_lnp", [128, 2, 1024], F32)
    stg0 = sb("t_stg0", [128, 2048], F32)
    stg = None
    wr = sb("t_wr", [128, 8, 36], F32)
    br = sb("t_br", [1, 36], F32)
    ycF = sb("t_ycT", [128, max(8 * T, 16384)], BF16)
    ycT = ycF[:, 0:8 * T].rearrange("p (a b) -> p a b", a=8)
    h1T = sb("t_h1T", [128, 8, T], BF16)
    acc = sb("t_acc", [128, NT, 1024], F32)
    xb = [sb("t_xb%d" % i, [128, 1024], F32) for i in range(2)]
    hb1 = sb("t_hb", [128, 1024], F32)
    hb = [hb1, hb1]
    h1b1 = sb("t_h1b", [128, 1024], F32)
    h1b = [h1b1, h1b1]
    hTf = sb("t_hTf", [128, 8, 128], F32)
    st6 = sb("t_st6", [128, 2, 6], F32)
    mv = sb("t_mv", [128, 2], F32)
    sm = sb("t_sm", [128, 8], F32)
    rlog = sb("t_rlog", [128, NT, 36], F32)
    gates = sb("t_gates", [128, NT, 32], F32)
    rt = sb("t_rt", [128, 12, NT, 8], F32)
    w1b = [ycF[:, (0 + i) * 2048:(1 + i) * 2048].rearrange("p (a b) -> p a b", a=8) for i in range(2)]
    w3b = [ycF[:, (2 + i) * 2048:(3 + i) * 2048].rearrange("p (a b) -> p a b", a=8) for i in range(2)]
    w2b = [ycF[:, (4 + i) * 2048:(5 + i) * 2048].rearrange("p (a b) -> p a b", a=2) for i in range(2)]
    ssb = [sb("t_ssb%d" % i, [128, 512], F32) for i in range(2)]
    hidT = [sb("t_hidT%d" % i, [128, 2, 512], BF16) for i in range(2)]
    ob = xb

    S.dma("sp", ident[:], dr["ident"], writes=["ident"])
    S.op("pool", lambda e: e.memset(ones1[:], 1.0), writes=["ones1"])
    for i in range(4):
        S.dma("sp", stg0[:].rearrange("p (a b) -> p a b", a=2),
              dr["w_out"].rearrange("(kc p) n -> p kc n", p=128)[:, 2 * i:2 * i + 2, :], writes=["stg0"])
        S.op("pool", lambda e, i=i: e.tensor_copy(wout[:, 2 * i, :], stg0[:, 0:1024]), reads=["stg0"], writes=[("wout", 2 * i)])
        S.op("dve", lambda e, i=i: e.tensor_copy(wout[:, 2 * i + 1, :], stg0[:, 1024:2048]), reads=["stg0"], writes=[("wout", 2 * i + 1)])
    for i, nm in enumerate(("ln1_g", "ln1_b")):
        S.dma("sp", lnp[:, i, :], dr[nm].partition_broadcast(128), writes=["lnp"])
    S.dma("sp", wr[:], dr["w_route"].rearrange("(kc p) n -> p kc n", p=128), writes=["wr"])
    S.dma("sp", br[:], dr["b_route"].unsqueeze(0), writes=["br"])
    def load_ycT(q):
        if "ycat_q" in dr:
            S.dma("sp", ycT[:, :, q * 512:(q + 1) * 512], dr["ycat_q"][q].rearrange("(kc p) t -> p kc t", p=128),
                  reads=[("rs_out", q)], writes=[("ycT", q)])
            return
        S.dma("sp", ycT[:, :, q * 512:(q + 1) * 512],
              dr["ycatT"].rearrange("(kc p) t -> p kc t", p=128)[:, :, q * 512:(q + 1) * 512],
              reads=dr.get("ycat_keys", []), writes=[("ycT", q)])

    stg = [stg0[:], ycF[:, 12288:16384].bitcast(F32)]
    pc = [0]

    def load_piece(dst, src, a, dkey, extra):
        k = pc[0] % 2
        pc[0] += 1
        S.dma("sp", stg[k].rearrange("p (a b) -> p a b", a=a), src, writes=["stg%d" % k] + (extra if k == 1 else []))
        S.op("pool", lambda e, k=k: e.tensor_copy(dst, stg[k].rearrange("p (a b) -> p a b", a=a)),
             reads=["stg%d" % k], writes=[dkey] + extra)

    def load_expert(e):
        s = e % 2
        yk = [("ycT", q) for q in range(NG)] if e < 2 else []
        load_piece(w1b[s], dr["w1"][e].rearrange("(kc p) f -> p kc f", p=128), 8, ("w1b", s), yk)
        load_piece(w3b[s], dr["w3"][e].rearrange("(kc p) f -> p kc f", p=128), 8, ("w3b", s), yk)
        load_piece(w2b[s], dr["w2"][e].rearrange("(fc p) d -> p fc d", p=128), 2, ("w2b", s), yk)

    def layer_norm(src_ap, srck, gi, dst_ap, dstk, tmp_ap, tmpk, eps=LN_EPS):
        for h in range(2):
            S.op("dve", lambda e, h=h: e.bn_stats(st6[:, h, :], src_ap[:, h * 512:(h + 1) * 512]),
                 reads=[srck], writes=["st6"])
        S.op("dve", lambda e: e.bn_aggr(mv[:], st6[:].rearrange("p a b -> p (a b)")), reads=["st6"], writes=["mv"])
        S.op("dve", lambda e: e.tensor_scalar(sm[:, 0:1], mv[:, 1:2], eps, None, ALU.add),
             reads=["mv"], writes=["sm0"])
        S.op("act", lambda e: e.activation(sm[:, 1:2], sm[:, 0:1], AF.Sqrt), reads=["sm0"], writes=["sm1"])
        S.op("dve", lambda e: e.reciprocal(sm[:, 2:3], sm[:, 1:2]), reads=["sm1"], writes=["sm2"])
        S.op("dve", lambda e: e.tensor_scalar(sm[:, 3:4], mv[:, 0:1], sm[:, 2:3], -1.0, ALU.mult, ALU.mult),
             reads=["mv", "sm2"], writes=["sm3"])
        S.op("act", lambda e: e.activation(tmp_ap, src_ap, AF.Identity, bias=sm[:, 3:4], scale=sm[:, 2:3]),
             reads=[srck, "sm2", "sm3"], writes=[tmpk])
        S.op("dve", lambda e: e.tensor_tensor(tmp_ap, tmp_ap, lnp[:, gi, :], ALU.mult),
             reads=[tmpk, "lnp"], writes=[tmpk])
        S.op("pool", lambda e: e.tensor_tensor(dst_ap, tmp_ap, lnp[:, gi + 1, :], ALU.add),
             reads=[tmpk, "lnp"], writes=[dstk])

    cut(1, ['ident', 'wout', 'lnp', 'wr', 'br', ('ycT', 0), ('ycT', NG - 1)])
    def t1_outproj(t):
        s = t % 2
        if t % 4 == 0:
            load_ycT(t // 4)
        S.dma("sp", xb[s][:], dr["x_tok"][t * 128:(t + 1) * 128, :], writes=[("xbt", s)])
        for half in range(2):
            for kc in range(8):
                S.op("pe", lambda e, half=half, kc=kc: e.matmul(
                    P[s][:, half * 512:(half + 1) * 512], lhsT=ycT[:, kc, t * 128:(t + 1) * 128],
                    rhs=wout[:, kc, half * 512:(half + 1) * 512], start=(kc == 0), stop=(kc == 7)),
                    reads=[("ycT", t // 4), ("wout", kc)], writes=[("P", s)], inc=(kc == 7 and half == 1))

    def t1_norm(t):
        s = t % 2
        hbt = (hb1, h1b1)[s]
        hk = ("hb", s)
        S.op("dve", lambda e: e.scalar_tensor_tensor(hbt[:], xb[s][:], ALPHA, P[s][:], ALU.mult, ALU.add),
             reads=[("xbt", s), ("P", s)], writes=[hk])
        layer_norm(hbt[:], hk, 0, acc[:, t, :], ("acc", t), hbt[:], hk)

    def t1_route(t):
        for j in range(8):
            S.op("pe", lambda e, j=j: e.transpose(P[2][:, j * 128:(j + 1) * 128], acc[:, t, j * 128:(j + 1) * 128], ident[:]),
                 reads=[("acc", t), "ident"], writes=[("PA", 0), ("PB", 0)], inc=(j == 7))
        S.op("act", lambda e: e.activation(h1T[:, :, t * 128:(t + 1) * 128],
                                           P[2][:].rearrange("p (a b) -> p a b", a=8), AF.Copy),
             reads=[("PA", 0), ("PB", 0)], writes=[("h1T", t // 4)])
        S.op("dve", lambda e: e.tensor_copy(hTf[:], P[2][:].rearrange("p (a b) -> p a b", a=8)),
             reads=[("PA", 0), ("PB", 0)], writes=["hTf"])
        for kc in range(8):
            S.op("pe", lambda e, kc=kc: e.matmul(P[3][:, 0:36], lhsT=hTf[:, kc, :], rhs=wr[:, kc, :],
                                                 start=(kc == 0), stop=False),
                 reads=["hTf", "wr"], writes=[("PA", 1), ("PB", 1)], inc=False)
        S.op("pe", lambda e: e.matmul(P[3][:, 0:36], lhsT=ones1[:], rhs=br[:], start=False, stop=True),
             reads=["ones1", "br"], writes=[("PA", 1), ("PB", 1)])
        S.op("act", lambda e: e.activation(rlog[:, t, :], P[3][:, 0:36], AF.Copy),
             reads=[("PA", 1), ("PB", 1)], writes=["rlog"])

    t1_outproj(0)
    if NT > 1:
        t1_outproj(1)
    t1_norm(0)
    for t in range(NT):
        if t + 1 < NT:
            t1_norm(t + 1)
        if t + 2 < NT:
            t1_outproj(t + 2)
        t1_route(t)
    if not do_route:
        S.op('dve', lambda e: e.memset(gates[:], 0.03), writes=['gates'])
    gl = rlog[:, :, 0:4]
    _op = S.op
    if not do_route:
        S.op = lambda *a, **k: None
    R = lambda i, w=8: rt[:, i, :, 0:w]
    R1 = lambda i: rt[:, i, :, 0]
    bc = lambda ap, w: ap.unsqueeze(2).to_broadcast([128, NT, w])

    def dv(fn, reads, writes):
        S.op("dve", fn, reads=reads, writes=writes)
    dv(lambda e: e.tensor_reduce(R1(0), gl, AX.X, ALU.max), ["rlog"], ["r0"])
    dv(lambda e: e.tensor_tensor(R(1, 4), gl, bc(R1(0), 4), ALU.is_equal), ["rlog", "r0"], ["r1"])
    dv(lambda e: e.tensor_tensor(R(2, 4), gl, bc(R1(0), 4), ALU.subtract), ["rlog", "r0"], ["r2"])
    S.op("act", lambda e: e.activation(R(2, 4), R(2, 4), AF.Exp), reads=["r2"], writes=["r2"])
    dv(lambda e: e.tensor_reduce(R1(3), R(2, 4), AX.X, ALU.add), ["r2"], ["r3"])
    for g in range(4):
        oh = rt[:, 1, :, g]
        el = rlog[:, :, 4 + g * 8:12 + g * 8]
        if g == 0:
            dv(lambda e, oh=oh, el=el: e.tensor_tensor(R(4), el, bc(oh, 8), ALU.mult), ["rlog", "r1"], ["r4"])
        else:
            dv(lambda e, oh=oh, el=el: e.tensor_tensor(R(5), el, bc(oh, 8), ALU.mult), ["rlog", "r1"], ["r5"])
            dv(lambda e: e.tensor_tensor(R(4), R(4), R(5), ALU.add), ["r4", "r5"], ["r4"])
    dv(lambda e: e.tensor_reduce(R1(6), R(4), AX.X, ALU.max), ["r4"], ["r6"])
    dv(lambda e: e.tensor_tensor(R(7), R(4), bc(R1(6), 8), ALU.is_equal), ["r4", "r6"], ["r7"])
    dv(lambda e: e.scalar_tensor_tensor(R(8), R(7), -1e30, R(4), ALU.mult, ALU.add), ["r7", "r4"], ["r8"])
    dv(lambda e: e.tensor_reduce(R1(9), R(8), AX.X, ALU.max), ["r8"], ["r9"])
    dv(lambda e: e.tensor_tensor(R(5), R(4), bc(R1(6), 8), ALU.subtract), ["r4", "r6"], ["r5"])
    S.op("act", lambda e: e.activation(R(5), R(5), AF.Exp), reads=["r5"], writes=["r5"])
    dv(lambda e: e.tensor_tensor(R(7), R(4), bc(R1(9), 8), ALU.is_ge), ["r4", "r9"], ["r7"])
    dv(lambda e: e.tensor_tensor(R(5), R(5), R(7), ALU.mult), ["r5", "r7"], ["r5"])
    dv(lambda e: e.tensor_reduce(R1(10), R(5), AX.X, ALU.add), ["r5"], ["r10"])
    dv(lambda e: e.tensor_tensor(R1(10), R1(10), R1(3), ALU.mult), ["r10", "r3"], ["r10"])
    dv(lambda e: e.tensor_scalar(R1(10), R1(10), ALPHA, None, ALU.mult), ["r10"], ["r10"])
    dv(lambda e: e.reciprocal(R1(11), R1(10)), ["r10"], ["r11"])
    dv(lambda e: e.tensor_tensor(R(5), R(5), bc(R1(11), 8), ALU.mult), ["r5", "r11"], ["r5"])
    for g in range(4):
        oh = rt[:, 1, :, g]
        dv(lambda e, oh=oh, g=g: e.tensor_tensor(gates[:, :, g * 8:(g + 1) * 8], R(5), bc(oh, 8), ALU.mult),
           ["r5", "r1"], ["gates"])

    S.op = _op
    if NE > 0:
        load_expert(0)
    if NE > 1:
        load_expert(1)
    PA = [P[2][:, 0:512], P[3][:, 0:512]]
    PB = [P[2][:, 512:1024], P[3][:, 512:1024]]
    it = 0
    pend = None
    for ex in range(NE):
        s = ex % 2
        for tg in range(NG):
            hs = (ex * NG + tg) % 2
            for fc in range(2):
                u = it % 2
                it += 1
                for kc in range(8):
                    S.op("pe", lambda e, kc=kc, fc=fc, tg=tg, s=s, u=u: e.matmul(
                        PA[u], lhsT=w1b[s][:, kc, fc * 128:(fc + 1) * 128], rhs=h1T[:, kc, tg * 512:(tg + 1) * 512],
                        start=(kc == 0), stop=(kc == 7)),
                        reads=[("w1b", s), ("h1T", tg)], writes=[("PA", u)], inc=(kc == 7))
                for kc in range(8):
                    S.op("pe", lambda e, kc=kc, fc=fc, tg=tg, s=s, u=u: e.matmul(
                        PB[u], lhsT=w3b[s][:, kc, fc * 128:(fc + 1) * 128], rhs=h1T[:, kc, tg * 512:(tg + 1) * 512],
                        start=(kc == 0), stop=(kc == 7)),
                        reads=[("w3b", s), ("h1T", tg)], writes=[("PB", u)], inc=(kc == 7))
                S.op("act", lambda e, u=u: e.activation(ssb[u][:], PA[u], AF.Silu),
                     reads=[("PA", u)], writes=[("ssb", u)])
                S.op("dve", lambda e, u=u, hs=hs, fc=fc: e.tensor_tensor(hidT[hs][:, fc, :], PB[u], ssb[u][:], ALU.mult),
                     reads=[("PB", u), ("ssb", u)], writes=[("hidT", hs)])
                if pend is not None:
                    pend(fc)

            def second(part, ex=ex, tg=tg, s=s, hs=hs):
                for tt in range(2 * part, 2 * part + 2):
                    t = tg * 4 + tt
                    o = t % 2
                    for half in range(2):
                        for fc in range(2):
                            S.op("pe", lambda e, half=half, fc=fc, tt=tt, o=o: e.matmul(
                                P[o][:, half * 512:(half + 1) * 512], lhsT=hidT[hs][:, fc, tt * 128:(tt + 1) * 128],
                                rhs=w2b[s][:, fc, half * 512:(half + 1) * 512], start=(fc == 0), stop=(fc == 1)),
                                reads=[("hidT", hs), ("w2b", s)], writes=[("P", o)], inc=(half == 1 and fc == 1))
                    S.op("dve", lambda e, t=t, o=o: e.scalar_tensor_tensor(
                        acc[:, t, :], P[o][:], gates[:, t, ex:ex + 1], acc[:, t, :], ALU.mult, ALU.add),
                        reads=[("P", o), "gates", ("acc", t)], writes=[("acc", t)])
                if part == 1 and tg == NG - 1 and ex + 2 < NE:
                    load_expert(ex + 2)
            pend = second
    if pend is not None:
        pend(0)
        pend(1)
    for i, nm in enumerate(("ln2_g", "ln2_b")):
        S.dma("sp", lnp[:, i, :], dr[nm].partition_broadcast(128), writes=["lnp"])
    for t in range(NT):
        s = t % 2
        layer_norm(acc[:, t, :], ("acc", t), 0, ob[s][:], ("xbt", s), acc[:, t, :], ("acc", t), eps=LN_EPS / (ALPHA * ALPHA))
        S.dma("sp", dr["out"][t * 128:(t + 1) * 128, :], ob[s][:], reads=[("xbt", s)], writes=[("out", t)])
    S.finish([("out", t) for t in range(NT)])


def build_token_nc(T=2048, NE=32, do_route=True):
    nc = bass.Bass("TRN2", target_bir_lowering=False)
    dr = {}

    def inp(name, shape, dt=F32):
        dr[name] = nc.dram_tensor(name, list(shape), dt, kind="ExternalInput").ap()
    inp("ycatT", [1024, T], BF16)
    inp("x_tok", [T, 1024])
    inp("w_out", [1024, 1024])
    for nm in ("ln1_g", "ln1_b", "ln2_g", "ln2_b"):
        inp(nm, [1024])
    inp("w_route", [1024, 36])
    inp("b_route", [36])
    inp("w1", [32, 1024, 256])
    inp("w3", [32, 1024, 256])
    inp("w2", [32, 256, 1024])
    inp("ident", [128, 128])
    dr["out"] = nc.dram_tensor("out", [T, 1024], F32, kind="ExternalOutput").ap()
    with ExitStack() as es:
        S = Sched(nc, es)
        try:
            token_phase(S, nc, es, T, dr, NE, do_route)
        except _Cut:
            pass
        S.replay()
    return nc


NCOL = 928
G_R, G_K, G_V, G_L1, G_GD, G_Q, G_MK, G_MV = 0, 128, 256, 384, 448, 544, 672, 800
MNEG = -30000.0


def mixer_phase(S, nc, es, Tm, dr, do_rwkv=True, do_moba=True, fused=False):
    NGm = Tm // 512
    NKT = Tm // 128

    def sb(name, shape, dt):
        return es.enter_context(nc.sbuf_tensor(name, shape, dt))

    def ps(name, shape, dt=F32):
        return es.enter_context(nc.psum_tensor(name, shape, dt))

    PI = [ps("mPI%d" % i, [128, 512]) for i in range(2)]
    PS = [ps("mPS%d" % i, [128, 512]) for i in range(2)]
    PO = ps("mPO", [128, 512])
    PM = ps("mPM", [128, 512])
    PR = [ps("mPR%d" % i, [128, 512]) for i in range(2)]

    ident = sb("m_ident", [128, 128], F32)
    identb = sb("m_identb", [128, 128], BF16)
    wb = sb("m_wb", [128, 8, NCOL], BF16)
    xs0 = sb("m_xs", [128, 4, 512], F32)
    xs = [xs0, xs0]
    xb0 = sb("m_xb", [128, 8, 512], BF16)
    xb = [xb0, xb0]
    QT = [sb("m_QT%d" % h, [98, 512], BF16) for h in range(2)]
    KT = [sb("m_KT%d" % h, [98, Tm], BF16) for h in range(2)]
    VA = sb("m_VA", [128, NKT, 2, 65], BF16)
    qf = [sb("m_qf%d" % h, [64, 512], F32) for h in range(2)]
    kmean = [sb("m_km%d" % h, [64, 32], F32) for h in range(2)]
    biasT = sb("m_biasT", [128, 2, 68], F32)
    cm = sb("m_cm", [128, 4, 512], BF16)
    est = xs0[:].rearrange("p a b -> p (a b)")[:, 0:2048]
    wst = xs0[:].rearrange("p a b -> p (a b)")[:, 0:NCOL]
    PT = [sb("m_PT%d" % i, [128, 512], BF16) for i in range(2)]
    gsel = sb("m_gsel", [128, 4, 32], F32)
    top8 = sb("m_top8", [128, 4, 8], F32)
    mbp = sb("m_mbp", [128, 4, 96], F32)
    osb = sb("m_osb", [65, 512], F32)
    rcp = sb("m_rcp", [65, 512], F32)
    ones65 = sb("m_ones65", [65, 64], F32)
    ybs = [sb("m_yb%d" % h, [64, 512], BF16) for h in range(2 if fused else 1)]
    if fused:
        Esel = sb("m_Esel", [64, 4, 1024], BF16)
        pstg = [sb("m_pstg%d" % i, [128, 512], BF16) for i in range(2)]

    S.dma("sp", ident[:], dr["ident"], writes=["ident"])
    S.op("dve", lambda e: e.tensor_copy(identb[:], ident[:]), reads=["ident"], writes=["identb"])
    S.dma("sp", biasT[:], dr["biasT"], writes=["biasT"])
    S.op("dve", lambda e: e.memset(ones65[:], 1.0), writes=["ones65"])
    S.op("dve", lambda e: e.memset(mbp[:], 0.0), writes=[("mbp", i) for i in range(4)])
    S.op("pool", lambda e: e.memset(VA[:], 1.0), writes=["VA"])
    for h in range(2):
        S.op("dve", lambda e, h=h: e.memset(kmean[h][:], 0.0), writes=[("kmean", h)])
    wst2 = [xs0[:].rearrange("p a b -> p (a b)")[:, 0:NCOL], xs0[:].rearrange("p a b -> p (a b)")[:, 1024:1024 + NCOL]]
    for kc in range(8):
        S.dma("sp", wst2[kc % 2], dr["w_sel"][kc * 128:(kc + 1) * 128, :], writes=[("wst", kc % 2)])
        S.op("pool" if kc % 2 == 0 else "dve", lambda e, kc=kc: e.tensor_copy(wb[:, kc, :], wst2[kc % 2]),
             reads=[("wst", kc % 2)], writes=[("wb", kc)])

    rw = rwkv_setup(S, nc, es, dr, sb) if do_rwkv else None

    pic = [0]
    NY = [None]
    pending_place = [None]

    def inproj_group(g, col0, M, consume):
        s = g % 2
        u = pic[0] % 2
        pic[0] += 1
        for kc in range(8):
            S.op("pe", lambda e, kc=kc, u=u, s=s: e.matmul(PI[u][0:M, :], lhsT=wb[:, kc, col0:col0 + M], rhs=xb[s][:, kc, :],
                                                          start=(kc == 0), stop=(kc == 7)),
                 reads=[("wb", kc), "xb"], writes=[("PI", u)], inc=(kc == 7))
        consume(PI[u], ("PI", u))

    def load_x(gg):
        for half in range(2):
            S.dma("sp", xs0[:], dr["xT"].rearrange("(kc p) t -> p kc t", p=128)[:, 4 * half:4 * half + 4, gg * 512:(gg + 1) * 512],
                  writes=["xs", ("wst", 0), ("wst", 1)])
            S.op("pool", lambda e, half=half: e.tensor_copy(xb0[:, 4 * half:4 * half + 4, :], xs0[:]),
                 reads=["xs"], writes=["xb"])
    load_x(0)
    S.dma("sp", cm[:], dr["cmask"].rearrange("j p t -> p j t"), writes=["cm"])
    for h in range(2):
        S.dma("sp", KT[h][64:98, :], dr["epat"][h], writes=[("KTe", h)])
        S.dma("sp", QT[h][96:98, :], dr["qpos"][:, 0:512], writes=[("QTp", h)])
    if fused:
        S.dma("sp", Esel[:], dr["esel"], writes=["Esel"])
    for g in range(NGm):
        s = g % 2
        tsl = slice(g * 512, (g + 1) * 512)
        if do_moba:
            for h in range(2):
                def cq(P, pk, h=h):
                    S.op("act", lambda e: e.activation(QT[h][0:64, :], P[0:64, :], AF.Copy), reads=[pk], writes=[("QTq", h)])
                    S.op("dve", lambda e: e.tensor_copy(qf[h][:], P[0:64, :]), reads=[pk], writes=[("qf", h)])
                inproj_group(g, G_Q + 64 * h, 64, cq)

                def ck(P, pk, h=h):
                    S.op("act", lambda e: e.activation(KT[h][0:64, tsl], P[0:64, :], AF.Copy), reads=[pk], writes=[("KTk", h)])
                    S.op("dve", lambda e: e.tensor_reduce(kmean[h][:, 2 * g:2 * g + 2], P[0:64, :].rearrange("p (a b) -> p a b", a=2), AX.X, ALU.add),
                         reads=[pk], writes=[("kmean", h)])
                inproj_group(g, G_MK + 64 * h, 64, ck)
            for tt in range(4):
                u = pic[0] % 2
                pic[0] += 1
                kt = g * 4 + tt
                for kc in range(8):
                    S.op("pe", lambda e, kc=kc, u=u, s=s, tt=tt: e.matmul(PI[u][:, 0:128], lhsT=xb[s][:, kc, tt * 128:(tt + 1) * 128],
                                                                         rhs=wb[:, kc, G_MV:G_MV + 128], start=(kc == 0), stop=(kc == 7)),
                         reads=[("wb", kc), "xb"], writes=[("PI", u)], inc=(kc == 7))
                S.op("act", lambda e, u=u, kt=kt: e.activation(VA[:, kt, :, 0:64], PI[u][:, 0:128].rearrange("p (a b) -> p a b", a=2), AF.Copy),
                     reads=[("PI", u)], writes=["VA"])
        adv = lambda: None
        gen = None
        if do_rwkv:
            rwkv_inproj(S, nc, rw, g, inproj_group, dr, ident)
            if NY[0] is None:
                class _Null:
                    op = staticmethod(lambda *a, **k: None)
                    dma = staticmethod(lambda *a, **k: None)
                NY[0] = sum(1 for _ in rwkv_compute(_Null, nc, rw, g, dr, PR, PI[0], ("PI", 0), ident))
            gen = rwkv_compute(S, nc, rw, g, dr, PR, PI[0], ("PI", 0), ident)
            n_it = 2 * (4 * g + 4) + 8
            per = -(-NY[0] // n_it)

            def adv(gen=gen, per=per):
                for _ in range(per):
                    try:
                        next(gen)
                    except StopIteration:
                        return
        if pending_place[0] is not None:
            pending_place[0]()
            pending_place[0] = None
        pf = []
        if g + 1 < NGm:
            def x_dma(half, gg=g + 1):
                S.dma("sp", xs0[:], dr["xT"].rearrange("(kc p) t -> p kc t", p=128)[:, 4 * half:4 * half + 4, gg * 512:(gg + 1) * 512],
                      writes=["xs"])

            def x_cast(half):
                S.op("act", lambda e: e.activation(xb0[:, 4 * half:4 * half + 4, :], xs0[:], AF.Copy), reads=["xs"], writes=["xb"])
            x_dma(0)
            pf = [lambda: (x_cast(0), x_dma(1)), lambda: x_cast(1)]
        if not do_moba:
            for _ in gen:
                pass
            while pf:
                pf.pop(0)()
            continue
        for h in range(2):
            for cq4 in range(4):
                blk = (g * 4 + cq4) // 2
                if blk > 0:
                    S.op("pe", lambda e, h=h, cq4=cq4, blk=blk: e.matmul(PM[:, cq4 * 32:cq4 * 32 + blk], lhsT=qf[h][:, cq4 * 128:(cq4 + 1) * 128],
                                                                       rhs=kmean[h][:, 0:blk], start=True, stop=True),
                         reads=[("qf", h), ("kmean", h)], writes=[("PM", 0)])
            adv()
            for cq4 in range(4):
                blk = (g * 4 + cq4) // 2
                mk, gk, tk = ("mbp", cq4), ("gsel", cq4), ("top8", cq4)
                mb_, gs_, t8_ = mbp[:, cq4, :], gsel[:, cq4, :], top8[:, cq4, :]
                S.op("dve", lambda e, mb_=mb_: e.memset(mb_[:, 64:96], MNEG), writes=[mk])
                if blk > 3:
                    S.op("dve", lambda e, gs_=gs_: e.memset(gs_, -1e30), writes=[gk])
                    S.op("dve", lambda e, gs_=gs_, cq4=cq4, blk=blk: e.tensor_copy(gs_[:, 0:blk], PM[:, cq4 * 32:cq4 * 32 + blk]),
                         reads=[("PM", 0)], writes=[gk])
                    S.op("dve", lambda e, gs_=gs_, t8_=t8_: e.max(t8_, gs_), reads=[gk], writes=[tk])
                    S.op("dve", lambda e, mb_=mb_, gs_=gs_, t8_=t8_, blk=blk: e.tensor_scalar(mb_[:, 64:64 + blk], gs_[:, 0:blk], t8_[:, 2:3], -MNEG,
                                                                                   ALU.is_ge, ALU.mult),
                         reads=[gk, tk], writes=[mk])
                    S.op("dve", lambda e, mb_=mb_, blk=blk: e.tensor_scalar(mb_[:, 64:64 + blk], mb_[:, 64:64 + blk], MNEG, None, ALU.add),
                         reads=[mk], writes=[mk])
                elif blk > 0:
                    S.op("dve", lambda e, mb_=mb_, blk=blk: e.memset(mb_[:, 64:64 + blk], 0.0), writes=[mk])
                S.op("dve", lambda e, mb_=mb_, blk=blk: e.memset(mb_[:, 64 + blk:65 + blk], 0.0), writes=[mk])
                adv()
            for cq4 in range(4):
                S.op("pe", lambda e, cq4=cq4: e.matmul(PO[0:96, cq4 * 128:(cq4 + 1) * 128],
                                                       lhsT=mbp[:, cq4, :], rhs=ident[:], start=True, stop=True),
                     reads=[("mbp", cq4), "ident"], writes=[("PO", 0)])
            S.op("act", lambda e, h=h: e.activation(QT[h][64:96, :], PO[64:96, :], AF.Copy), reads=[("PO", 0)], writes=[("QTm", h)])
            if pf:
                pf.pop(0)()
            nkt = 4 * g + 4

            def emit_st(kt, h=h):
                u = kt % 2
                dl = kt - 4 * g
                S.op("pe", lambda e: e.matmul(PS[u][:], lhsT=KT[h][0:98, kt * 128:(kt + 1) * 128], rhs=QT[h][0:98, :],
                                              start=True, stop=(dl < 0)),
                     reads=[("KTk", h), ("KTe", h), ("QTq", h), ("QTm", h), ("QTp", h)], writes=[("PS", u)], inc=(dl < 0))
                if dl >= 0:
                    S.op("pe", lambda e: e.matmul(PS[u][:], lhsT=identb[:], rhs=cm[:, dl, :], start=False, stop=True),
                         reads=["identb", "cm"], writes=[("PS", u)])
            emit_st(0)
            for kt in range(nkt):
                u = kt % 2
                dl = kt - 4 * g
                if kt + 1 < nkt:
                    emit_st(kt + 1)
                S.op("act", lambda e, h=h, u=u, dl=dl: e.activation(PT[u][:], PS[u][:], AF.Exp, bias=biasT[:, h, dl + 64:dl + 65], scale=0.125),
                     reads=[("PS", u), "biasT"], writes=[("PT", u)])
                adv()
                S.op("pe", lambda e, h=h, kt=kt, u=u, nkt=nkt: e.matmul(PO[0:65, :], lhsT=VA[:, kt, h, :], rhs=PT[u][:],
                                                                       start=(kt == 0), stop=(kt == nkt - 1)),
                     reads=["VA", ("PT", u)], writes=[("PO", 0)])
            S.op("act", lambda e: e.activation(rcp[64:65, :], PO[64:65, :], AF.Ln), reads=[("PO", 0)], writes=["rcp"])
            S.op("act", lambda e: e.activation(rcp[64:65, :], rcp[64:65, :], AF.Exp, scale=-1.0), reads=["rcp"], writes=["rcp"])
            S.op("act", lambda e: e.activation(osb[0:64, :], PO[0:64, :], AF.Copy), reads=[("PO", 0)], writes=["osb"])
            adv()
            adv()
            S.op("pe", lambda e: e.matmul(PM[0:64, :], lhsT=ones65[64:65, :], rhs=rcp[64:65, :], start=True, stop=True),
                 reads=["ones65", "rcp"], writes=[("PM", 0)])
            yb = ybs[h if fused else 0]
            ybk = ("yb", h if fused else 0)
            S.op("dve", lambda e: e.tensor_tensor(yb[:], osb[0:64, :], PM[0:64, :], ALU.mult), reads=["osb", ("PM", 0)], writes=[ybk])
            if not fused:
                S.dma("sp", dr["ycT"][128 + 64 * h:192 + 64 * h, tsl], yb[:], reads=[ybk], writes=[("ycT_out", g, h)])
        if gen is not None:
            for _ in gen:
                pass
        while pf:
            pf.pop(0)()
        if fused:
            def place(g=g):
                seg = g // 4
                srcs = [(rw["ya", 0], ("r_ya", 0)), (rw["ya", 1], ("r_ya", 1)), (ybs[0], ("yb", 0)), (ybs[1], ("yb", 1))]
                for kc in range(8):
                    u = pic[0] % 2
                    pic[0] += 1
                    for si, (yt_, yk_) in enumerate(srcs):
                        S.op("pe", lambda e, si=si, yt_=yt_, u=u, kc=kc: e.matmul(PI[u][:, :], lhsT=Esel[:, si, kc * 128:(kc + 1) * 128], rhs=yt_[:],
                                                                                 start=(si == 0), stop=(si == 3)),
                             reads=["Esel", yk_], writes=[("PI", u)], inc=(si == 3))
                    st = pstg[kc % 2]
                    S.op("act" if kc % 2 == 0 else "dve",
                         (lambda e, st=st, u=u: e.activation(st[:], PI[u][:, :], AF.Copy)) if kc % 2 == 0 else
                         (lambda e, st=st, u=u: e.tensor_copy(st[:], PI[u][:, :])),
                         reads=[("PI", u)], writes=[("pstg", kc % 2)])
                    S.dma("sp", dr["rs_in"][g % 4][seg * 1024 + kc * 128:seg * 1024 + (kc + 1) * 128, :], st[:],
                          reads=[("pstg", kc % 2)], writes=[("rsin", g, kc)])
                if "on_group_done" in dr:
                    dr["on_group_done"](g)
            if g + 1 < NGm:
                pending_place[0] = place
            else:
                place()
    outs = []
    for g in range(NGm):
        if fused:
            outs += [("rsin", g, kc) for kc in range(8)]
            continue
        for h in range(2):
            if do_moba:
                outs.append(("ycT_out", g, h))
            if do_rwkv:
                outs.append(("ya_out", g, h))
    if not fused:
        S.finish(outs)
    return outs


def build_mixer_nc(Tm=8192, do_rwkv=True, do_moba=True):
    nc = bass.Bass("TRN2", target_bir_lowering=False)
    dr = {}

    def inp(name, shape, dt=F32):
        dr[name] = nc.dram_tensor(name, list(shape), dt, kind="ExternalInput").ap()
    inp("xT", [1024, Tm])
    inp("w_sel", [1024, NCOL])
    inp("ident", [128, 128])
    inp("biasT", [128, 2, 68])
    inp("cmask", [4, 128, 512], BF16)
    inp("epat", [2, 34, Tm], BF16)
    inp("qpos", [2, Tm], BF16)
    rwkv_inputs(inp)
    dr["ycT"] = nc.dram_tensor("ycT", [256, Tm], BF16, kind="ExternalOutput").ap()
    with ExitStack() as es:
        S = Sched(nc, es)
        mixer_phase(S, nc, es, Tm, dr, do_rwkv, do_moba)
        S.replay()
    return nc


def rwkv_inputs(inp):
    pass


def mixer_consts(hg, Tm):
    heads = [2 * hg, 2 * hg + 1]
    slopes = [2.0 ** (-(h + 1)) for h in heads]
    p = np.arange(128, dtype=np.float32)[:, None]
    dl = (np.arange(68, dtype=np.float32) - 64)[None, :]
    biasT = np.stack([sl * (dl * 128 + p) for sl in slopes], 1).astype(np.float32)
    k = np.arange(128)[:, None]
    q = np.arange(512)[None, :]
    cmask = np.stack([np.where(j * 128 + k <= q, 0.0, MNEG) for j in range(4)], 0).astype(np.float32)
    epat = np.zeros((2, 34, Tm), np.float32)
    for n in range(min(32, Tm // 256)):
        epat[:, n, n * 256:(n + 1) * 256] = 1.0
    for i, sl in enumerate(slopes):
        epat[i, 32, :] = -8.0 * sl * 64
        epat[i, 33, :] = -8.0 * sl
    t = np.arange(Tm) % 512
    qpos = np.stack([t // 64, t % 64], 0).astype(np.float32)
    bf = ml_dtypes.bfloat16
    return dict(biasT=biasT, cmask=cmask.astype(bf), epat=epat.astype(bf), qpos=qpos.astype(bf), ident=np.eye(128, dtype=np.float32))


def mixer_inputs(d, c, Tm=8192):
    b, hg = c // 4, c % 4
    w_in = d["w_in"]
    cols = []
    for base in (0, 512, 1024):
        cols.append(np.arange(base + hg * 128, base + hg * 128 + 128))
    cols.append(np.arange(1536, 1696))
    for base in (1696, 1696 + 512, 1696 + 1024):
        cols.append(np.arange(base + hg * 128, base + hg * 128 + 128))
    cols = np.concatenate(cols)
    m = dict(mixer_consts(hg, Tm))
    m["xT"] = np.ascontiguousarray(d["x"][b, :Tm, :].T)
    m["w_sel"] = np.ascontiguousarray(w_in[:, cols])
    return m, cols


LAM = 0.6065306597126334
GN_EPS = 64e-5


def rwkv_inputs(inp):
    inp("mu_cols", [128, 8])
    inp("pcols", [64, 2, 5])
    inp("wlu", [32, 128])
    inp("alu", [32, 128])
    inp("glu", [96, 128])
    inp("gnw", [128])
    inp("gnb", [128])
    inp("rmask", [64, 384])


def rwkv_host_inputs(d, c):
    b, hg = c // 4, c % 4
    sl = slice(hg * 128, hg * 128 + 128)
    mu = d["mu_shift"]
    mu_cols = np.zeros((128, 8), np.float32)
    for i, base in enumerate((0, 512, 1024)):
        for h in range(2):
            mu_cols[0:64, 2 * i + h] = mu[base + hg * 128 + h * 64: base + hg * 128 + h * 64 + 64]
    mu_cols[0:64, 6] = mu[1536:1600]
    mu_cols[0:96, 7] = mu[1600:1696]
    pcols = np.zeros((64, 2, 5), np.float32)
    for h in range(2):
        s2 = slice(hg * 128 + h * 64, hg * 128 + h * 64 + 64)
        pcols[:, h, 0] = d["w0"][s2]
        pcols[:, h, 1] = d["a0"][s2]
        pcols[:, h, 2] = d["k_k"][s2]
        pcols[:, h, 3] = d["k_a"][s2]
        pcols[:, h, 4] = d["r_k"].reshape(-1)[s2]
    s_ = np.arange(64)[:, None]
    t_ = np.arange(64)[None, :]
    Ms = (s_ < t_).astype(np.float32)
    Mi = (s_ <= t_).astype(np.float32)
    rmask = np.concatenate([Ms, Mi, Ms, Mi, Ms.T, Ms.T], 1).astype(np.float32)[:, :384]
    return dict(mu_cols=mu_cols, pcols=pcols, wlu=np.ascontiguousarray(d["w_lora_up"][:, sl]),
                alu=np.ascontiguousarray(d["a_lora_up"][:, sl]), glu=np.ascontiguousarray(d["g_lora_up"][:, sl]),
                gnw=np.ascontiguousarray(d["gn_w"][sl]), gnb=np.ascontiguousarray(d["gn_b"][sl]), rmask=rmask)


def rwkv_setup(S, nc, es, dr, sb):
    rw = {}
    for nm, shp in (("mu", [128, 8]), ("pc", [64, 2, 5]), ("omk", [64, 2]), ("wlu", [32, 128]), ("alu", [64, 128]), ("glu", [96, 128]),
                    ("gnw", [64, 128]), ("gnb", [64, 128]), ("rmask", [64, 384]), ("ones", [64, 64]),
                    ("l1m", [64, 512]), ("gdm", [96, 512]), ("tmp", [96, 512])):
        rw[nm] = sb("r_" + nm, shp, F32)
    rw["tw"] = rw["l1m"]
    rw["gs"] = rw["gdm"]
    rw["praw"] = sb("r_praw", [96, 513], F32)
    rw["last"] = sb("r_last", [96, 8], F32)
    for nm in ("sg", "asig", "kk", "kkn", "kmod", "E1", "E3", "rkr"):
        rw[nm, 0] = rw[nm, 1] = sb("r_%s" % nm, [64, 512], F32)
    rw["cum", 0] = rw["cum", 1] = rw["kk", 0]
    rw["E2", 0] = rw["E2", 1] = rw["sg", 0]
    for nm in ("AR", "BK", "BKh"):
        rw[nm, 0] = rw[nm, 1] = sb("r_%s" % nm, [64, 8, 128], F32)
    for h in range(2):
        for nm in ("rm", "km", "vm"):
            rw[nm, h] = sb("r_%s%d" % (nm, h), [64, 512], F32)
        rw["S", h] = sb("r_S%d" % h, [64, 64], F32)
        rw["ya", h] = sb("r_ya%d" % h, [64, 512], BF16)
    for nm, shp in (("AAm", [64, 4, 256]), ("Nt", [64, 4, 64]), ("NPg", [64, 2, 4, 128]), ("TK", [64, 4, 256]), ("TG", [64, 4, 65]),
                    ("Z", [64, 2, 4, 128]), ("Tt", [64, 2, 4, 64]), ("Afm", [64, 4, 64]), ("M", [64, 4, 64]), ("Sl", [64, 4, 64]), ("Rf", [64, 4, 64]),
                    ("y", [64, 4, 64]), ("yt", [64, 4, 64]), ("ysq", [64, 4, 64]), ("sst", [64, 6, 4])):
        rw[nm] = sb("r_" + nm, shp, F32)
    S.dma("sp", rw["mu"][:], dr["mu_cols"], writes=["r_mu"])
    S.dma("sp", rw["pc"][:], dr["pcols"], writes=["r_pc"])
    S.dma("sp", rw["wlu"][:], dr["wlu"], writes=["r_wlu"])
    S.dma("sp", rw["alu"][32:64, :], dr["alu"], writes=["r_alu"])
    S.dma("sp", rw["glu"][:], dr["glu"], writes=["r_glu"])
    S.dma("sp", rw["gnw"][:], dr["gnw"].partition_broadcast(64), writes=["r_gn"])
    S.dma("sp", rw["gnb"][:], dr["gnb"].partition_broadcast(64), writes=["r_gn"])
    S.dma("sp", rw["rmask"][:], dr["rmask"], writes=["r_rmask"])
    S.op("dve", lambda e: e.memset(rw["ones"][:], 1.0), writes=["r_ones"])
    S.op("dve", lambda e: e.tensor_scalar(rw["omk"][:], rw["pc"][:, :, 3], -1.0, 1.0, ALU.mult, ALU.add), reads=["r_pc"], writes=["r_omk"])
    S.op("pool", lambda e: e.memset(rw["last"][:], 0.0), writes=["r_last"])
    for h in range(2):
        S.op("pool", lambda e, h=h: e.memset(rw["S", h][:], 0.0), writes=[("r_S", h)])
    return rw


def rwkv_inproj(S, nc, rw, g, inproj_group, dr, ident):
    tsl = slice(g * 512, (g + 1) * 512)
    I64 = ident[0:64, 0:64]
    mu = rw["mu"]
    tmp = rw["tmp"]

    def shift_mix(gi, M, dst, dkey):
        praw = rw["praw"]
        last = rw["last"]

        def consume(P, pk):
            S.op("act", lambda e: e.activation(praw[0:M, 1:513], P[0:M, :], AF.Copy), reads=[pk], writes=["r_praw"])
            S.op("act", lambda e: e.activation(praw[0:M, 0:1], last[0:M, gi:gi + 1], AF.Copy), reads=["r_last"], writes=["r_praw"])
            S.op("dve", lambda e: e.tensor_tensor(tmp[0:M, :], praw[0:M, 0:512], praw[0:M, 1:513], ALU.subtract),
                 reads=["r_praw"], writes=["r_tmp"])
            S.op("dve", lambda e: e.scalar_tensor_tensor(dst[0:M, :], tmp[0:M, :], mu[0:M, gi:gi + 1], praw[0:M, 1:513], ALU.mult, ALU.add),
                 reads=["r_tmp", "r_mu", "r_praw"], writes=[dkey])
            S.op("act", lambda e: e.activation(last[0:M, gi:gi + 1], praw[0:M, 512:513], AF.Copy), reads=["r_praw"], writes=["r_last"])
        return consume
    for h in range(2):
        inproj_group(g, G_R + 64 * h, 64, shift_mix(0 + h, 64, rw["rm", h], ("r_rm", h)))
        inproj_group(g, G_K + 64 * h, 64, shift_mix(2 + h, 64, rw["km", h], ("r_km", h)))
        inproj_group(g, G_V + 64 * h, 64, shift_mix(4 + h, 64, rw["vm", h], ("r_vm", h)))
    inproj_group(g, G_L1, 64, shift_mix(6, 64, rw["l1m"], "r_l1m"))
    inproj_group(g, G_GD, 96, shift_mix(7, 96, rw["gdm"], "r_gdm"))


def rwkv_compute(S, nc, rw, g, dr, PR, PM, kPM, ident):
    tsl = slice(g * 512, (g + 1) * 512)
    I64 = ident[0:64, 0:64]
    tmp = rw["tmp"]
    deferred = [None]
    S.op("act", lambda e: e.activation(rw["l1m"][0:32, :], rw["l1m"][0:32, :], AF.Tanh), reads=["r_l1m"], writes=["r_l1m"])
    yield
    S.op("act", lambda e: e.activation(rw["gdm"][:], rw["gdm"][:], AF.Sigmoid), reads=["r_gdm"], writes=["r_gdm"])
    yield
    pc = rw["pc"]
    for h in range(2):
        hs = slice(h * 64, h * 64 + 64)
        rm, km, vm, sg, asig, kk, kkn, kmod, cum, E1, E2, E3, rkr = [rw[n, h] for n in
                                                                      ("rm", "km", "vm", "sg", "asig", "kk", "kkn", "kmod", "cum", "E1", "E2", "E3", "rkr")]
        AR, BK, BKh, Sst, ya = rw["AR", h], rw["BK", h], rw["BKh", h], rw["S", h], rw["ya", h]
        P0, P1 = PR[0], PR[1]
        k0, k1 = ("PR", 0), ("PR", 1)
        S.op("pe", lambda e: e.matmul(P0[0:64, :], lhsT=rw["wlu"][:, hs], rhs=rw["l1m"][0:32, :], start=True, stop=True),
             reads=["r_wlu", "r_l1m"], writes=[k0])
        S.op("act", lambda e: e.activation(sg[:], P0[0:64, :], AF.Sigmoid, bias=pc[:, h, 0:1]), reads=[k0, "r_pc"], writes=[("r_sg", 0)])
        yield
        S.op("pe", lambda e: e.matmul(P1[0:64, :], lhsT=rw["alu"][32:64, hs], rhs=rw["l1m"][32:64, :], start=True, stop=True),
             reads=["r_alu", "r_l1m"], writes=[k1])
        S.op("act", lambda e: e.activation(asig[:], P1[0:64, :], AF.Sigmoid, bias=pc[:, h, 1:2]), reads=[k1, "r_pc"], writes=[("r_asig", 0)])
        yield
        S.op("dve", lambda e: e.tensor_scalar(kk[:], km[:], pc[:, h, 2:3], None, ALU.mult), reads=[("r_km", h), "r_pc"], writes=[("r_kk", 0)])
        yield
        S.op("dve", lambda e: e.tensor_tensor(tmp[0:64, :], kk[:], kk[:], ALU.mult), reads=[("r_kk", 0)], writes=["r_tmp"])
        yield
        S.op("pe", lambda e: e.matmul(P0[0:64, :], lhsT=rw["ones"][:], rhs=tmp[0:64, :], start=True, stop=True),
             reads=["r_ones", "r_tmp"], writes=[k0])
        S.op("dve", lambda e: e.tensor_scalar(tmp[0:64, :], P0[0:64, :], 1e-18, None, ALU.max), reads=[k0], writes=["r_tmp"])
        yield
        S.op("act", lambda e: e.activation(tmp[0:64, :], tmp[0:64, :], AF.Ln), reads=["r_tmp"], writes=["r_tmp"])
        yield
        S.op("act", lambda e: e.activation(tmp[0:64, :], tmp[0:64, :], AF.Exp, scale=-0.5), reads=["r_tmp"], writes=["r_tmp"])
        yield
        S.op("dve", lambda e: e.tensor_tensor(kkn[:], kk[:], tmp[0:64, :], ALU.mult), reads=[("r_kk", 0), "r_tmp"], writes=[("r_kkn", 0)])
        yield
        S.op("dve", lambda e: e.tensor_scalar(tmp[0:64, :], asig[:], pc[:, h, 3:4], rw["omk"][:, h:h + 1], ALU.mult, ALU.add),
             reads=[("r_asig", 0), "r_pc", "r_omk"], writes=["r_tmp"])
        yield
        S.op("dve", lambda e: e.tensor_tensor(kmod[:], km[:], tmp[0:64, :], ALU.mult), reads=[("r_km", h), "r_tmp"], writes=[("r_kmod", 0)])
        yield
        S.op("dve", lambda e: e.scalar_tensor_tensor(rkr[:], rm[:], pc[:, h, 4:5], kmod[:], ALU.mult, ALU.mult),
             reads=[("r_rm", h), ("r_kmod", 0), "r_pc"], writes=[("r_rkr", 0)])
        yield
        for c in range(8):
            cs = slice(c * 64, c * 64 + 64)
            S.op("dve", lambda e, cs=cs: e.tensor_tensor_scan(cum[:, cs], rw["ones"][:], sg[:, cs], 0.0, ALU.mult, ALU.add),
                 reads=[("r_sg", 0), "r_ones"], writes=[("r_kk", 0)])
            yield
        S.op("act", lambda e: e.activation(E1[:], cum[:], AF.Exp, scale=-LAM), reads=[("r_kk", 0)], writes=[("r_E1", 0)])
        yield
        S.op("dve", lambda e: e.tensor_tensor(tmp[0:64, :], cum[:], sg[:], ALU.subtract), reads=[("r_kk", 0), ("r_sg", 0)], writes=["r_tmp"])
        yield
        S.op("act", lambda e: e.activation(E3[:], tmp[0:64, :], AF.Exp, scale=-LAM), reads=["r_tmp"], writes=[("r_E3", 0)])
        yield
        S.op("act", lambda e: e.activation(E2[:], cum[:], AF.Exp, scale=LAM), reads=[("r_kk", 0)], writes=[("r_sg", 0)])
        yield
        v3 = lambda ap: ap.rearrange("p (c t) -> p c t", c=8)
        S.op("dve", lambda e: e.scalar_tensor_tensor(AR[:, :, 0:64], v3(kkn[:]), -1.0, v3(E3[:]), ALU.mult, ALU.mult),
             reads=[("r_kkn", 0), ("r_E3", 0)], writes=[("r_AR", 0)])
        yield
        S.op("dve", lambda e: e.tensor_tensor(AR[:, :, 64:128], v3(rm[:]), v3(E1[:]), ALU.mult),
             reads=[("r_rm", h), ("r_E1", 0)], writes=[("r_AR", 0)])
        yield
        S.op("dve", lambda e: e.tensor_tensor(tmp[0:64, :], kkn[:], asig[:], ALU.mult), reads=[("r_kkn", 0), ("r_asig", 0)], writes=["r_tmp"])
        yield
        S.op("dve", lambda e: e.tensor_tensor(BK[:, :, 0:64], v3(tmp[0:64, :]), v3(E2[:]), ALU.mult), reads=["r_tmp", ("r_sg", 0)], writes=[("r_BK", 0)])
        yield
        S.op("dve", lambda e: e.tensor_tensor(BK[:, :, 64:128], v3(kmod[:]), v3(E2[:]), ALU.mult), reads=[("r_kmod", 0), ("r_sg", 0)], writes=[("r_BK", 0)])
        yield
        S.op("dve", lambda e: e.tensor_tensor(BKh[:], BK[:], v3(E1[:])[:, :, 63:64].to_broadcast([64, 8, 128]), ALU.mult),
             reads=[("r_BK", 0), ("r_E1", 0)], writes=[("r_BKh", 0)])
        yield
        Tt = rw["Tt"]
        AAm, Nt, NPg, TK, TG, Z, Afm, Mm, Sl, Rf, y, yt, ysq, sst = [rw[n] for n in
                                                                       ("AAm", "Nt", "NPg", "TK", "TG", "Z", "Afm", "M", "Sl", "Rf", "y", "yt", "ysq", "sst")]
        rmask = rw["rmask"]
        B0, B1, B2 = PR[0], PR[1], PM
        kB0, kB1, kB2 = ("PR", 0), ("PR", 1), kPM
        NB = 4
        E1v = v3(E1[:])
        rd = [("r_AR", 0), ("r_BK", 0)]

        def mm(out, lhsT, rhs, reads, wk, inc, start=True, stop=True):
            S.op("pe", lambda e: e.matmul(out, lhsT=lhsT, rhs=rhs, start=start, stop=stop), reads=reads, writes=[wk], inc=inc)
        for hb in range(2):
            c0 = hb * NB
            banks = [(B0, kB0), (B0, kB0), (B1, kB1), (B1, kB1)]
            for j in range(NB):
                c = c0 + j
                Bj, kBj = banks[j]
                off = (j % 2) * 256
                mm(Bj[0:64, off:off + 128], BK[:, c, 0:64], AR[:, c, :], rd, kBj, False)
                mm(Bj[0:64, off + 128:off + 256], BK[:, c, 64:128], AR[:, c, :], rd, kBj, j % 2 == 1)
            for b2, (Bj, kBj) in enumerate(((B0, kB0), (B1, kB1))):
                S.op("dve", lambda e, b2=b2, Bj=Bj: e.tensor_tensor(AAm[:, 2 * b2:2 * b2 + 2, :], Bj[0:64, 0:512].rearrange("p (a b) -> p a b", a=2),
                                                                  rmask[:, 0:256].unsqueeze(1).to_broadcast([64, 2, 256]), ALU.mult),
                     reads=[kBj, "r_rmask"], writes=["r_AAm"])
                yield
            for j in range(NB):
                c = c0 + j
                mm(B2[0:64, j * 64:(j + 1) * 64], AR[:, c, 0:64], BK[:, c, 0:64], rd, kB2, j == NB - 1)
            S.op("dve", lambda e: e.tensor_tensor(Nt[:], B2[0:64, 0:256].rearrange("p (a b) -> p a b", a=NB),
                                                  rmask[:, 256:320].unsqueeze(1).to_broadcast([64, NB, 64]), ALU.mult),
                 reads=[kB2, "r_rmask"], writes=["r_Nt"])
            yield
            for j in range(NB):
                c = c0 + j
                cs = slice(c * 64, c * 64 + 64)
                Bj, kBj = banks[j]
                off = (j % 2) * 256
                mm(Bj[0:64, off:off + 64], vm[:, cs], I64, [("r_vm", h), "ident"], kBj, False)
                mm(Bj[0:64, off + 64:off + 128], AR[:, c, 0:64], I64, rd + ["ident"], kBj, False)
                mm(Bj[0:64, off + 128:off + 192], BKh[:, c, 0:64], I64, [("r_BKh", 0), "ident"], kBj, False)
                mm(Bj[0:64, off + 192:off + 256], BKh[:, c, 64:128], I64, [("r_BKh", 0), "ident"], kBj, j % 2 == 1)
            for b2, (Bj, kBj) in enumerate(((B0, kB0), (B1, kB1))):
                S.op("act", lambda e, b2=b2, Bj=Bj: e.activation(TK[:, 2 * b2:2 * b2 + 2, :], Bj[0:64, 0:512].rearrange("p (a b) -> p a b", a=2), AF.Copy),
                     reads=[kBj], writes=["r_TK"])
                yield
            for j in range(NB):
                c = c0 + j
                cs = slice(c * 64, c * 64 + 64)
                mm(B2[0:64, j * 65:j * 65 + 64], rw["gdm"][:, cs], rw["glu"][:, hs], ["r_gdm", "r_glu"], kB2, False)
                mm(B2[0:64, j * 65 + 64:j * 65 + 65], rkr[:, cs], rw["ones"][:, 0:1], [("r_rkr", 0), "r_ones"], kB2, j == NB - 1)
            S.op("act", lambda e: e.activation(TG[:], B2[0:64, 0:NB * 65].rearrange("p (a b) -> p a b", a=NB), AF.Copy), reads=[kB2], writes=["r_TG"])
            yield
            if deferred[0] is not None:
                deferred[0]()
                deferred[0] = None
                yield
            for j in range(NB):
                mm(B2[0:64, j * 64:(j + 1) * 64], AAm[:, j, 128:192], TK[:, j, 0:64], ["r_AAm", "r_TK"], kB2, j == NB - 1)
            S.op("dve", lambda e: e.tensor_copy(Z[:, 0, :, 64:128], B2[0:64, 0:256].rearrange("p (a b) -> p a b", a=NB)), reads=[kB2], writes=[("r_Z", 0)])
            yield
            S.op("dve", lambda e: e.tensor_copy(Z[:, 0, :, 0:64], TK[:, :, 64:128]), reads=["r_TK"], writes=[("r_Z", 0)])
            yield
            S.op("dve", lambda e: e.tensor_tensor(Tt[:, 1], I64.unsqueeze(1).to_broadcast([64, NB, 64]), AAm[:, :, 0:64], ALU.add),
                 reads=["ident", "r_AAm"], writes=[("r_T", 1)])
            yield
            for k in range(6):
                Nk = (lambda j: AAm[:, j, 0:64]) if k == 0 else (lambda j, k=k: NPg[:, k % 2, j, 0:64])
                Ntk = (lambda j: Nt[:, j, :]) if k == 0 else (lambda j, k=k: NPg[:, k % 2, j, 64:128])
                rdk = ["r_AAm", "r_Nt"] if k == 0 else [("r_NP", k % 2)]
                if k < 5:
                    for j in range(NB):
                        if k < 4:
                            mm(B1[0:64, j * 128:j * 128 + 64], Ntk(j), Nk(j), rdk, kB1, False)
                        mm(B1[0:64, j * 128 + 64:(j + 1) * 128], Nk(j), Ntk(j), rdk, kB1, j == NB - 1)
                    S.op("act", lambda e, k=k: e.activation(NPg[:, (k + 1) % 2], B1[0:64, 0:512].rearrange("p (a b) -> p a b", a=NB), AF.Copy),
                         reads=[kB1], writes=[("r_NP", (k + 1) % 2)])
                    yield
                if k >= 1:
                    ti, to = k % 2, (k + 1) % 2
                    for j in range(NB):
                        mm(B0[0:64, j * 64:(j + 1) * 64], Ntk(j), Tt[:, ti, j, :], rdk + [("r_T", ti)], kB0, j == NB - 1)
                    S.op("dve", lambda e, ti=ti, to=to: e.tensor_tensor(Tt[:, to], B0[0:64, 0:NB * 64].rearrange("p (a b) -> p a b", a=NB), Tt[:, ti], ALU.add),
                         reads=[kB0, ("r_T", ti)], writes=[("r_T", to)])
                    yield
            for j in range(NB):
                mm(B0[0:64, j * 128:(j + 1) * 128], Tt[:, 0, j, :], Z[:, 0, j, :], [("r_T", 0), ("r_Z", 0)], kB0, j == NB - 1)
            S.op("act", lambda e: e.activation(Z[:, 1], B0[0:64, 0:512].rearrange("p (a b) -> p a b", a=NB), AF.Copy), reads=[kB0], writes=[("r_Z", 1)])
            yield
            Zf = Z[:, 1]
            zk = [("r_Z", 1)]
            for j in range(NB):
                Ah, ul = Zf[:, j, 0:64], Zf[:, j, 64:128]
                vt, bh, kh = TK[:, j, 0:64], TK[:, j, 128:192], TK[:, j, 192:256]
                mm(B0[0:64, 256 + j * 64:256 + (j + 1) * 64], Ah, bh, zk + ["r_TK"], kB0, False)
                mm(B1[0:64, j * 64:(j + 1) * 64], bh, ul, zk + ["r_TK"], kB1, False, start=True, stop=False)
                mm(B1[0:64, j * 64:(j + 1) * 64], kh, vt, ["r_TK"], kB1, False, start=False, stop=True)
                mm(B1[0:64, 256 + j * 64:256 + (j + 1) * 64], Ah, AAm[:, j, 64:128], zk + ["r_AAm"], kB1, j == NB - 1)
            v4 = lambda ap: ap.rearrange("p (a b) -> p a b", a=NB)
            S.op("dve", lambda e: e.tensor_tensor(Mm[:], I64.unsqueeze(1).to_broadcast([64, NB, 64]),
                                                  E1v[:, c0:c0 + NB, 63:64].to_broadcast([64, NB, 64]), ALU.mult),
                 reads=["ident", ("r_E1", 0)], writes=["r_M"])
            yield
            S.op("dve", lambda e: e.tensor_tensor(Mm[:], Mm[:], v4(B0[0:64, 256:512]), ALU.add), reads=[kB0, "r_M"], writes=["r_M"])
            yield
            S.op("act", lambda e: e.activation(Sl[:], v4(B1[0:64, 0:256]), AF.Copy), reads=[kB1], writes=["r_Sl"])
            yield
            S.op("dve", lambda e: e.tensor_tensor(Rf[:], v4(B1[0:64, 256:512]), AR[:, c0:c0 + NB, 64:128], ALU.add), reads=[kB1] + rd, writes=["r_Rf"])
            yield
            for j in range(NB):
                yo = B0[0:64, j * 64:(j + 1) * 64]
                mm(yo, Rf[:, j, :], Sst[:], ["r_Rf", ("r_S", h)], kB0, False, start=True, stop=False)
                mm(yo, AAm[:, j, 64:128], Zf[:, j, 64:128], zk + ["r_AAm"], kB0, False, start=False, stop=False)
                mm(yo, AAm[:, j, 192:256], TK[:, j, 0:64], ["r_AAm", "r_TK"], kB0, False, start=False, stop=True)
                mm(B2[0:64, 0:64], Mm[:, j, :], Sst[:], ["r_M", ("r_S", h)], kB2, True)
                S.op("dve", lambda e, j=j: e.tensor_tensor(Sst[:], B2[0:64, 0:64], Sl[:, j, :], ALU.add), reads=[kB2, "r_Sl"], writes=[("r_S", h)])
                yield
            S.op("act", lambda e: e.activation(y[:], v4(B0[0:64, 0:256]), AF.Copy), reads=[kB0], writes=["r_y"])
            yield
            b3 = lambda ap: ap.unsqueeze(2).to_broadcast([64, NB, 64])
            dv = lambda fn, r_, w_: S.op("dve", fn, reads=r_, writes=w_)
            dv(lambda e: e.tensor_reduce(sst[:, 0, :], y[:], AX.X, ALU.add), ["r_y"], ["r_sst"])
            yield
            dv(lambda e: e.tensor_tensor(ysq[:], y[:], y[:], ALU.mult), ["r_y"], ["r_ysq"])
            yield
            dv(lambda e: e.tensor_reduce(sst[:, 1, :], ysq[:], AX.X, ALU.add), ["r_ysq"], ["r_sst"])
            yield
            dv(lambda e: e.tensor_scalar(sst[:, 2, :], sst[:, 0, :], 1.0 / 64, None, ALU.mult), ["r_sst"], ["r_sst"])
            yield
            dv(lambda e: e.tensor_tensor(sst[:, 3, :], sst[:, 2, :], sst[:, 2, :], ALU.mult), ["r_sst"], ["r_sst"])
            yield
            dv(lambda e: e.scalar_tensor_tensor(sst[:, 4, :], sst[:, 1, :], 1.0 / 64, sst[:, 3, :], ALU.mult, ALU.subtract), ["r_sst"], ["r_sst"])
            yield
            dv(lambda e: e.tensor_scalar(sst[:, 4, :], sst[:, 4, :], GN_EPS, None, ALU.add), ["r_sst"], ["r_sst"])
            yield
            S.op("act", lambda e: e.activation(sst[:, 5, :], sst[:, 4, :], AF.Ln), reads=["r_sst"], writes=["r_sst"])
            yield
            S.op("act", lambda e: e.activation(sst[:, 5, :], sst[:, 5, :], AF.Exp, scale=-0.5), reads=["r_sst"], writes=["r_sst"])
            yield
            dv(lambda e: e.tensor_tensor(yt[:], y[:], b3(sst[:, 2, :]), ALU.subtract), ["r_y", "r_sst"], ["r_yt"])
            yield
            dv(lambda e: e.tensor_tensor(yt[:], yt[:], b3(sst[:, 5, :]), ALU.mult), ["r_yt", "r_sst"], ["r_yt"])
            yield
            dv(lambda e: e.tensor_tensor(yt[:], yt[:], rw["gnw"][:, hs].unsqueeze(1).to_broadcast([64, NB, 64]), ALU.mult), ["r_yt", "r_gn"], ["r_yt"])
            yield
            dv(lambda e: e.tensor_tensor(yt[:], yt[:], rw["gnb"][:, hs].unsqueeze(1).to_broadcast([64, NB, 64]), ALU.add), ["r_yt", "r_gn"], ["r_yt"])
            yield
            dv(lambda e: e.tensor_tensor(ysq[:], TK[:, :, 0:64], TG[:, :, 64:65].to_broadcast([64, NB, 64]), ALU.mult), ["r_TK", "r_TG"], ["r_ysq"])
            yield
            dv(lambda e: e.tensor_tensor(yt[:], yt[:], ysq[:], ALU.add), ["r_yt", "r_ysq"], ["r_yt"])
            yield
            dv(lambda e: e.tensor_tensor(yt[:], yt[:], TG[:, :, 0:64], ALU.mult), ["r_yt", "r_TG"], ["r_yt"])
            yield
            def fin(c0=c0, ya=ya, h=h):
                for j in range(NB):
                    mm(B1[0:64, j * 64:(j + 1) * 64], yt[:, j, :], I64, ["r_yt", "ident"], kB1, j == NB - 1)
                S.op("act", lambda e: e.activation(ya[:, c0 * 64:(c0 + NB) * 64], B1[0:64, 0:NB * 64], AF.Copy), reads=[kB1], writes=[("r_ya", h)])
            deferred[0] = fin
        if h == 1 or "ycT" in dr:
            if deferred[0] is not None:
                deferred[0]()
                deferred[0] = None
                yield
        if "ycT" in dr:
            S.dma("sp", dr["ycT"][64 * h:64 * h + 64, tsl], ya[:], reads=[("r_ya", h)], writes=[("ya_out", g, h)])


def build_fused_nc():
    Tm = 8192
    nc = bass.Bass("TRN2", target_bir_lowering=False)
    dr = {}

    def inp(name, shape, dt=F32):
        dr[name] = nc.dram_tensor(name, list(shape), dt, kind="ExternalInput").ap()
    inp("xT", [1024, Tm])
    inp("w_sel", [1024, NCOL])
    inp("ident", [128, 128])
    inp("biasT", [128, 2, 68])
    inp("cmask", [4, 128, 512], BF16)
    inp("epat", [2, 34, Tm], BF16)
    inp("qpos", [2, Tm], BF16)
    inp("esel", [64, 4, 1024], BF16)
    rwkv_inputs(inp)
    inp("x_tok", [2048, 1024])
    inp("w_out", [1024, 1024])
    for nm in ("ln1_g", "ln1_b", "ln2_g", "ln2_b"):
        inp(nm, [1024])
    inp("w_route", [1024, 36])
    inp("b_route", [36])
    inp("w1", [32, 1024, 256])
    inp("w3", [32, 1024, 256])
    inp("w2", [32, 256, 1024])
    dr["out"] = nc.dram_tensor("out", [2048, 1024], F32, kind="ExternalOutput").ap()
    rs_in = [nc.dram_tensor("rs_in%d" % q, [4096, 512], BF16).ap() for q in range(4)]
    rs_out = [nc.dram_tensor("rs_out%d" % q, [1024, 512], BF16).ap() for q in range(4)]
    dr["rs_in"] = rs_in
    with ExitStack() as es0:
        S = Sched(nc, es0)

        def on_group_done(g):
            if g < 12:
                return
            q = g - 12
            S.coll(lambda e: e.collective_compute("ReduceScatter", ALU.add, replica_groups=[[0, 1, 2, 3], [4, 5, 6, 7]],
                                                  ins=[rs_in[q]], outs=[rs_out[q]]),
                   reads=[("rsin", gg, kc) for gg in (q, 4 + q, 8 + q, 12 + q) for kc in range(8)], writes=[("rs_out", q)])
        dr["on_group_done"] = on_group_done
        with ExitStack() as es1:
            mixer_phase(S, nc, es1, Tm, dr, True, True, fused=True)
            S.barrier(include_coll=False)
            S.replay()
        dr["ycat_q"] = rs_out
        with ExitStack() as es2:
            token_phase(S, nc, es2, 2048, dr)
            S.replay()
    return nc


def kernel(**inputs):
    d = {k: np.ascontiguousarray(np.asarray(v)) for k, v in inputs.items()}
    Tm = 8192
    nc = build_fused_nc()
    w_route = np.ascontiguousarray(np.concatenate([d["w_group"], d["w_expert"]], 1))
    b_route = np.ascontiguousarray(np.concatenate([d["b_group"], d["b_expert"]], 0))
    common = dict(w_out=d["w_out"], ln1_g=d["ln1_g"], ln1_b=d["ln1_b"], ln2_g=d["ln2_g"], ln2_b=d["ln2_b"],
                  w_route=w_route, b_route=b_route, w1=d["w1_exp"].reshape(32, 1024, 256),
                  w3=d["w3_exp"].reshape(32, 1024, 256), w2=d["w2_exp"].reshape(32, 256, 1024))
    maps = []
    for c in range(8):
        b, hg = c // 4, c % 4
        m, _ = mixer_inputs(d, c, Tm)
        m.update(rwkv_host_inputs(d, c))
        m.update(common)
        esel = np.zeros((64, 4, 1024), np.float32)
        i = np.arange(64)
        for si, base in enumerate((hg * 128, hg * 128 + 64, 512 + hg * 128, 512 + hg * 128 + 64)):
            esel[i, si, base + i] = 1.0
        m["esel"] = esel.astype(ml_dtypes.bfloat16)
        off = hg * 2048
        m["x_tok"] = np.ascontiguousarray(d["x"][b, off:off + 2048, :])
        maps.append(m)
    res = run_bass_kernel_spmd(nc, maps, core_ids=list(range(8)))
    out = np.concatenate([r["out"] for r in res.results], 0).reshape(2, Tm, 1024)
    return out.astype(np.float32)
```
